# Optimizing a Trainium2 kernel written in Bass

```python
import math
import jax, jax.numpy as jnp
from jax import lax
import numpy as np

D_MODEL = 4096
BATCH = 4
SEQ = 4096
DEPTH = 2

GRID_W = 64
CTX_LEN = 256

C_CONV = 1024
CONV_W = 31
N_HEADS_R = 16
HEAD_R = 64
C_R = N_HEADS_R * HEAD_R
W_LORA = 64
A_LORA = 64
G_LORA = 160
GN_EPS = 64e-5
N_HEADS_M = 16
NOPE = 128
ROPE = 64
V_HEAD = 128
Q_LORA = 1536
KV_LORA = 512
C_M = N_HEADS_M * V_HEAD
MIX = C_CONV + C_R + C_M
ROPE_BASE = 10000.0
ATTN_SCALE = (NOPE + ROPE) ** -0.5
Q_BLOCK = 128

OFF_CONV = 0
OFF_MQ = OFF_CONV + 2 * C_CONV
OFF_RR = OFF_MQ + Q_LORA
OFF_RG = OFF_RR + C_R
OFF_RK = OFF_RG + G_LORA
OFF_RV = OFF_RK + C_R
OFF_RW = OFF_RV + C_R
OFF_RA = OFF_RW + 2 * W_LORA
OFF_MKV = OFF_RA + 2 * A_LORA
OFF_MKR = OFF_MKV + KV_LORA
IN_COLS = OFF_MKR + ROPE
RWKV_COLS = OFF_MKV - OFF_RR

N_EXPERTS = 16
N_GROUPS = 4
EXPERTS_PER_GROUP = N_EXPERTS // N_GROUPS
TOP_K = 2
D_EXPERT = 1024
MOE_BLOCK = 128

kernel_name = "hybrid_dit_conv_rwkv7_mla_moe"


def rmsnorm(x, g, eps=1e-6):
    xf = x.astype(jnp.float32)
    y = xf * lax.rsqrt(jnp.mean(xf * xf, -1, keepdims=True) + eps)
    return (y * g.astype(jnp.float32)).astype(x.dtype)


def layernorm(x, g, b, eps=1e-5):
    xf = x.astype(jnp.float32)
    mu = jnp.mean(xf, -1, keepdims=True)
    var = jnp.mean(jnp.square(xf - mu), -1, keepdims=True)
    return ((xf - mu) * lax.rsqrt(var + eps) * g.astype(jnp.float32) + b.astype(jnp.float32)).astype(x.dtype)


def col(p, base, lo, hi):
    return p[..., lo - base:hi - base]


def rope2d_tables(rows):
    half = ROPE // 2
    inv_freq = ROPE_BASE ** (-jnp.arange(0, half, 2, dtype=jnp.float32) / half)
    r = jnp.repeat(jnp.arange(rows, dtype=jnp.float32), GRID_W)
    cl = jnp.tile(jnp.arange(GRID_W, dtype=jnp.float32), rows)
    ar = r[:, None] * inv_freq
    ac = cl[:, None] * inv_freq
    ang = jnp.concatenate([ar, ar, ac, ac], -1)
    return jnp.cos(ang), jnp.sin(ang)


def apply_rope2d(x, cos, sin):
    x1, x2, x3, x4 = jnp.split(x, 4, axis=-1)
    rot = jnp.concatenate([-x2, x1, -x4, x3], -1)
    return (x * cos + rot * sin).astype(x.dtype)


def conformer_conv(u, dw, db, ln_g, ln_b):
    a, gate = jnp.split(u, 2, axis=-1)
    hdn = a * jax.nn.sigmoid(gate)
    hdn = lax.conv_general_dilated(hdn, dw[:, None, :], window_strides=(1,),
                                   padding=((CONV_W // 2, CONV_W // 2),),
                                   dimension_numbers=("NWC", "WIO", "NWC"),
                                   feature_group_count=C_CONV) + db
    return jax.nn.silu(layernorm(hdn, ln_g, ln_b))


def centred_shift(p, mu_prev, mu_next):
    prev = jnp.pad(p, ((0, 0), (1, 0), (0, 0)))[:, :-1]
    nxt = jnp.pad(p, ((0, 0), (0, 1), (0, 0)))[:, 1:]
    return p + mu_prev * (prev - p) + mu_next * (nxt - p)


def to_heads(t):
    return t.reshape(t.shape[:-1] + (N_HEADS_R, HEAD_R))


def rwkv_features(pr, base, rp):
    k = col(pr, base, OFF_RK, OFF_RV)
    v = col(pr, base, OFF_RV, OFF_RW)
    wd = jnp.tanh(col(pr, base, OFF_RW, OFF_RA))
    wd = wd.reshape(wd.shape[:-1] + (2, W_LORA))
    ad = col(pr, base, OFF_RA, OFF_MKV)
    ad = ad.reshape(ad.shape[:-1] + (2, A_LORA))
    w = -jax.nn.softplus(-(rp["w0"] + jnp.einsum("bldr,drc->bldc", wd, rp["w2"]))) - 0.5
    a = jax.nn.sigmoid(rp["a0"] + jnp.einsum("bldr,drc->bldc", ad, rp["a2"]))
    kk = to_heads(k * rp["kk"])
    kk = kk / jnp.maximum(jnp.sqrt(jnp.sum(kk * kk, -1, keepdims=True)), 1e-12)
    k_eff = k[:, :, None] * (1.0 + (a - 1.0) * rp["ka"])
    return {"decay": to_heads(jnp.exp(-jnp.exp(w))),
            "kk": kk,
            "kka": kk[:, :, None] * to_heads(a),
            "k": to_heads(k_eff),
            "v": to_heads(v)}


def wkv_scan(S0, f, d, r, reverse):
    tm = lambda t: jnp.moveaxis(t, 1, 0)
    xs = (tm(f["decay"][:, :, d]), tm(f["kk"]), tm(f["kka"][:, :, d]), tm(f["k"][:, :, d]),
          tm(f["v"]), None if r is None else tm(r))

    def step(S, inp):
        w_t, kk_t, kka_t, k_t, v_t, r_t = inp
        S = (S * w_t[:, :, None, :]
             - jnp.einsum("bhij,bhj->bhi", S, kk_t)[..., None] * kka_t[:, :, None, :]
             + v_t[..., None] * k_t[:, :, None, :])
        y = None if r_t is None else jnp.einsum("bhij,bhj->bhi", S, r_t)
        return S, y

    S, ys = lax.scan(step, S0, xs, reverse=reverse)
    return S, (None if ys is None else jnp.moveaxis(ys, 0, 1))


def rwkv_mixer(pr, base, S0_fwd, S0_bwd, with_out, rp):
    f = rwkv_features(pr, base, rp)
    r = to_heads(col(pr, base, OFF_RR, OFF_RG)) if with_out else None
    S_f, y_f = wkv_scan(S0_fwd, f, 0, r, reverse=False)
    S_b, y_b = wkv_scan(S0_bwd, f, 1, r, reverse=True)
    if not with_out:
        return S_f, S_b, None
    y = y_f + y_b
    mu = jnp.mean(y, -1, keepdims=True)
    var = jnp.mean(jnp.square(y - mu), -1, keepdims=True)
    yn = ((y - mu) * lax.rsqrt(var + GN_EPS)).reshape(y.shape[:2] + (C_R,)) * rp["ln_g"] + rp["ln_b"]
    bonus = jnp.sum(jnp.sum(r[:, :, None] * f["k"] * rp["rk"], -1, keepdims=True) * f["v"][:, :, None], axis=2)
    g = jax.nn.sigmoid(col(pr, base, OFF_RG, OFF_RK)) @ rp["g2"]
    return S_f, S_b, (yn + bonus.reshape(yn.shape)) * g


def mla_keys(p, base, mp, cos, sin):
    B, L = p.shape[:2]
    ckv = rmsnorm(col(p, base, OFF_MKV, OFF_MKR), mp["ckv_g"])
    kv = (ckv @ mp["w_ukv"]).reshape(B, L, N_HEADS_M, NOPE + V_HEAD)
    k_nope = rmsnorm(kv[..., :NOPE], mp["kn_g"])
    k_rope = rmsnorm(col(p, base, OFF_MKR, IN_COLS), mp["kr_g"])
    if cos is not None:
        k_rope = apply_rope2d(k_rope, cos, sin)
    return k_nope, k_rope, kv[..., NOPE:]


def mla_queries(p, base, mp, cos, sin):
    B, L = p.shape[:2]
    cq = rmsnorm(col(p, base, OFF_MQ, OFF_RR), mp["cq_g"])
    q = (cq @ mp["w_uq"]).reshape(B, L, N_HEADS_M, NOPE + ROPE)
    q_nope = rmsnorm(q[..., :NOPE], mp["qn_g"])
    q_rope = rmsnorm(q[..., NOPE:], mp["qr_g"])
    if cos is not None:
        q_rope = apply_rope2d(q_rope, cos[:, None], sin[:, None])
    return q_nope, q_rope


def attend(q_nope, q_rope, k_nope, k_rope, v):
    s = (jnp.einsum("bqhd,bkhd->bhqk", q_nope, k_nope)
         + jnp.einsum("bqhr,bkr->bhqk", q_rope, k_rope))
    pr = jax.nn.softmax(s.astype(jnp.float32) * ATTN_SCALE, axis=-1).astype(v.dtype)
    return jnp.einsum("bhqk,bkhd->bqhd", pr, v)


def latent_attention(q_nope, q_rope, k_nope, k_rope, v):
    B, L, H, _ = q_nope.shape
    nb = L // Q_BLOCK
    blk = lambda t: jnp.moveaxis(t.reshape((B, nb, Q_BLOCK) + t.shape[2:]), 1, 0)
    out = lax.map(lambda qs: attend(qs[0], qs[1], k_nope, k_rope, v), (blk(q_nope), blk(q_rope)))
    return jnp.moveaxis(out, 0, 1).reshape(B, L, H * V_HEAD)


def route(tok, w_router, router_bias):
    scores = jax.nn.sigmoid((tok @ w_router).astype(jnp.float32))
    biased = (scores + router_bias.astype(jnp.float32)).reshape(-1, N_GROUPS, EXPERTS_PER_GROUP)
    group_score = jnp.sum(lax.top_k(biased, 2)[0], -1)
    chosen = jnp.argmax(group_score, -1)
    in_group = jnp.arange(N_GROUPS)[None, :] == chosen[:, None]
    masked = jnp.where(in_group[:, :, None], biased, -jnp.inf).reshape(-1, N_EXPERTS)
    _, idx = lax.top_k(masked, TOP_K)
    w = jnp.take_along_axis(scores, idx, -1)
    return idx, w / jnp.sum(w, -1, keepdims=True)


def moe_ffn(tok, w_router, router_bias, w1, w3, w2):
    T, D = tok.shape
    idx, wts = route(tok, w_router, router_bias)
    M = T * TOP_K
    flat_e = idx.reshape(-1)
    flat_t = jnp.repeat(jnp.arange(T, dtype=jnp.int32), TOP_K)
    flat_w = wts.reshape(-1)
    order = jnp.argsort(flat_e)
    se, st, sw = flat_e[order], flat_t[order], flat_w[order]
    counts = jnp.bincount(flat_e, length=N_EXPERTS)
    starts = jnp.cumsum(counts) - counts
    pcounts = (counts + MOE_BLOCK - 1) // MOE_BLOCK * MOE_BLOCK
    pends = jnp.cumsum(pcounts)
    pstarts = pends - pcounts
    dest = pstarts[se] + jnp.arange(M) - starts[se]
    n_blocks = -(-M // MOE_BLOCK) + N_EXPERTS
    P = n_blocks * MOE_BLOCK
    row_tok = jnp.zeros((P,), jnp.int32).at[dest].set(st)
    row_w = jnp.zeros((P,), jnp.float32).at[dest].set(sw)
    block_e = jnp.clip(jnp.searchsorted(pends, jnp.arange(n_blocks) * MOE_BLOCK, side="right"),
                       0, N_EXPERTS - 1)

    def expert_block(args):
        e, toks = args
        xb = tok[toks]
        hb = jax.nn.silu(xb @ w1[e]) * (xb @ w3[e])
        return hb @ w2[e]

    yb = lax.map(expert_block, (block_e, row_tok.reshape(n_blocks, MOE_BLOCK))).reshape(P, D)
    return jax.ops.segment_sum(yb * row_w[:, None].astype(yb.dtype), row_tok, num_segments=T)


def setup_inputs(seed: int = 0) -> dict:
    key = jax.random.key(seed)
    ks = jax.random.split(key, 38)
    f32 = jnp.float32
    Lr, D = DEPTH, D_MODEL

    def nrm(i, shape, scale=1.0):
        return jax.random.normal(ks[i], shape, f32) * scale

    def gain(i, shape):
        return 1.0 + nrm(i, shape, 0.02)

    return {
        "x": nrm(0, (BATCH, SEQ, D)),
        "c": nrm(1, (BATCH, D)),
        "ctx": nrm(2, (BATCH, CTX_LEN, D)),
        "c_ctx": nrm(3, (D,)),
        "w_mod": nrm(4, (Lr, D, 6 * D), 0.5 * D ** -0.5),
        "b_mod": nrm(5, (Lr, 6 * D), 0.02),
        "norm1_g": gain(6, (Lr, D)),
        "norm2_g": gain(7, (Lr, D)),
        "w_in": nrm(8, (Lr, D, IN_COLS), D ** -0.5),
        "w_out": nrm(9, (Lr, MIX, D), MIX ** -0.5),
        "conv_dw": nrm(10, (Lr, CONV_W, C_CONV), CONV_W ** -0.5),
        "conv_b": nrm(11, (Lr, C_CONV), 0.02),
        "conv_ln_g": gain(12, (Lr, C_CONV)),
        "conv_ln_b": nrm(13, (Lr, C_CONV), 0.02),
        "r_mu": jax.random.uniform(ks[14], (Lr, 2, RWKV_COLS), f32, 0.0, 0.5),
        "r_w0": jax.random.uniform(ks[15], (Lr, 2, C_R), f32, -6.0, 1.0),
        "r_w2": nrm(16, (Lr, 2, W_LORA, C_R), 0.1 * W_LORA ** -0.5),
        "r_a0": nrm(17, (Lr, 2, C_R), 0.1),
        "r_a2": nrm(18, (Lr, 2, A_LORA, C_R), 0.1 * A_LORA ** -0.5),
        "r_g2": nrm(19, (Lr, G_LORA, C_R), G_LORA ** -0.5),
        "r_kk": 0.85 + nrm(20, (Lr, C_R), 0.02),
        "r_ka": gain(21, (Lr, C_R)),
        "r_rk": nrm(22, (Lr, N_HEADS_R, HEAD_R), 0.1),
        "r_ln_g": gain(23, (Lr, C_R)),
        "r_ln_b": nrm(24, (Lr, C_R), 0.02),
        "m_cq_g": gain(25, (Lr, Q_LORA)),
        "m_w_uq": nrm(26, (Lr, Q_LORA, N_HEADS_M * (NOPE + ROPE)), Q_LORA ** -0.5),
        "m_ckv_g": gain(27, (Lr, KV_LORA)),
        "m_w_ukv": nrm(28, (Lr, KV_LORA, N_HEADS_M * (NOPE + V_HEAD)), KV_LORA ** -0.5),
        "m_qn_g": gain(29, (Lr, NOPE)),
        "m_qr_g": gain(30, (Lr, ROPE)),
        "m_kn_g": gain(31, (Lr, NOPE)),
        "m_kr_g": gain(32, (Lr, ROPE)),
        "w_router": nrm(33, (D, N_EXPERTS), D ** -0.5),
        "router_bias": nrm(34, (N_EXPERTS,), 0.01),
        "moe_w1": nrm(35, (Lr, N_EXPERTS, D, D_EXPERT), D ** -0.5),
        "moe_w3": nrm(36, (Lr, N_EXPERTS, D, D_EXPERT), D ** -0.5),
        "moe_w2": nrm(37, (Lr, N_EXPERTS, D_EXPERT, D), D_EXPERT ** -0.5),
    }


def reference(x, c, ctx, c_ctx, w_mod, b_mod, norm1_g, norm2_g, w_in, w_out,
              conv_dw, conv_b, conv_ln_g, conv_ln_b,
              r_mu, r_w0, r_w2, r_a0, r_a2, r_g2, r_kk, r_ka, r_rk, r_ln_g, r_ln_b,
              m_cq_g, m_w_uq, m_ckv_g, m_w_ukv, m_qn_g, m_qr_g, m_kn_g, m_kr_g,
              w_router, router_bias, moe_w1, moe_w3, moe_w2):
    B, L, D = x.shape
    Lc = ctx.shape[1]
    rows = L // GRID_W
    cos, sin = rope2d_tables(rows)
    zero_state = jnp.zeros((B, N_HEADS_R, HEAD_R, HEAD_R), jnp.float32)
    h, hc = x, ctx
    for l in range(DEPTH):
        last = l == DEPTH - 1
        mod = jax.nn.silu(c) @ w_mod[l] + b_mod[l]
        sh1, sc1, ga1, sh2, sc2, ga2 = jnp.split(mod[:, None, :], 6, axis=-1)
        n_cm = 2 if last else 6
        cmods = jnp.split(jax.nn.silu(c_ctx) @ w_mod[l][:, :n_cm * D] + b_mod[l][:n_cm * D], n_cm)

        n = rmsnorm(h, norm1_g[l]) * (1.0 + sc1) + sh1
        nc = rmsnorm(hc, norm1_g[l]) * (1.0 + cmods[1]) + cmods[0]
        cbase = OFF_RK if last else OFF_CONV
        p = n @ w_in[l]
        pc = nc @ w_in[l][:, cbase:]

        conv_lat = conformer_conv(p[..., OFF_CONV:OFF_MQ], conv_dw[l], conv_b[l], conv_ln_g[l], conv_ln_b[l])

        rp = {"w0": r_w0[l], "w2": r_w2[l], "a0": r_a0[l], "a2": r_a2[l], "g2": r_g2[l],
              "kk": r_kk[l], "ka": r_ka[l], "rk": r_rk[l], "ln_g": r_ln_g[l], "ln_b": r_ln_b[l]}
        rbase = max(cbase, OFF_RR)
        pr_c = centred_shift(col(pc, cbase, rbase, OFF_MKV).astype(jnp.float32),
                             r_mu[l][0, rbase - OFF_RR:], r_mu[l][1, rbase - OFF_RR:])
        S_f, S_b, rw_ctx = rwkv_mixer(pr_c, rbase, zero_state, zero_state, not last, rp)
        pr = centred_shift(p[..., OFF_RR:OFF_MKV].astype(jnp.float32), r_mu[l][0], r_mu[l][1])
        _, _, rw_lat = rwkv_mixer(pr, OFF_RR, S_f, S_b, True, rp)

        mp = {"cq_g": m_cq_g[l], "w_uq": m_w_uq[l], "ckv_g": m_ckv_g[l], "w_ukv": m_w_ukv[l],
              "qn_g": m_qn_g[l], "qr_g": m_qr_g[l], "kn_g": m_kn_g[l], "kr_g": m_kr_g[l]}
        kn_c, kr_c, v_c = mla_keys(pc, cbase, mp, None, None)
        kn, kr, v = mla_keys(p, 0, mp, cos, sin)
        qn, qr = mla_queries(p, 0, mp, cos, sin)
        mla_lat = latent_attention(qn, qr, jnp.concatenate([kn_c, kn], 1),
                                   jnp.concatenate([kr_c, kr], 1), jnp.concatenate([v_c, v], 1))

        mix = jnp.concatenate([conv_lat, rw_lat.astype(h.dtype), mla_lat], -1) @ w_out[l]
        h = h + ga1 * mix
        n2 = rmsnorm(h, norm2_g[l]) * (1.0 + sc2) + sh2
        if last:
            h = h + ga2 * moe_ffn(n2.reshape(B * L, D), w_router, router_bias,
                                  moe_w1[l], moe_w3[l], moe_w2[l]).reshape(B, L, D)
        else:
            conv_ctx = conformer_conv(pc[..., OFF_CONV:OFF_MQ], conv_dw[l], conv_b[l], conv_ln_g[l], conv_ln_b[l])
            qn_c, qr_c = mla_queries(pc, 0, mp, None, None)
            mla_ctx = attend(qn_c, qr_c, kn_c, kr_c, v_c).reshape(B, Lc, C_M)
            mix_c = jnp.concatenate([conv_ctx, rw_ctx.astype(hc.dtype), mla_ctx], -1) @ w_out[l]
            hc = hc + cmods[2] * mix_c
            n2c = rmsnorm(hc, norm2_g[l]) * (1.0 + cmods[4]) + cmods[3]
            y = moe_ffn(jnp.concatenate([n2.reshape(B * L, D), n2c.reshape(B * Lc, D)], 0),
                        w_router, router_bias, moe_w1[l], moe_w3[l], moe_w2[l])
            h = h + ga2 * y[:B * L].reshape(B, L, D)
            hc = hc + cmods[5] * y[B * L:].reshape(B, Lc, D)
    return h
```

```python
import contextlib
import numpy as np
import ml_dtypes
import concourse.bass as bass
import concourse.mybir as mybir
from concourse.bass_utils import run_bass_kernel_spmd

F32 = mybir.dt.float32
BF16 = mybir.dt.bfloat16
I32 = mybir.dt.int32
AF = mybir.ActivationFunctionType
ALU = mybir.AluOpType
AX = mybir.AxisListType

ENGS = ("pe", "act", "dve", "pool", "sp")

D = 4096
T = 4352
MT = 2176
LC = 256
LL = 4096
NCOL = 5088
COLS = {}
_off = 0
for _n, _s in (("ca", 512), ("cg", 512), ("mq", 1536), ("rr", 512), ("rk", 512), ("rv", 512), ("rw", 128),
               ("ra", 128), ("mkv", 512), ("rg", 160), ("mkr", 64)):
    COLS[_n] = (_off, _s)
    _off += _s
assert _off == NCOL
CHUNKS = [(i * 128, 128) for i in range(39)] + [(4992, 32), (5024, 64)]
PAIRS = [[0, 1], [2, 3], [4, 5], [6, 7]]
QUADS = [[0, 1, 2, 3], [4, 5, 6, 7]]
P4 = [[0, 4], [1, 5], [2, 6], [3, 7]]


import os
RW_LEVEL = int(os.environ.get("RW_LEVEL", "9"))
RW_NBLK = int(os.environ.get("RW_NBLK", "99"))
RW_BAR = int(os.environ.get("RW_BAR", "0"))
RW_NFIN = int(os.environ.get("RW_NFIN", "99"))
RW_NLEV = int(os.environ.get("RW_NLEV", "6"))


class Buf:
    __slots__ = ("name", "lw", "rd", "dsem", "dcount", "t")

    def __init__(self, name, t=None):
        self.name = name
        self.t = t
        self.lw = []
        self.rd = []
        self.dsem = None
        self.dcount = 0

    def __getitem__(self, k):
        return self.t[k]


class Sched:
    def __init__(self, nc):
        self.nc = nc
        self.ops = {e: [] for e in ENGS}
        self.ndsem = 0
        self.stacks = [contextlib.ExitStack()]
        self.n_tiles = 0
        self.dbufs = []
        self.free_sems = []
        self.scope_sems = [[]]
        self.lane = 0

    def sb(self, shape, dtype, name="t"):
        self.n_tiles += 1
        t = self.stacks[-1].enter_context(self.nc.sbuf_tensor(f"{name}_{self.n_tiles}", list(shape), dtype))
        return Buf(name, t)

    def ps(self, shape, dtype, name="p"):
        self.n_tiles += 1
        t = self.stacks[-1].enter_context(self.nc.psum_tensor(f"{name}_{self.n_tiles}", list(shape), dtype))
        return Buf(name, t)

    def dram(self, name, shape, dtype, kind="Internal"):
        t = self.nc.dram_tensor(name, list(shape), dtype, kind=kind).ap()
        return Buf(name, t)

    def push(self):
        self.stacks.append(contextlib.ExitStack())
        self.scope_sems.append([])

    def pop(self):
        self.barrier()
        self.stacks.pop().close()
        for b in self.scope_sems.pop():
            if b.dsem is not None:
                self.free_sems.append((b.dsem, b.dcount))
                self.dbufs.remove(b)
                b.dsem = None

    def _events_for(self, r, w):
        ev = []
        for b in r:
            ev.extend(b.lw)
        for b in w:
            ev.extend(b.lw)
            ev.extend(b.rd)
        return ev

    def op(self, eng, fn, r=(), w=()):
        idx = len(self.ops[eng])
        ev = self._events_for(r, w)
        self.ops[eng].append({"fn": fn, "ev": ev, "dma": None, "lane": self.lane})
        me = ("e", eng, idx)
        for b in r:
            b.rd = [e for e in b.rd if not (e[0] == "e" and e[1] == eng)] + [me]
        for b in w:
            b.lw = [me]
            b.rd = []
        return idx

    def i(self, eng, meth, *a, r=(), w=(), **kw):
        return self.op(eng, lambda e: getattr(e, meth)(*a, **kw), r=r, w=w)

    def _async(self, eng, fn, sem_buf, inc, r, w):
        idx = len(self.ops[eng])
        ev = self._events_for(r, w)
        b = sem_buf
        if b.dsem is None:
            if self.free_sems and self.ndsem >= 72:
                j = min(range(len(self.free_sems)), key=lambda q: self.free_sems[q][1])
                b.dsem, b.dcount = self.free_sems.pop(j)
            else:
                b.dsem = self.ndsem
                self.ndsem += 1
                b.dcount = 0
            self.dbufs.append(b)
            self.scope_sems[-1].append(b)
        b.dcount += inc
        me = ("d", b.dsem, b.dcount)
        self.ops[eng].append({"fn": fn, "ev": ev, "dma": b.dsem, "inc": inc, "lane": self.lane})
        for x in r:
            x.rd = [e for e in x.rd if not (e[0] == "d" and e[1] == b.dsem)] + [me]
        for x in w:
            x.lw = [me]
            x.rd = []
        return idx

    def dma(self, eng, out, in_, sem_buf, r=(), w=(), **kw):
        return self._async(eng, lambda e: e.dma_start(out=out, in_=in_, **kw), sem_buf, 16, r, w)

    def gather_rows(self, out, in_, idx_ap, sem_buf, r=(), w=()):
        return self._async("pool", lambda e: e.indirect_dma_start(
            out=out, out_offset=None, in_=in_, in_offset=bass.IndirectOffsetOnAxis(ap=idx_ap, axis=0)),
            sem_buf, 16, r, w)

    def coll(self, kind, ins_ap, outs_ap, groups, sem_buf, r=(), w=()):
        return self._async("pool", lambda e: e.collective_compute(
            kind, ALU.bypass, replica_groups=groups, ins=[ins_ap], outs=[outs_ap]), sem_buf, 1, r, w)

    def barrier(self):
        ev = []
        for e in ENGS:
            for i in range(len(self.ops[e]) - 1, -1, -1):
                o = self.ops[e][i]
                if o["fn"] is not None and o["dma"] is None:
                    ev.append(("e", e, i))
                    break
        for b in self.dbufs:
            ev.append(("d", b.dsem, b.dcount))
        for e in ENGS:
            self.ops[e].append({"fn": None, "ev": list(ev), "dma": None, "lane": self.lane})

    def emit(self):
        nc = self.nc
        self.barrier()
        NLANE = 4
        marked = {e: set() for e in ENGS}
        lane_of = {e: [o["lane"] % NLANE for o in self.ops[e]] for e in ENGS}
        for eng in ENGS:
            seen_e = {}
            seen_d = {}
            for i, o in enumerate(self.ops[eng]):
                need_e = {}
                need_d = {}
                for ev in o["ev"]:
                    if ev[0] == "e":
                        _, e2, i2 = ev
                        if e2 == eng and eng == "pe":
                            continue
                        key = (e2, lane_of[e2][i2])
                        if seen_e.get(key, -1) >= i2:
                            continue
                        need_e[key] = max(need_e.get(key, -1), i2)
                    else:
                        _, s, c = ev
                        if seen_d.get(s, 0) >= c:
                            continue
                        need_d[s] = max(need_d.get(s, 0), c)
                for key, i2 in need_e.items():
                    seen_e[key] = i2
                    marked[key[0]].add(i2)
                for s, c in need_d.items():
                    seen_d[s] = c
                o["need_e"] = need_e
                o["need_d"] = need_d
        cum = {}
        for eng in ENGS:
            c = [0] * NLANE
            arr = []
            for i in range(len(self.ops[eng])):
                ln = lane_of[eng][i]
                if i in marked[eng]:
                    c[ln] += 1
                arr.append(c[ln])
            cum[eng] = arr
        with contextlib.ExitStack() as st:
            esem = {(e, ln): st.enter_context(nc.semaphore(f"es_{e}{ln}")) for e in ENGS for ln in range(NLANE)}
            dsem = [st.enter_context(nc.semaphore(f"ds_{i}")) for i in range(self.ndsem)]
            block = st.enter_context(nc.Block())

            def body(eng):
                def run(engine):
                    for i, o in enumerate(self.ops[eng]):
                        for (e2, ln), i2 in o["need_e"].items():
                            engine.wait_ge(esem[(e2, ln)], cum[e2][i2])
                        for s, c in o["need_d"].items():
                            engine.wait_ge(dsem[s], c)
                        if o["fn"] is None:
                            continue
                        ins = o["fn"](engine)
                        if o["dma"] is not None:
                            ins.then_inc(dsem[o["dma"]], o["inc"])
                        elif i in marked[eng]:
                            ins.then_inc(esem[(eng, lane_of[eng][i])], 1)
                return run

            block.tensor(body("pe"))
            block.scalar(body("act"))
            block.vector(body("dve"))
            block.gpsimd(body("pool"))
            block.sync(body("sp"))
        while self.stacks:
            self.stacks.pop().close()
        return {e: len(self.ops[e]) for e in ENGS}


class _View:
    def __init__(self, buf, ap):
        self.buf = buf
        self.ap = ap


def xin_view(xin, n):
    return xin


class RR:
    def __init__(self, items):
        self.items = list(items)
        self.i = 0

    def __call__(self):
        x = self.items[self.i % len(self.items)]
        self.i += 1
        return x


def copy_on(S, eng, out_ap, in_ap, r, w):
    if eng == "act":
        S.op("act", lambda e: e.copy(out=out_ap, in_=in_ap), r=r, w=w)
    else:
        S.op(eng, lambda e: e.tensor_copy(out=out_ap, in_=in_ap), r=r, w=w)


class K:
    def __init__(self, nc, dbg=()):
        self.nc = nc
        self.S = Sched(nc)
        self.dbg = set(dbg)
        self.outs = {}

    def ext_in(self, name, shape, dtype):
        return Buf(name, self.nc.dram_tensor(name, list(shape), dtype, kind="ExternalInput").ap())

    def ext_out(self, name, shape, dtype):
        b = Buf(name, self.nc.dram_tensor(name, list(shape), dtype, kind="ExternalOutput").ap())
        self.outs[name] = b
        return b

    def consts(self):
        S = self.S
        self.ident = S.sb([128, 128], BF16, "ident")
        identf = S.sb([128, 128], F32, "identf")
        self.ones_bf = S.sb([128, 128], BF16, "ones")
        cid = self.ext_in("c_ident", [128, 128], F32)
        S.dma("sp", identf[:], cid.t, identf, w=[identf])
        S.op("dve", lambda e: e.tensor_copy(out=self.ident[:], in_=identf[:]), r=[identf], w=[self.ident])
        S.op("dve", lambda e: e.memset(self.ones_bf[:], 1.0), w=[self.ones_bf])
        self.identf = identf

    def gather8(self, src, rows, cols, dtype, name):
        S = self.S
        mid = S.dram(name + "_q", [4 * rows, cols], dtype)
        dst = S.dram(name + "_g", [8 * rows, cols], dtype)
        sem = Buf(name + "_sem")
        S.coll("AllGather", src.t, mid.t, QUADS, sem, r=[src], w=[mid])
        S.coll("AllGather", mid.t, dst.t, P4, sem, r=[mid], w=[dst])
        return dst

    def pair_gather(self, src, dst, nch, rows, src_ap=None):
        sem = Buf(src.name + "_pgsem")
        ap = src.t if src_ap is None else src_ap
        for i in range(nch):
            self.S.coll("AllGather", ap[i * rows:(i + 1) * rows, :], dst.t[i].rearrange("r p n -> (r p) n"), PAIRS, sem,
                        r=[src], w=[dst])

    def mod_stage(self):
        S = self.S
        cvT = self.ext_in("cvT", [128, 32, 5], F32)
        wmod = self.ext_in("wmod", [self.NL, D, 3072], F32)
        bmodT = self.ext_in("bmodT", [128, 2, 24], F32)
        sel = self.ext_in("sel", [128, 2, 5], F32)
        S.push()
        cv = S.sb([128, 32, 5], F32, "cv")
        cvb = S.sb([128, 32, 5], BF16, "cvb")
        bm = S.sb([128, 2, 24], F32, "bm")
        part = S.sb([128, 2, 24, 5], F32, "modpart")
        S.dma("sp", cv[:], cvT.t, cv, w=[cv])
        S.dma("sp", bm[:], bmodT.t, bm, w=[bm])
        S.op("act", lambda e: e.activation(out=cvb[:], in_=cv[:], func=AF.Silu), r=[cv], w=[cvb])
        wf = [S.sb([128, 32, 128], F32, "wf") for _ in range(2)]
        wb = [S.sb([128, 32, 128], BF16, "wb") for _ in range(2)]
        pp = [S.ps([128, 8], F32, "modps") for _ in range(2)]
        cv_eng = RR(["dve", "pool", "act"])
        it = 0
        S.op("dve", lambda e: e.memset(part[:], 0.0), w=[part])
        for l in range(self.NL):
            for lc in range(24):
                f = wf[it % 2]
                b = wb[it % 2]
                p = pp[it % 2]
                src = wmod.t[l, :, lc * 128:(lc + 1) * 128].rearrange("(c p) n -> p c n", p=128)
                S.dma("sp" if it % 2 == 0 else "act", f[:], src, f, w=[f])
                for q in range(4):
                    copy_on(S, cv_eng(), b[:, q * 8:(q + 1) * 8, :], f[:, q * 8:(q + 1) * 8, :], [f], [b])
                for c in range(32):
                    S.op("pe", lambda e, p=p, b=b, c=c: e.matmul(p[:, 0:5], lhsT=b[:, c, :], rhs=cvb[:, c, :],
                                                               start=(c == 0), stop=(c == 31)), r=[b, cvb], w=[p])
                S.op("dve", lambda e, p=p, l=l, lc=lc: e.tensor_scalar(
                    out=part[:, l, lc, :], in0=p[:, 0:5], scalar1=bm[:, l, lc:lc + 1], scalar2=None, op0=ALU.add),
                    r=[p, bm], w=[part])
                it += 1
        msrc = S.dram("modsrc", [128, 240], F32)
        S.dma("pool", msrc.t, part[:].rearrange("p l c v -> p (l c v)"), part, r=[part], w=[msrc])
        mall = self.gather8(msrc, 128, 240, F32, "modg")
        S.pop()
        tab = S.sb([128, 8, 2, 24, 5], F32, "modtab")
        selt = S.sb([128, 2, 5], F32, "selt")
        S.dma("sp", tab[:].rearrange("p r l c v -> p r (l c v)"), mall.t.rearrange("(r p) n -> p r n", p=128), tab,
              r=[mall], w=[tab])
        S.dma("sp", selt[:], sel.t, selt, w=[selt])
        self.mods = [S.sb([128, 2, 192], F32, f"modsel{w_}") for w_ in range(2)]
        S.push()
        tmp = S.sb([128, 8 * 2 * 24, 5], F32, "modtmp")
        for wsel in range(2):
            m = self.mods[wsel]
            S.op("dve", lambda e, wsel=wsel: e.tensor_tensor(
                out=tmp[:], in0=tab[:].rearrange("p r l c v -> p (r l c) v"),
                in1=selt[:, wsel:wsel + 1, :].to_broadcast([128, 384, 5]), op=ALU.mult), r=[tab, selt], w=[tmp])
            S.op("dve", lambda e, m=m: e.tensor_reduce(
                out=m[:].rearrange("p l (r c) -> p r l c", r=8),
                in_=tmp[:].rearrange("p (r l c) v -> p r l c v", r=8, l=2), axis=AX.X, op=ALU.add), r=[tmp], w=[m])
        S.pop()

    def modcol(self, which, l, k, c):
        return self.mods[which][:, l, k * 32 + c:k * 32 + c + 1]

    def convert_win(self):
        S = self.S
        self.win = self.ext_in("win", [self.NL, D, NCOL], F32)
        self.winb = S.dram("winb", [self.NL, 41, 128, 32, 128], BF16)
        S.push()
        wf = [S.sb([128, NCOL], F32, "cwf") for _ in range(2)]
        wb = [S.sb([128, NCOL], BF16, "cwb") for _ in range(2)]
        eng = RR(["dve", "pool", "act"])
        it = 0
        for l in range(self.NL):
            for c in range(32):
                f, b = wf[it % 2], wb[it % 2]
                S.dma("sp", f[:], self.win.t[l, c * 128:(c + 1) * 128, :], f, w=[f])
                for q in range(6):
                    copy_on(S, eng(), b[:, q * 848:(q + 1) * 848], f[:, q * 848:(q + 1) * 848], [f], [b])
                S.dma("pool", self.winb.t[l, 0:39, :, c, :].rearrange("j p n -> p j n"),
                      b[:, 0:4992].rearrange("p (j n) -> p j n", n=128), b, r=[b], w=[self.winb])
                S.dma("pool", self.winb.t[l, 39, :, c, 0:32], b[:, 4992:5024], b, r=[b], w=[self.winb])
                S.dma("pool", self.winb.t[l, 40, :, c, 0:64], b[:, 5024:5088], b, r=[b], w=[self.winb])
                it += 1
        S.pop()

    def norm_stage(self, l, h_src, gname, ktypes, out_nT):
        S = self.S
        k_sh, k_sc = ktypes
        S.push()
        g = S.sb([128, 2, 32], F32, "ng")
        S.dma("sp", g[:], self.ext[gname].t, g, w=[g])
        gm = [S.sb([128, 32], F32, "gm") for _ in range(2)]
        for w_ in range(2):
            S.op("dve", lambda e, w_=w_: e.scalar_tensor_tensor(
                out=gm[w_][:], in0=self.mods[w_][:, l, k_sc * 32:(k_sc + 1) * 32], scalar=1.0, in1=g[:, l, :],
                op0=ALU.add, op1=ALU.mult), r=[self.mods[w_], g], w=[gm[w_]])
        hb = [S.sb([128, D], F32, "hb") for _ in range(2)]
        junk = S.sb([128, D], BF16, "junk")
        hs = [S.sb([128, D], BF16, "hs") for _ in range(2)]
        st = [S.sb([128, 4], F32, "nst") for _ in range(2)]
        tp = [S.ps([128, 1024], BF16, "ntp") for _ in range(4)]
        nblk = [S.sb([128, 32, 512], BF16, "nblk") for _ in range(2)]
        ev = RR(["dve", "act"])
        tiles = [(0, 1, 0)] + [(128 + 512 * q, 4, 0) for q in range(4)]
        tiles[0] = (0, 1, 1)
        tcount = 0
        for bi, (t0, nt, which) in enumerate(tiles):
            nb = nblk[bi % 2]
            for ti in range(nt):
                h = hb[tcount % 2]
                hsb = hs[tcount % 2]
                s_ = st[tcount % 2]
                r0 = t0 + ti * 128
                S.dma("sp", h[:], h_src.t[r0:r0 + 128, :], h, r=[h_src], w=[h])
                S.op("act", lambda e, h=h, s_=s_: e.activation(out=junk[:], in_=h[:], func=AF.Square, accum_out=s_[:, 0:1]),
                     r=[h], w=[junk, s_])
                S.op("dve", lambda e, s_=s_: e.tensor_scalar(out=s_[:, 1:2], in0=s_[:, 0:1], scalar1=1.0 / D, scalar2=1e-6,
                                                            op0=ALU.mult, op1=ALU.add), r=[s_], w=[s_])
                S.op("act", lambda e, s_=s_: e.activation(out=s_[:, 2:3], in_=s_[:, 1:2], func=AF.Sqrt), r=[s_], w=[s_])
                S.op("dve", lambda e, s_=s_: e.reciprocal(out=s_[:, 3:4], in_=s_[:, 2:3]), r=[s_], w=[s_])
                S.op("act", lambda e, h=h, hsb=hsb, s_=s_: e.activation(out=hsb[:], in_=h[:], func=AF.Identity, scale=s_[:, 3:4]),
                     r=[h, s_], w=[hsb])
                for q in range(4):
                    p = tp[q]
                    for j in range(8):
                        c = q * 8 + j
                        S.op("pe", lambda e, p=p, hsb=hsb, c=c, j=j: e.transpose(
                            out=p[:, j * 128:(j + 1) * 128], in_=hsb[:, c * 128:(c + 1) * 128], identity=self.ident[:]),
                            r=[hsb, self.ident], w=[p])
                    for j in range(8):
                        c = q * 8 + j
                        en = ev()
                        o_ap = nb[:, c, ti * 128:(ti + 1) * 128]
                        i_ap = p[:, j * 128:(j + 1) * 128]
                        sc_ap = gm[which][:, c:c + 1]
                        sh_ap = self.modcol(which, l, k_sh, c)
                        if en == "dve":
                            S.op("dve", lambda e, o_ap=o_ap, i_ap=i_ap, sc_ap=sc_ap, sh_ap=sh_ap: e.tensor_scalar(
                                out=o_ap, in0=i_ap, scalar1=sc_ap, scalar2=sh_ap, op0=ALU.mult, op1=ALU.add),
                                r=[p, gm[which], self.mods[which]], w=[nb])
                        else:
                            S.op("act", lambda e, o_ap=o_ap, i_ap=i_ap, sc_ap=sc_ap, sh_ap=sh_ap: e.activation(
                                out=o_ap, in_=i_ap, func=AF.Identity, scale=sc_ap, bias=sh_ap),
                                r=[p, gm[which], self.mods[which]], w=[nb])
                tcount += 1
            n = nt * 128
            S.dma("pool", out_nT.t[:, t0:t0 + n].rearrange("(c p) n -> p c n", p=128), nb[:, :, 0:n], nb,
                  r=[nb], w=[out_nT])
        S.pop()

    def inproj_stage(self, l, nT_all, pT):
        S = self.S
        S.push()
        nt = S.sb([128, 32, 1024], BF16, "pnT")
        wr = [S.sb([128, 32, 128], BF16, "pw") for _ in range(3)]
        pp = [S.ps([128, 512], F32, "pps") for _ in range(4)]
        ob = [S.sb([128, 512], F32, "pob") for _ in range(4)]
        ev = RR(["act", "dve"])
        sbs = [(0, [(0, 0, 128), (1, 0, 128)])]
        for q in range(4):
            sbs.append((256 + q * 1024, [(q // 2, 128 + (q % 2) * 1024, 1024)]))
        wi = 0
        oi = 0
        for s0, parts in sbs:
            off = 0
            for (rk, o, n) in parts:
                for ci in range(2):
                    S.dma("sp", nt[:, :, off:off + n].rearrange("p (ch ci) n -> p ch ci n", ci=2)[:, :, ci, :],
                          nT_all.t[:, rk, ci * 128:(ci + 1) * 128, o:o + n].rearrange("ch p n -> p ch n"), nt,
                          r=[nT_all], w=[nt])
                off += n
            ntok = off
            for j, (c0, m) in enumerate(CHUNKS):
                w = wr[wi % 3]
                wi += 1
                S.dma("act" if wi % 2 else "sp", w[:, :, 0:m], self.winb.t[l, j, :, :, 0:m], w, r=[self.winb], w=[w])
                for hf in range((ntok + 511) // 512):
                    nn = min(512, ntok - hf * 512)
                    p = pp[oi % 4]
                    o_ = ob[oi % 4]
                    oi += 1
                    for c in range(32):
                        S.op("pe", lambda e, p=p, w=w, c=c, m=m, hf=hf, nn=nn: e.matmul(
                            p[0:m, 0:nn], lhsT=w[:, c, 0:m], rhs=nt[:, c, hf * 512:hf * 512 + nn],
                            start=(c == 0), stop=(c == 31)), r=[w, nt], w=[p])
                    copy_on(S, ev(), o_[0:m, 0:nn], p[0:m, 0:nn], [p], [o_])
                    S.dma("pool", pT.t[c0:c0 + m, s0 + hf * 512:s0 + hf * 512 + nn], o_[0:m, 0:nn], o_, r=[o_], w=[pT])
        S.pop()


    def mix_dst(self, mixT, row0, nrows, s0, n):
        out = []
        segs = [(0, 128, 0, 0), (128, 256, 1, 0)]
        for half in range(2):
            for bq in range(4):
                a0 = 256 + half * 2048 + bq * 512
                segs.append((a0, a0 + 512, half, 1 + bq))
        for (a, b, half, blk) in segs:
            lo, hi = max(a, s0), min(b, s0 + n)
            if lo < hi:
                out.append((mixT.t[half, blk, row0:row0 + nrows, lo - a:hi - a], lo - s0, hi - lo))
        return out

    def bcast_row(self, ps_ap, row_hi, row_lo, n, r):
        S = self.S
        S.op("pe", lambda e: e.matmul(ps_ap, lhsT=self.ones_bf[0:1, :], rhs=row_hi, start=True, stop=False), r=r + [self.ones_bf], w=[])
        return None

    def conv_stage(self, l, pT, mixT):
        S = self.S
        NP = 4397
        NA = 4367
        S.push()
        cw = S.sb([128, 2, 4, 31], F32, "cw")
        cb = S.sb([128, 2, 4, 3], F32, "cb")
        S.dma("sp", cw[:], self.ext["cw"].t, cw, w=[cw])
        S.dma("sp", cb[:], self.ext["cb"].t, cb, w=[cb])
        acc = S.sb([128, 4, NA], F32, "cacc")
        xb = S.sb([128, 2, 512], BF16, "cxb")
        ca0, cg0 = COLS["ca"][0], COLS["cg"][0]
        S.push()
        hp = S.sb([128, NP], F32, "chp")
        gt = S.sb([128, T], F32, "cgt")
        S.op("pool", lambda e: e.memset(hp[:], 0.0), w=[hp])
        for cc in range(4):
            S.dma("sp", gt[:], pT.t[cg0 + cc * 128:cg0 + (cc + 1) * 128, :], gt, r=[pT], w=[gt])
            S.op("act", lambda e: e.activation(out=gt[:], in_=gt[:], func=AF.Sigmoid), r=[gt], w=[gt])
            S.dma("act", hp[:, 15:271], pT.t[ca0 + cc * 128:ca0 + (cc + 1) * 128, 0:256], hp, r=[pT], w=[hp])
            S.dma("act", hp[:, 286:4382], pT.t[ca0 + cc * 128:ca0 + (cc + 1) * 128, 256:T], hp, r=[pT], w=[hp])
            S.op("dve", lambda e: e.tensor_tensor(out=hp[:, 15:271], in0=hp[:, 15:271], in1=gt[:, 0:256], op=ALU.mult), r=[hp, gt], w=[hp])
            S.op("dve", lambda e: e.tensor_tensor(out=hp[:, 286:4382], in0=hp[:, 286:4382], in1=gt[:, 256:T], op=ALU.mult), r=[hp, gt], w=[hp])
            S.op("dve", lambda e, cc=cc: e.tensor_scalar(out=acc[:, cc, :], in0=hp[:, 0:NA], scalar1=cw[:, l, cc, 0:1],
                                                          scalar2=cb[:, l, cc, 0:1], op0=ALU.mult, op1=ALU.add), r=[hp, cw, cb], w=[acc])
            for j in range(1, 31):
                S.op("dve", lambda e, cc=cc, j=j: e.scalar_tensor_tensor(
                    out=acc[:, cc, :], in0=hp[:, j:j + NA], scalar=cw[:, l, cc, j:j + 1], in1=acc[:, cc, :],
                    op0=ALU.mult, op1=ALU.add), r=[hp, cw, acc], w=[acc])
        S.pop()
        strow = [S.sb([1, 2, 512], F32, "cstrow") for _ in range(2)]
        pst = [S.ps([128, 512], F32, "cps") for _ in range(4)]
        nblk = (NA + 511) // 512
        cst = S.dram(f"cst{l}", [2, NA], F32)
        cstg = S.dram(f"cstg{l}", [4, NA], F32)
        for bi in range(nblk):
            c0 = bi * 512
            n = min(512, NA - c0)
            p1, p2 = pst[(bi % 2) * 2], pst[(bi % 2) * 2 + 1]
            sr = strow[bi % 2]
            for cc in range(4):
                S.op("act", lambda e, cc=cc, c0=c0, n=n: e.copy(out=xb[:, 0, 0:n], in_=acc[:, cc, c0:c0 + n]), r=[acc], w=[xb])
                S.op("act", lambda e, cc=cc, c0=c0, n=n: e.activation(out=xb[:, 1, 0:n], in_=acc[:, cc, c0:c0 + n], func=AF.Square), r=[acc], w=[xb])
                S.op("pe", lambda e, p1=p1, n=n, cc=cc: e.matmul(p1[0:1, 0:n], lhsT=self.ones_bf[:, 0:1], rhs=xb[:, 0, 0:n],
                                                                 start=(cc == 0), stop=(cc == 3)), r=[xb, self.ones_bf], w=[p1])
                S.op("pe", lambda e, p2=p2, n=n, cc=cc: e.matmul(p2[0:1, 0:n], lhsT=self.ones_bf[:, 0:1], rhs=xb[:, 1, 0:n],
                                                                 start=(cc == 0), stop=(cc == 3)), r=[xb, self.ones_bf], w=[p2])
            S.op("dve", lambda e, p1=p1, sr=sr, n=n: e.tensor_copy(out=sr[0:1, 0, 0:n], in_=p1[0:1, 0:n]), r=[p1], w=[sr])
            S.op("dve", lambda e, p2=p2, sr=sr, n=n: e.tensor_copy(out=sr[0:1, 1, 0:n], in_=p2[0:1, 0:n]), r=[p2], w=[sr])
            S.dma("sp", cst.t[:, c0:c0 + n].rearrange("(o a) n -> o a n", o=1), sr[0:1, :, 0:n], sr, r=[sr], w=[cst])
        S.coll("AllGather", cst.t, cstg.t, PAIRS, Buf("cstsem"), r=[cst], w=[cstg])
        both = [S.sb([1, 4, 512], F32, "cboth") for _ in range(2)]
        mrs = [S.sb([1, 4, 512], F32, "cmr") for _ in range(2)]
        hls = [S.sb([1, 2, 2, 512], BF16, "chl") for _ in range(2)]
        ot = [S.sb([128, 512], F32, "cot") for _ in range(2)]
        ob = [S.sb([128, 4, 512], BF16, "cob") for _ in range(2)]
        for bi in range(nblk):
            c0 = bi * 512
            n = min(512, NA - c0)
            p1, p2 = pst[(bi % 2) * 2], pst[(bi % 2) * 2 + 1]
            bo, mr, hl = both[bi % 2], mrs[bi % 2], hls[bi % 2]
            S.dma("sp", bo[0:1, :, 0:n], cstg.t[:, c0:c0 + n].rearrange("(o a) n -> o a n", o=1), bo, r=[cstg], w=[bo])
            S.op("dve", lambda e, bo=bo, mr=mr, n=n: e.tensor_tensor(out=mr[0:1, 0:2, 0:n], in0=bo[0:1, 0:2, 0:n], in1=bo[0:1, 2:4, 0:n], op=ALU.add), r=[bo], w=[mr])
            S.op("dve", lambda e, mr=mr, n=n: e.tensor_scalar(out=mr[0:1, 0:2, 0:n], in0=mr[0:1, 0:2, 0:n], scalar1=1.0 / 1024, scalar2=None, op0=ALU.mult), r=[mr], w=[mr])
            S.op("dve", lambda e, mr=mr, n=n: e.tensor_tensor(out=mr[0:1, 3, 0:n], in0=mr[0:1, 0, 0:n], in1=mr[0:1, 0, 0:n], op=ALU.mult), r=[mr], w=[mr])
            S.op("dve", lambda e, mr=mr, n=n: e.tensor_tensor(out=mr[0:1, 1, 0:n], in0=mr[0:1, 1, 0:n], in1=mr[0:1, 3, 0:n], op=ALU.subtract), r=[mr], w=[mr])
            S.op("dve", lambda e, mr=mr, n=n: e.tensor_scalar(out=mr[0:1, 1, 0:n], in0=mr[0:1, 1, 0:n], scalar1=1e-5, scalar2=None, op0=ALU.add), r=[mr], w=[mr])
            S.op("act", lambda e, mr=mr, n=n: e.activation(out=mr[0:1, 1, 0:n], in_=mr[0:1, 1, 0:n], func=AF.Sqrt), r=[mr], w=[mr])
            S.op("dve", lambda e, mr=mr, n=n: e.reciprocal(out=mr[0:1, 1, 0:n], in_=mr[0:1, 1, 0:n]), r=[mr], w=[mr])
            S.op("dve", lambda e, mr=mr, n=n: e.scalar_tensor_tensor(out=mr[0:1, 2, 0:n], in0=mr[0:1, 0, 0:n], scalar=-1.0, in1=mr[0:1, 1, 0:n],
                                                                     op0=ALU.mult, op1=ALU.mult), r=[mr], w=[mr])
            S.op("dve", lambda e, mr=mr, hl=hl, n=n: e.tensor_copy(out=hl[0:1, 0, :, 0:n], in_=mr[0:1, 1:3, 0:n]), r=[mr], w=[hl])
            S.op("dve", lambda e, mr=mr, hl=hl, n=n: e.tensor_tensor(out=mr[0:1, 1:3, 0:n], in0=mr[0:1, 1:3, 0:n], in1=hl[0:1, 0, :, 0:n], op=ALU.subtract), r=[mr, hl], w=[mr])
            S.op("dve", lambda e, mr=mr, hl=hl, n=n: e.tensor_copy(out=hl[0:1, 1, :, 0:n], in_=mr[0:1, 1:3, 0:n]), r=[mr], w=[hl])
            for which, p in ((0, p1), (1, p2)):
                for hq in range(2):
                    S.op("pe", lambda e, p=p, which=which, hq=hq, hl=hl, n=n: e.matmul(
                        p[:, 0:n], lhsT=self.ones_bf[0:1, :], rhs=hl[0:1, hq, which, 0:n], start=(hq == 0), stop=(hq == 1)),
                        r=[hl, self.ones_bf], w=[p])
            o_b = ob[bi % 2]
            for cc in range(4):
                o_ = ot[cc % 2]
                S.op("dve", lambda e, o_=o_, cc=cc, c0=c0, n=n, p1=p1: e.tensor_tensor(out=o_[:, 0:n], in0=acc[:, cc, c0:c0 + n], in1=p1[:, 0:n], op=ALU.mult), r=[acc, p1], w=[o_])
                S.op("dve", lambda e, o_=o_, n=n, p2=p2: e.tensor_tensor(out=o_[:, 0:n], in0=o_[:, 0:n], in1=p2[:, 0:n], op=ALU.add), r=[o_, p2], w=[o_])
                S.op("act", lambda e, o_=o_, o_b=o_b, cc=cc, n=n: e.activation(out=o_b[:, cc, 0:n], in_=o_[:, 0:n], func=AF.Silu,
                                                                              scale=cb[:, l, cc, 1:2], bias=cb[:, l, cc, 2:3]), r=[o_, cb], w=[o_b])
            for (a_lo, a_hi, s_lo) in ((0, 256, 0), (271, NA, 256)):
                lo, hi = max(a_lo, c0), min(a_hi, c0 + n)
                if lo >= hi:
                    continue
                for (ap, off, ln) in self.mix_dst(mixT, 0, 512, s_lo + lo - a_lo, hi - lo):
                    S.dma("pool", ap.rearrange("(cc p) n -> p cc n", p=128), o_b[:, :, lo - c0 + off:lo - c0 + off + ln], o_b, r=[o_b], w=[mixT])
        S.pop()


    def rwkv_stage(self, l, pT, mixT):
        S = self.S
        I = S.i
        NB = 256
        ones = self.ones_bf
        yd = [S.dram(f"rw_y{d}_{l}", [512, T], F32) for d in range(2)]
        bon = [S.dram(f"rw_b{d}_{l}", [512, T], F32) for d in range(2)]
        c_r, c_k, c_v, c_w, c_a, c_g = (COLS[n][0] for n in ("rr", "rk", "rv", "rw", "ra", "rg"))
        S.push()
        rwt = S.sb([64, 8, 19], F32, "rwt")
        S.dma("sp", rwt[:], self.ext["rwt"].t[:, l], rwt, w=[rwt])
        rwl = S.sb([64, 2, 2, 3], F32, "rwl")
        S.dma("sp", rwl[:], self.ext["rwl"].t[:, l], rwl, w=[rwl])
        w2f = S.sb([64, 2, 2, 512], F32, "w2f")
        w2b = S.sb([64, 2, 2, 512], BF16, "w2b")
        S.dma("sp", w2f[:], self.ext["rw2"].t[:, l], w2f, w=[w2f])
        I("dve", "tensor_copy", out=w2b[:], in_=w2f[:], r=[w2f], w=[w2b])
        msk = S.sb([64, 2, 192], F32, "rmask")
        S.dma("sp", msk[:], self.ext["rmask"].t, msk, w=[msk])

        def tb(col):
            return rwt[:, :, col:col + 1]

        for d in range(2):
            S.push()
            ST = S.sb([64, 8, 64], F32, "ST")
            STb = S.sb([64, 8, 64], BF16, "STb")
            I("dve", "memset", ST[:], 0.0, w=[ST])
            I("dve", "memset", STb[:], 0.0, w=[STb])
            xr = S.sb([64, 8, NB + 2], F32, "xr")
            xk = S.sb([64, 8, NB + 2], F32, "xk")
            xv = S.sb([64, 8, NB + 2], F32, "xv")
            xl = S.sb([64, 2, NB + 2], F32, "xl")
            rs_ = S.sb([64, 8, NB], F32, "rs_")
            ks_ = S.sb([64, 8, NB], F32, "ks_")
            vs_ = S.sb([64, 8, NB], F32, "vs_")
            ls_ = S.sb([64, 2, NB], F32, "ls_")
            lb_ = S.sb([64, 2, NB], BF16, "lb_")
            t1 = S.sb([64, 8, NB], F32, "t1")
            t2 = S.sb([64, 8, NB], F32, "t2")
            tbf = S.sb([64, 8, NB], BF16, "tbf")
            lw = S.sb([64, 8, NB], F32, "lw")
            av = S.sb([64, 8, NB], F32, "av")
            kk = S.sb([64, 8, NB], F32, "kk")
            Lp = S.sb([64, 64 + 8 * NB], F32, "Lp")
            onesf = S.sb([64, 8 * NB], BF16, "onesf")
            tot = S.sb([64, 32], F32, "tot")
            wcc = S.sb([64, 32], F32, "wcc")
            AR = S.sb([64, 8, 4, 2, 64], BF16, "AR")
            KT = S.sb([64, 8, 4, 64], BF16, "KT")
            BT = S.sb([64, 8, 4, 64], BF16, "BT")
            vb = S.sb([64, 8, NB], BF16, "vb")
            vtok = S.sb([64, 8, 64], BF16, "vtok")
            kbtok = S.sb([64, 2, 8, 64], BF16, "kbtok")
            Mkb = S.sb([64, 4, 128], BF16, "Mkb")
            Mbb = S.sb([64, 4, 128], BF16, "Mbb")
            Pk = [S.sb([64, 2, 4, 64], BF16, "Pk") for _ in range(2)]
            U = S.sb([64, 4, 64], F32, "U")
            Ub = S.sb([64, 4, 64], BF16, "Ub")
            yblk = S.sb([64, 8, NB], F32, "yblk")
            tS = S.sb([64, 4, 64], F32, "tS")
            psA = S.ps([64, 512], F32, "rpsA")
            psB = S.ps([64, 512], F32, "rpsB")
            psP = S.ps([64, 512], F32, "rpsP")
            psQ = S.ps([64, 512], F32, "rpsQ")
            psX = S.ps([64, 512], F32, "rpsX")
            psT = S.ps([64, 1024], BF16, "rpsT")
            psT2 = S.ps([64, 1024], BF16, "rpsT2")
            I("dve", "memset", Lp[:, 0:64], 0.0, w=[Lp])
            I("dve", "memset", onesf[:], 1.0, w=[onesf])
            blks = [(0, 0, 256)] + [(256 + NB * q, 256, T) for q in range(16)]
            if d == 1:
                blks = [blks[0]] + blks[:0:-1]
            blks = blks[:RW_NBLK]
            for (s0, lo, hi) in blks:
                a0, a1 = max(lo, s0 - 1), min(hi, s0 + NB + 1)
                c0_, c1_ = a0 - (s0 - 1), a1 - (s0 - 1)
                for (xt, col) in ((xr, c_r), (xk, c_k), (xv, c_v)):
                    I("pool", "memset", xt[:], 0.0, w=[xt])
                    S.dma("sp", xt[:, :, c0_:c1_], pT.t[col:col + 512, a0:a1].rearrange("(h i) t -> i h t", i=64), xt, r=[pT], w=[xt])
                I("pool", "memset", xl[:], 0.0, w=[xl])
                S.dma("act", xl[:, 0, c0_:c1_], pT.t[c_w + d * 64:c_w + d * 64 + 64, a0:a1], xl, r=[pT], w=[xl])
                S.dma("act", xl[:, 1, c0_:c1_], pT.t[c_a + d * 64:c_a + d * 64 + 64, a0:a1], xl, r=[pT], w=[xl])
                for (xt, out_, tcol) in ((xr, rs_, 0), (xk, ks_, 3), (xv, vs_, 6)):
                    I("dve", "tensor_tensor", out=out_[:], in0=xt[:, :, 1:NB + 1], in1=tb(tcol).to_broadcast([64, 8, NB]), op=ALU.mult, r=[xt, rwt], w=[out_])
                    I("dve", "tensor_tensor", out=t1[:], in0=xt[:, :, 0:NB], in1=tb(tcol + 1).to_broadcast([64, 8, NB]), op=ALU.mult, r=[xt, rwt], w=[t1])
                    I("dve", "tensor_tensor", out=out_[:], in0=out_[:], in1=t1[:], op=ALU.add, r=[out_, t1], w=[out_])
                    I("dve", "tensor_tensor", out=t1[:], in0=xt[:, :, 2:NB + 2], in1=tb(tcol + 2).to_broadcast([64, 8, NB]), op=ALU.mult, r=[xt, rwt], w=[t1])
                    I("dve", "tensor_tensor", out=out_[:], in0=out_[:], in1=t1[:], op=ALU.add, r=[out_, t1], w=[out_])
                for q in range(2):
                    I("dve", "tensor_scalar", out=ls_[:, q, :], in0=xl[:, q, 1:NB + 1], scalar1=rwl[:, d, q, 0:1], scalar2=None, op0=ALU.mult, r=[xl, rwl], w=[ls_])
                    I("dve", "scalar_tensor_tensor", out=ls_[:, q, :], in0=xl[:, q, 0:NB], scalar=rwl[:, d, q, 1:2], in1=ls_[:, q, :], op0=ALU.mult, op1=ALU.add, r=[xl, rwl, ls_], w=[ls_])
                    I("dve", "scalar_tensor_tensor", out=ls_[:, q, :], in0=xl[:, q, 2:NB + 2], scalar=rwl[:, d, q, 2:3], in1=ls_[:, q, :], op0=ALU.mult, op1=ALU.add, r=[xl, rwl, ls_], w=[ls_])
                I("act", "activation", out=lb_[:, 0, :], in_=ls_[:, 0, :], func=AF.Tanh, r=[ls_], w=[lb_])
                I("act", "copy", out=lb_[:, 1, :], in_=ls_[:, 1, :], r=[ls_], w=[lb_])
                for (q, dst, bcol) in ((0, lw, 15 + d), (1, av, 17 + d)):
                    for hp in range(4):
                        ps = psA if hp % 2 == 0 else psB
                        for hh in range(2):
                            h = hp * 2 + hh
                            I("pe", "matmul", ps[:, hh * NB:(hh + 1) * NB], lhsT=w2b[:, d, q, h * 64:(h + 1) * 64], rhs=lb_[:, q, :], start=True, stop=True, r=[w2b, lb_], w=[ps])
                        I("dve", "tensor_tensor", out=dst[:, hp * 2:hp * 2 + 2, :], in0=ps[:, 0:2 * NB].rearrange("p (h n) -> p h n", h=2),
                          in1=rwt[:, hp * 2:hp * 2 + 2, bcol:bcol + 1].to_broadcast([64, 2, NB]), op=ALU.add, r=[ps, rwt], w=[dst])
                    I("act", "activation", out=dst[:], in_=dst[:], func=AF.Sigmoid, r=[dst], w=[dst])
                I("dve", "tensor_scalar", out=lw[:], in0=lw[:], scalar1=-float(np.exp(-0.5)), scalar2=None, op0=ALU.mult, r=[lw], w=[lw])
                I("dve", "tensor_tensor", out=kk[:], in0=ks_[:], in1=tb(9).to_broadcast([64, 8, NB]), op=ALU.mult, r=[ks_, rwt], w=[kk])
                I("act", "activation", out=tbf[:], in_=kk[:], func=AF.Square, r=[kk], w=[tbf])
                for hp in range(4):
                    ps = psA if hp % 2 == 0 else psB
                    I("pe", "matmul", ps[:, 0:2 * NB], lhsT=ones[0:64, 0:64], rhs=tbf[:, hp * 2:hp * 2 + 2, :], start=True, stop=True, r=[ones, tbf], w=[ps])
                    I("act", "activation", out=t1[:, hp * 2:hp * 2 + 2, :], in_=ps[:, 0:2 * NB].rearrange("p (h n) -> p h n", h=2), func=AF.Sqrt, r=[ps], w=[t1])
                I("dve", "tensor_scalar", out=t1[:], in0=t1[:], scalar1=1e-12, scalar2=None, op0=ALU.max, r=[t1], w=[t1])
                I("dve", "reciprocal", out=t1[:], in_=t1[:], r=[t1], w=[t1])
                I("dve", "tensor_tensor", out=kk[:], in0=kk[:], in1=t1[:], op=ALU.mult, r=[kk, t1], w=[kk])
                I("dve", "tensor_tensor", out=t1[:], in0=av[:], in1=tb(10).to_broadcast([64, 8, NB]), op=ALU.mult, r=[av, rwt], w=[t1])
                I("dve", "tensor_tensor", out=t1[:], in0=t1[:], in1=tb(11).to_broadcast([64, 8, NB]), op=ALU.add, r=[t1, rwt], w=[t1])
                I("dve", "tensor_tensor", out=ks_[:], in0=ks_[:], in1=t1[:], op=ALU.mult, r=[ks_, t1], w=[ks_])
                I("dve", "tensor_tensor", out=av[:], in0=av[:], in1=kk[:], op=ALU.mult, r=[av, kk], w=[av])
                I("dve", "tensor_tensor", out=t1[:], in0=rs_[:], in1=ks_[:], op=ALU.mult, r=[rs_, ks_], w=[t1])
                I("dve", "tensor_tensor", out=tbf[:], in0=t1[:], in1=tb(12).to_broadcast([64, 8, NB]), op=ALU.mult, r=[t1, rwt], w=[tbf])
                for hp in range(4):
                    ps = psA if hp % 2 == 0 else psB
                    I("pe", "matmul", ps[:, 0:2 * NB], lhsT=ones[0:64, 0:64], rhs=tbf[:, hp * 2:hp * 2 + 2, :], start=True, stop=True, r=[ones, tbf], w=[ps])
                    I("dve", "tensor_tensor", out=t2[:, hp * 2:hp * 2 + 2, :], in0=ps[:, 0:2 * NB].rearrange("p (h n) -> p h n", h=2), in1=vs_[:, hp * 2:hp * 2 + 2, :], op=ALU.mult, r=[ps, vs_], w=[t2])
                S.dma("act", bon[d].t[:, s0:s0 + NB].rearrange("(h i) t -> i h t", i=64), t2[:], t2, r=[t2], w=[bon[d]])
                I("dve", "tensor_tensor_scan", out=Lp[:, 64:], data0=onesf[:], data1=lw[:].rearrange("p h n -> p (h n)"), initial=0.0, op0=ALU.mult, op1=ALU.add, r=[onesf, lw], w=[Lp])
                Lc = Lp[:, 64:].rearrange("p (m n) -> p m n", n=64)
                Lprev = Lp[:, 0:8 * NB].rearrange("p (m n) -> p m n", n=64)[:, :, 63:64]
                t1v = t1[:].rearrange("p h (c n) -> p (h c) n", n=64)
                t2v = t2[:].rearrange("p h (c n) -> p (h c) n", n=64)
                lwv = lw[:].rearrange("p h (c n) -> p (h c) n", n=64)
                I("dve", "tensor_tensor", out=t1v, in0=Lc, in1=Lprev.to_broadcast([64, 32, 64]), op=ALU.subtract, r=[Lp], w=[t1])
                I("dve", "tensor_copy", out=tot[:].unsqueeze(2), in_=t1v[:, :, 63:64], r=[t1], w=[tot])
                I("act", "activation", out=wcc[:], in_=tot[:], func=AF.Exp, r=[tot], w=[wcc])
                I("dve", "tensor_tensor", out=t2v, in0=t1v, in1=lwv, op=ALU.subtract, r=[t1, lw], w=[t2])
                if d == 1:
                    I("dve", "tensor_tensor", out=lwv, in0=tot[:].unsqueeze(2).to_broadcast([64, 32, 64]), in1=t2v, op=ALU.subtract, r=[tot, t2], w=[lw])
                    I("dve", "tensor_tensor", out=t2v, in0=tot[:].unsqueeze(2).to_broadcast([64, 32, 64]), in1=t1v, op=ALU.subtract, r=[tot, t1], w=[t2])
                    I("dve", "tensor_copy", out=t1[:], in_=lw[:], r=[lw], w=[t1])
                ARv = AR[:].rearrange("p h c a n -> p (h c) a n")
                I("act", "activation", out=lw[:], in_=t2[:], func=AF.Exp, r=[t2], w=[lw])
                I("dve", "scalar_tensor_tensor", out=ARv[:, :, 0, :], in0=kk[:].rearrange("p h (c n) -> p (h c) n", n=64), scalar=-1.0, in1=lwv, op0=ALU.mult, op1=ALU.mult, r=[kk, lw], w=[AR])
                I("act", "activation", out=lw[:], in_=t1[:], func=AF.Exp, r=[t1], w=[lw])
                I("dve", "tensor_tensor", out=ARv[:, :, 1, :], in0=rs_[:].rearrange("p h (c n) -> p (h c) n", n=64), in1=lwv, op=ALU.mult, r=[rs_, lw], w=[AR])
                I("act", "activation", out=lw[:], in_=t1[:], func=AF.Exp, scale=-1.0, r=[t1], w=[lw])
                I("dve", "tensor_tensor", out=KT[:].rearrange("p h c n -> p h (c n)"), in0=ks_[:], in1=lw[:], op=ALU.mult, r=[ks_, lw], w=[KT])
                I("dve", "tensor_tensor", out=BT[:].rearrange("p h c n -> p h (c n)"), in0=av[:], in1=lw[:], op=ALU.mult, r=[av, lw], w=[BT])
                I("act", "copy", out=vb[:], in_=vs_[:], r=[vs_], w=[vb])
                chunks = range(4) if d == 0 else range(3, -1, -1)
                if RW_LEVEL < 2:
                    chunks = []
                for q in chunks:
                    for h in range(8):
                        I("pe", "transpose", out=psT[:, h * 64:(h + 1) * 64], in_=vb[:, h, q * 64:(q + 1) * 64], identity=self.ident[0:64, 0:64], r=[vb, self.ident], w=[psT])
                    I("act", "copy", out=vtok[:].rearrange("p h n -> p (h n)"), in_=psT[:, 0:512], r=[psT], w=[vtok])
                    for h in range(8):
                        I("pe", "transpose", out=psT2[:, h * 64:(h + 1) * 64], in_=KT[:, h, q, :], identity=self.ident[0:64, 0:64], r=[KT, self.ident], w=[psT2])
                        I("pe", "transpose", out=psT2[:, 512 + h * 64:512 + (h + 1) * 64], in_=BT[:, h, q, :], identity=self.ident[0:64, 0:64], r=[BT, self.ident], w=[psT2])
                    I("act", "copy", out=kbtok[:].rearrange("p a h n -> p (a h n)"), in_=psT2[:, 0:1024], r=[psT2], w=[kbtok])
                    for hg in (range(2) if RW_LEVEL >= 3 else []):
                        hs = [hg * 4 + x for x in range(4)]
                        for x, h in enumerate(hs):
                            I("pe", "matmul", psA[:, x * 128:(x + 1) * 128], lhsT=KT[:, h, q, :], rhs=AR[:, h, q, :, :], start=True, stop=True, r=[KT, AR], w=[psA])
                        for x, h in enumerate(hs):
                            I("pe", "matmul", psB[:, x * 128:(x + 1) * 128], lhsT=BT[:, h, q, :], rhs=AR[:, h, q, :, :], start=True, stop=True, r=[BT, AR], w=[psB])
                        for x, h in enumerate(hs):
                            I("pe", "matmul", psP[:, x * 64:(x + 1) * 64], lhsT=AR[:, h, q, 0, :], rhs=BT[:, h, q, :], start=True, stop=True, r=[AR, BT], w=[psP])
                        I("dve", "tensor_tensor", out=Mkb[:], in0=psA[:, 0:512].rearrange("p (x n) -> p x n", x=4), in1=msk[:, d:d + 1, 0:128].to_broadcast([64, 4, 128]), op=ALU.mult, r=[psA, msk], w=[Mkb])
                        I("dve", "tensor_tensor", out=Mbb[:], in0=psB[:, 0:512].rearrange("p (x n) -> p x n", x=4), in1=msk[:, d:d + 1, 0:128].to_broadcast([64, 4, 128]), op=ALU.mult, r=[psB, msk], w=[Mbb])
                        pk = Pk[0]
                        I("dve", "tensor_tensor", out=pk[:, 0, :, :], in0=psP[:, 0:256].rearrange("p (x n) -> p x n", x=4), in1=msk[:, d:d + 1, 128:192].to_broadcast([64, 4, 64]), op=ALU.mult, r=[psP, msk], w=[pk])
                        I("act", "copy", out=pk[:, 1, :, :], in_=Mbb[:, :, 0:64], r=[Mbb], w=[pk])
                        if RW_LEVEL < 4:
                            continue
                        for x, h in enumerate(hs):
                            I("pe", "matmul", psX[:, x * 64:(x + 1) * 64], lhsT=Mkb[:, x, 0:64], rhs=vtok[:, h, :], start=True, stop=False, r=[Mkb, vtok], w=[psX])
                            I("pe", "matmul", psX[:, x * 64:(x + 1) * 64], lhsT=AR[:, h, q, 0, :], rhs=STb[:, h, :], start=False, stop=True, r=[AR, STb], w=[psX])
                        I("dve", "tensor_copy", out=U[:].rearrange("p x n -> p (x n)"), in_=psX[:, 0:256], r=[psX], w=[U])
                        I("dve", "tensor_copy", out=Ub[:], in_=U[:], r=[U], w=[Ub])
                        for lev in range(RW_NLEV):
                            pk = Pk[lev % 2]
                            for x in range(4):
                                I("pe", "matmul", psX[:, x * 64:(x + 1) * 64], lhsT=pk[:, 1, x, :], rhs=Ub[:, x, :], start=True, stop=True, r=[pk, Ub], w=[psX])
                            if lev < 5:
                                pn = Pk[(lev + 1) % 2]
                                for x in range(4):
                                    I("pe", "matmul", psQ[:, x * 64:(x + 1) * 64], lhsT=pk[:, 1, x, :], rhs=pk[:, 0, x, :], start=True, stop=True, r=[pk], w=[psQ])
                                    I("pe", "matmul", psQ[:, 256 + x * 64:256 + (x + 1) * 64], lhsT=pk[:, 0, x, :], rhs=pk[:, 1, x, :], start=True, stop=True, r=[pk], w=[psQ])
                                I("act", "copy", out=pn[:].rearrange("p a x n -> p (a x n)"), in_=psQ[:, 0:512], r=[psQ], w=[pn])
                            I("dve", "tensor_tensor", out=U[:].rearrange("p x n -> p (x n)"), in0=U[:].rearrange("p x n -> p (x n)"), in1=psX[:, 0:256], op=ALU.add, r=[U, psX], w=[U])
                            I("dve", "tensor_copy", out=Ub[:], in_=U[:], r=[U], w=[Ub])
                        if RW_LEVEL < 5:
                            continue
                        for x, h in enumerate(hs):
                            I("pe", "matmul", psP[:, 256 + x * 64:256 + (x + 1) * 64], lhsT=vtok[:, h, :], rhs=Mkb[:, x, 64:128], start=True, stop=False, r=[vtok, Mkb], w=[psP])
                            I("pe", "matmul", psP[:, 256 + x * 64:256 + (x + 1) * 64], lhsT=Ub[:, x, :], rhs=Mbb[:, x, 64:128], start=False, stop=False, r=[Ub, Mbb], w=[psP])
                            I("pe", "matmul", psP[:, 256 + x * 64:256 + (x + 1) * 64], lhsT=STb[:, h, :], rhs=AR[:, h, q, 1, :], start=False, stop=True, r=[STb, AR], w=[psP])
                        I("act", "copy", out=yblk[:, hg * 4:hg * 4 + 4, q * 64:(q + 1) * 64], in_=psP[:, 256:512].rearrange("p (x n) -> p x n", x=4), r=[psP], w=[yblk])
                        if RW_LEVEL < 6:
                            continue
                        for x, h in enumerate(hs):
                            I("pe", "matmul", psQ[:, x * 64:(x + 1) * 64], lhsT=kbtok[:, 0, h, :], rhs=vtok[:, h, :], start=True, stop=False, r=[kbtok, vtok], w=[psQ])
                            I("pe", "matmul", psQ[:, x * 64:(x + 1) * 64], lhsT=kbtok[:, 1, h, :], rhs=Ub[:, x, :], start=False, stop=True, r=[kbtok, Ub], w=[psQ])
                        I("dve", "tensor_tensor", out=tS[:], in0=ST[:, hg * 4:hg * 4 + 4, :], in1=psQ[:, 0:256].rearrange("p (x n) -> p x n", x=4), op=ALU.add, r=[ST, psQ], w=[tS])
                        wv = wcc[:].rearrange("p (h c) -> p h c", c=4)[:, hg * 4:hg * 4 + 4, q:q + 1]
                        I("dve", "tensor_tensor", out=ST[:, hg * 4:hg * 4 + 4, :], in0=tS[:], in1=wv.to_broadcast([64, 4, 64]), op=ALU.mult, r=[tS, wcc], w=[ST])
                        I("act", "copy", out=STb[:, hg * 4:hg * 4 + 4, :], in_=ST[:, hg * 4:hg * 4 + 4, :], r=[ST], w=[STb])
                        if RW_BAR:
                            S.barrier()
                S.dma("act", yd[d].t[:, s0:s0 + NB].rearrange("(h i) t -> i h t", i=64), yblk[:], yblk, r=[yblk], w=[yd[d]])
            S.pop()
        g2f = S.sb([128, 2, 512], F32, "g2f")
        g2b = S.sb([128, 2, 512], BF16, "g2b")
        S.dma("sp", g2f[:], self.ext["rg2"].t[:, l], g2f, w=[g2f])
        I("dve", "tensor_copy", out=g2b[:], in_=g2f[:], r=[g2f], w=[g2b])
        rgl = S.sb([128, 2, 3], F32, "rgl")
        S.dma("sp", rgl[:], self.ext["rgl"].t[:, l], rgl, w=[rgl])
        NF = 256
        y0 = S.sb([64, 8, NF], F32, "fy0")
        y1 = S.sb([64, 8, NF], F32, "fy1")
        b0 = S.sb([64, 8, NF], F32, "fb0")
        b1 = S.sb([64, 8, NF], F32, "fb1")
        yb = S.sb([64, 8, NF], BF16, "fyb")
        mu = S.sb([64, 8, NF], F32, "fmu")
        va = S.sb([64, 8, NF], F32, "fva")
        xg = S.sb([128, 2, NF + 2], F32, "fxg")
        sg = S.sb([128, 2, NF], F32, "fsg")
        sgb = S.sb([128, 2, NF], BF16, "fsgb")
        ob = S.sb([64, 8, NF], BF16, "fob")
        psA = S.ps([64, 512], F32, "fpsA")
        psB = S.ps([64, 512], F32, "fpsB")
        psG = S.ps([64, 512], F32, "fpsG")
        for (s0, lo, hi) in ([(0, 0, 256)] + [(256 + NF * q, 256, T) for q in range(16)])[:RW_NFIN]:
            for (tl, src) in ((y0, yd[0]), (y1, yd[1]), (b0, bon[0]), (b1, bon[1])):
                S.dma("sp", tl[:], src.t[:, s0:s0 + NF].rearrange("(h i) t -> i h t", i=64), tl, r=[src], w=[tl])
            I("dve", "tensor_tensor", out=y0[:], in0=y0[:], in1=y1[:], op=ALU.add, r=[y0, y1], w=[y0])
            I("dve", "tensor_tensor", out=b0[:], in0=b0[:], in1=b1[:], op=ALU.add, r=[b0, b1], w=[b0])
            I("act", "copy", out=yb[:], in_=y0[:], r=[y0], w=[yb])
            for hp in range(4):
                ps = psA if hp % 2 == 0 else psB
                I("pe", "matmul", ps[:, 0:2 * NF], lhsT=ones[0:64, 0:64], rhs=yb[:, hp * 2:hp * 2 + 2, :], start=True, stop=True, r=[ones, yb], w=[ps])
                I("act", "activation", out=mu[:, hp * 2:hp * 2 + 2, :], in_=ps[:, 0:2 * NF].rearrange("p (h n) -> p h n", h=2), func=AF.Identity, scale=1.0 / 64, r=[ps], w=[mu])
            I("dve", "tensor_tensor", out=y0[:], in0=y0[:], in1=mu[:], op=ALU.subtract, r=[y0, mu], w=[y0])
            I("act", "activation", out=yb[:], in_=y0[:], func=AF.Square, r=[y0], w=[yb])
            for hp in range(4):
                ps = psA if hp % 2 == 0 else psB
                I("pe", "matmul", ps[:, 0:2 * NF], lhsT=ones[0:64, 0:64], rhs=yb[:, hp * 2:hp * 2 + 2, :], start=True, stop=True, r=[ones, yb], w=[ps])
                I("dve", "tensor_scalar", out=va[:, hp * 2:hp * 2 + 2, :], in0=ps[:, 0:2 * NF].rearrange("p (h n) -> p h n", h=2), scalar1=1.0 / 64, scalar2=64e-5, op0=ALU.mult, op1=ALU.add, r=[ps], w=[va])
            I("act", "activation", out=va[:], in_=va[:], func=AF.Sqrt, r=[va], w=[va])
            I("dve", "reciprocal", out=va[:], in_=va[:], r=[va], w=[va])
            I("dve", "tensor_tensor", out=y0[:], in0=y0[:], in1=va[:], op=ALU.mult, r=[y0, va], w=[y0])
            I("dve", "tensor_tensor", out=y0[:], in0=y0[:], in1=rwt[:, :, 13:14].to_broadcast([64, 8, NF]), op=ALU.mult, r=[y0, rwt], w=[y0])
            I("dve", "tensor_tensor", out=y0[:], in0=y0[:], in1=rwt[:, :, 14:15].to_broadcast([64, 8, NF]), op=ALU.add, r=[y0, rwt], w=[y0])
            I("dve", "tensor_tensor", out=y0[:], in0=y0[:], in1=b0[:], op=ALU.add, r=[y0, b0], w=[y0])
            a0, a1 = max(lo, s0 - 1), min(hi, s0 + NF + 1)
            c0_, c1_ = a0 - (s0 - 1), a1 - (s0 - 1)
            I("pool", "memset", xg[:], 0.0, w=[xg])
            S.dma("act", xg[:, 0, c0_:c1_], pT.t[c_g:c_g + 128, a0:a1], xg, r=[pT], w=[xg])
            S.dma("act", xg[0:32, 1, c0_:c1_], pT.t[c_g + 128:c_g + 160, a0:a1], xg, r=[pT], w=[xg])
            for q in range(2):
                I("dve", "tensor_scalar", out=sg[:, q, :], in0=xg[:, q, 1:NF + 1], scalar1=rgl[:, q, 0:1], scalar2=None, op0=ALU.mult, r=[xg, rgl], w=[sg])
                I("dve", "scalar_tensor_tensor", out=sg[:, q, :], in0=xg[:, q, 0:NF], scalar=rgl[:, q, 1:2], in1=sg[:, q, :], op0=ALU.mult, op1=ALU.add, r=[xg, rgl, sg], w=[sg])
                I("dve", "scalar_tensor_tensor", out=sg[:, q, :], in0=xg[:, q, 2:NF + 2], scalar=rgl[:, q, 2:3], in1=sg[:, q, :], op0=ALU.mult, op1=ALU.add, r=[xg, rgl, sg], w=[sg])
            I("act", "activation", out=sgb[:], in_=sg[:], func=AF.Sigmoid, r=[sg], w=[sgb])
            for hp in range(4):
                for hh in range(2):
                    h = hp * 2 + hh
                    I("pe", "matmul", psG[:, hh * NF:(hh + 1) * NF], lhsT=g2b[:, 0, h * 64:(h + 1) * 64], rhs=sgb[:, 0, :], start=True, stop=False, r=[g2b, sgb], w=[psG])
                    I("pe", "matmul", psG[:, hh * NF:(hh + 1) * NF], lhsT=g2b[0:32, 1, h * 64:(h + 1) * 64], rhs=sgb[0:32, 1, :], start=False, stop=True, r=[g2b, sgb], w=[psG])
                I("dve", "tensor_tensor", out=ob[:, hp * 2:hp * 2 + 2, :], in0=y0[:, hp * 2:hp * 2 + 2, :], in1=psG[:, 0:2 * NF].rearrange("p (h n) -> p h n", h=2), op=ALU.mult, r=[y0, psG], w=[ob])
            for (ap, off, ln) in self.mix_dst(mixT, 512, 512, s0, NF):
                S.dma("pool", ap.rearrange("(h i) n -> i h n", i=64), ob[:, :, off:off + ln], ob, r=[ob], w=[mixT])
        S.pop()

    def convert_mla(self):
        S = self.S
        NL = self.NL
        wuq = self.ext_in("wuq", [NL, 1536, 1536], F32)
        wukv = self.ext_in("wukv", [NL, 512, 2048], F32)
        self.wuqb = S.dram("wuqb", [NL, 8, 128, 12, 192], BF16)
        self.wukvb = S.dram("wukvb", [NL, 128, 4, 2048], BF16)
        S.push()
        f = [S.sb([128, 2048], F32, "mcf") for _ in range(2)]
        b = [S.sb([128, 2048], BF16, "mcb") for _ in range(2)]
        eng = RR(["dve", "pool", "act"])
        it = 0
        for l in range(NL):
            for c in range(12):
                ff, bb = f[it % 2], b[it % 2]
                it += 1
                S.dma("sp", ff[:, 0:1536], wuq.t[l, c * 128:(c + 1) * 128, :], ff, w=[ff])
                for q in range(2):
                    copy_on(S, eng(), bb[:, q * 768:(q + 1) * 768], ff[:, q * 768:(q + 1) * 768], [ff], [bb])
                S.dma("pool", self.wuqb.t[l, :, :, c, :].rearrange("h p x -> p h x"),
                      bb[:, 0:1536].rearrange("p (h x) -> p h x", x=192), bb, r=[bb], w=[self.wuqb])
            for c in range(4):
                ff, bb = f[it % 2], b[it % 2]
                it += 1
                S.dma("sp", ff[:], wukv.t[l, c * 128:(c + 1) * 128, :], ff, w=[ff])
                for q in range(2):
                    copy_on(S, eng(), bb[:, q * 1024:(q + 1) * 1024], ff[:, q * 1024:(q + 1) * 1024], [ff], [bb])
                S.dma("pool", self.wukvb.t[l, :, c, :], bb[:], bb, r=[bb], w=[self.wukvb])
        S.pop()

    def rstd_from_ps(self, ps, out_sb, rows, n, inv_n, eps):
        S = self.S
        S.op("dve", lambda e: e.tensor_scalar(out=out_sb[0:rows, 0:n], in0=ps[0:rows, 0:n], scalar1=inv_n, scalar2=eps,
                                              op0=ALU.mult, op1=ALU.add), r=[ps], w=[out_sb])
        S.op("act", lambda e: e.activation(out=out_sb[0:rows, 0:n], in_=out_sb[0:rows, 0:n], func=AF.Sqrt), r=[out_sb], w=[out_sb])
        S.op("dve", lambda e: e.reciprocal(out=out_sb[0:rows, 0:n], in_=out_sb[0:rows, 0:n]), r=[out_sb], w=[out_sb])

    def mla_stage(self, l, pT, mixT):
        S = self.S
        I = S.i
        SCALE = float((128 + 64) ** -0.5)
        blocks = [(0, 256)] + [(256 + 512 * q, 512) for q in range(8)]
        S.push()
        mg = S.sb([128, 2, 24], F32, "mg")
        S.dma("sp", mg[:], self.ext["mlag"].t, mg, w=[mg])
        qsc = S.sb([128, 2], F32, "qsc")
        I("dve", "tensor_scalar", out=qsc[:], in0=mg[:, l, 16:18], scalar1=SCALE, scalar2=None, op0=ALU.mult, r=[mg], w=[qsc])
        rotf = S.sb([64, 64], F32, "rotf")
        rotb = S.sb([64, 64], BF16, "rotb")
        S.dma("sp", rotf[:], self.ext["rotm"].t, rotf, w=[rotf])
        I("dve", "tensor_copy", out=rotb[:], in_=rotf[:], r=[rotf], w=[rotb])
        nbias = S.sb([128, 1], F32, "nbias")
        I("dve", "memset", nbias[:], -6.0, w=[nbias])
        wkv = S.sb([128, 4, 2048], BF16, "wkv")
        S.dma("sp", wkv[:], self.wukvb.t[l], wkv, r=[self.wukvb], w=[wkv])
        krT = S.sb([64, T], BF16, "krT")
        kT = S.sb([128, 4, T], BF16, "kT")
        vt = S.sb([128, 34, 512], BF16, "vt")
        wq = [S.sb([128, 12, 192], BF16, "wq") for _ in range(2)]
        xin = S.sb([128, 4, 512], F32, "xin")
        krin = S.sb([64, 512], F32, "krin")
        ropeb = S.sb([64, 2, 512], F32, "ropeb")
        xsq = S.sb([128, 512], BF16, "xsq")
        xnb = S.sb([128, 12, 512], BF16, "xnb")
        rs = S.sb([128, 512], F32, "rs")
        rs2 = S.sb([128, 512], F32, "rs2")
        tmp = S.sb([128, 512], F32, "mtmp")
        tmp2 = S.sb([64, 512], F32, "mtmp2")
        xrb = S.sb([64, 512], BF16, "xrb")
        qnb = S.sb([128, 512], BF16, "qnb")
        qrb = S.sb([64, 512], BF16, "qrb")
        pb = [S.sb([128, 512], BF16, "pb") for _ in range(2)]
        ob = S.sb([128, 512], BF16, "mob")
        ps_s = [S.ps([128, 512], F32, "ps_s") for _ in range(2)]
        ps_o = S.ps([128, 512], F32, "ps_o")
        ps_d = S.ps([128, 512], F32, "ps_d")
        ps_a = S.ps([128, 512], F32, "ps_a")
        ps_b = S.ps([128, 512], F32, "ps_b")
        ps_n = S.ps([128, 512], F32, "ps_n")
        c_kv, c_kr, c_q = COLS["mkv"][0], COLS["mkr"][0], COLS["mq"][0]
        ones = self.ones_bf

        def norm_in(col0, nch, s0, n, gcol0, inv_n):
            for c4 in range(0, nch, 4):
                S.dma("sp", xin[:, :, 0:n], pT.t[col0 + c4 * 128:col0 + (c4 + 4) * 128, s0:s0 + n].rearrange("(c p) n -> p c n", p=128), xin, r=[pT], w=[xin])
                for cc in range(4):
                    c = c4 + cc
                    I("act", "activation", out=xsq[:, 0:n], in_=xin[:, cc, 0:n], func=AF.Square, r=[xin], w=[xsq])
                    I("pe", "matmul", ps_n[:, 0:n], lhsT=ones[:], rhs=xsq[:, 0:n], start=(c == 0), stop=(c == nch - 1), r=[xsq, ones], w=[ps_n])
            self.rstd_from_ps(ps_n, rs, 128, n, inv_n, 1e-6)
            for c4 in range(0, nch, 4):
                S.dma("sp", xin[:, :, 0:n], pT.t[col0 + c4 * 128:col0 + (c4 + 4) * 128, s0:s0 + n].rearrange("(c p) n -> p c n", p=128), xin, r=[pT], w=[xin])
                for cc in range(4):
                    c = c4 + cc
                    I("dve", "tensor_tensor", out=xin[:, cc, 0:n], in0=xin[:, cc, 0:n], in1=rs[:, 0:n], op=ALU.mult, r=[xin, rs], w=[xin])
                    I("act", "activation", out=xnb[:, c, 0:n], in_=xin[:, cc, 0:n], func=AF.Identity, scale=mg[:, l, gcol0 + c:gcol0 + c + 1], r=[xin, mg], w=[xnb])

        def head_norm(src, rows, n, gain_ap, out_ap, out_buf):
            I("act", "activation", out=xsq[0:rows, 0:n], in_=src[0:rows, 0:n], func=AF.Square, r=[src], w=[xsq])
            I("pe", "matmul", ps_n[0:rows, 0:n], lhsT=ones[0:rows, 0:rows], rhs=xsq[0:rows, 0:n], start=True, stop=True, r=[xsq, ones], w=[ps_n])
            self.rstd_from_ps(ps_n, rs2, rows, n, 1.0 / rows, 1e-6)
            I("dve", "tensor_tensor", out=tmp[0:rows, 0:n], in0=src[0:rows, 0:n], in1=rs2[0:rows, 0:n], op=ALU.mult, r=[src, rs2], w=[tmp])
            I("act", "activation", out=out_ap, in_=tmp[0:rows, 0:n], func=AF.Identity, scale=gain_ap, r=[tmp, mg, qsc], w=[out_buf])

        def rope_apply(n, out_ap, out_buf):
            I("dve", "tensor_copy", out=xrb[:, 0:n], in_=tmp2[:, 0:n], r=[tmp2], w=[xrb])
            I("pe", "matmul", ps_n[0:64, 0:n], lhsT=rotb[:], rhs=xrb[:, 0:n], start=True, stop=True, r=[rotb, xrb], w=[ps_n])
            I("dve", "tensor_tensor", out=tmp[0:64, 0:n], in0=ps_n[0:64, 0:n], in1=ropeb[:, 1, 0:n], op=ALU.mult, r=[ps_n, ropeb], w=[tmp])
            I("dve", "tensor_tensor", out=tmp2[:, 0:n], in0=tmp2[:, 0:n], in1=ropeb[:, 0, 0:n], op=ALU.mult, r=[tmp2, ropeb], w=[tmp2])
            I("dve", "tensor_tensor", out=out_ap, in0=tmp2[:, 0:n], in1=tmp[0:64, 0:n], op=ALU.add, r=[tmp2, tmp], w=[out_buf])

        def load_rope(s0, n):
            if s0 >= 256:
                S.dma("act", ropeb[:, :, 0:n], self.ext["ropeT"].t[:, :, s0 - 256:s0 - 256 + n], ropeb, w=[ropeb])

        wqi = 0
        for g in range(2):
            for bi, (s0, n) in enumerate(blocks):
                norm_in(c_kv, 4, s0, n, 12, 1.0 / 512)
                for hh in range(4):
                    h = g * 4 + hh
                    for c in range(4):
                        I("pe", "matmul", ps_a[:, 0:n], lhsT=wkv[:, c, h * 256:h * 256 + 128], rhs=xnb[:, c, 0:n], start=(c == 0), stop=(c == 3), r=[wkv, xnb], w=[ps_a])
                    head_norm(ps_a, 128, n, mg[:, l, 18:19], kT[:, hh, s0:s0 + n], kT)
                for tt in range(n // 128):
                    for hh in range(4):
                        h = g * 4 + hh
                        for c in range(4):
                            I("pe", "matmul", ps_b[:, hh * 128:(hh + 1) * 128], lhsT=xnb[:, c, tt * 128:(tt + 1) * 128],
                              rhs=wkv[:, c, h * 256 + 128:h * 256 + 256], start=(c == 0), stop=(c == 3), r=[wkv, xnb], w=[ps_b])
                    ti = s0 // 128 + tt
                    I("act", "copy", out=vt[:, ti, :], in_=ps_b[:, 0:512], r=[ps_b], w=[vt])
                if g == 0:
                    load_rope(s0, n)
                    S.dma("sp", krin[:, 0:n], pT.t[c_kr:c_kr + 64, s0:s0 + n], krin, r=[pT], w=[krin])
                    head_norm(krin, 64, n, mg[0:64, l, 19:20], tmp2[:, 0:n], tmp2)
                    if s0 >= 256:
                        rope_apply(n, krT[:, s0:s0 + n], krT)
                    else:
                        I("dve", "tensor_copy", out=krT[:, s0:s0 + n], in_=tmp2[:, 0:n], r=[tmp2], w=[krT])
            for bi, (s0, n) in enumerate(blocks):
                norm_in(c_q, 12, s0, n, 0, 1.0 / 1536)
                load_rope(s0, n)
                kcs = list(range(2)) if bi == 0 else list(range(34))
                for hh in range(4):
                    h = g * 4 + hh
                    wqh = wq[wqi % 2]
                    wqi += 1
                    S.dma("act", wqh[:], self.wuqb.t[l, h], wqh, r=[self.wuqb], w=[wqh])
                    for c in range(12):
                        I("pe", "matmul", ps_a[:, 0:n], lhsT=wqh[:, c, 0:128], rhs=xnb[:, c, 0:n], start=(c == 0), stop=(c == 11), r=[wqh, xnb], w=[ps_a])
                    for c in range(12):
                        I("pe", "matmul", ps_b[0:64, 0:n], lhsT=wqh[:, c, 128:192], rhs=xnb[:, c, 0:n], start=(c == 0), stop=(c == 11), r=[wqh, xnb], w=[ps_b])
                    head_norm(ps_a, 128, n, qsc[:, 0:1], qnb[:, 0:n], qnb)
                    head_norm(ps_b, 64, n, qsc[0:64, 1:2], tmp2[:, 0:n], tmp2)
                    if s0 >= 256:
                        rope_apply(n, qrb[:, 0:n], qrb)
                    else:
                        I("dve", "tensor_copy", out=qrb[:, 0:n], in_=tmp2[:, 0:n], r=[tmp2], w=[qrb])
                    for ki, kc in enumerate(kcs):
                        ps = ps_s[ki % 2]
                        pbb = pb[ki % 2]
                        I("pe", "matmul", ps[:, 0:n], lhsT=kT[:, hh, kc * 128:(kc + 1) * 128], rhs=qnb[:, 0:n], start=True, stop=False, r=[kT, qnb], w=[ps])
                        I("pe", "matmul", ps[:, 0:n], lhsT=krT[:, kc * 128:(kc + 1) * 128], rhs=qrb[:, 0:n], start=False, stop=True, r=[krT, qrb], w=[ps])
                        I("act", "activation", out=pbb[:, 0:n], in_=ps[:, 0:n], func=AF.Exp, bias=nbias[:, 0:1], r=[ps, nbias], w=[pbb])
                        first, last = (ki == 0), (ki == len(kcs) - 1)
                        I("pe", "matmul", ps_o[:, 0:n], lhsT=vt[:, kc, hh * 128:(hh + 1) * 128], rhs=pbb[:, 0:n], start=first, stop=last, r=[vt, pbb], w=[ps_o])
                        I("pe", "matmul", ps_d[:, 0:n], lhsT=ones[:], rhs=pbb[:, 0:n], start=first, stop=last, r=[ones, pbb], w=[ps_d])
                    I("dve", "reciprocal", out=rs2[:, 0:n], in_=ps_d[:, 0:n], r=[ps_d], w=[rs2])
                    I("dve", "tensor_tensor", out=ob[:, 0:n], in0=ps_o[:, 0:n], in1=rs2[:, 0:n], op=ALU.mult, r=[ps_o, rs2], w=[ob])
                    for (ap, off, ln) in self.mix_dst(mixT, 1024 + h * 128, 128, s0, n):
                        S.dma("pool", ap, ob[:, off:off + ln], ob, r=[ob], w=[mixT])
        S.pop()


    def gather_chunks(self, src, nch, name):
        S = self.S
        mid = S.dram(name + "_q", [nch, 4, 524288], BF16)
        dst = Buf(name + "_g")
        dst.t = [self.nc.dram_tensor(f"{name}_g{j}", [min(16, nch - 16 * j), 4, 2, 524288], BF16).ap() for j in range((nch + 15) // 16)]
        sem = Buf(name + "_sem")
        for i in range(nch):
            S.coll("AllGather", src.t[i:i + 1, :], mid.t[i], QUADS, sem, r=[src], w=[mid])
        for i in range(nch):
            for q in range(4):
                S.coll("AllGather", mid.t[i, q:q + 1, :], dst.t[i // 16][i % 16, q], P4, sem, r=[mid], w=[dst])
        return dst

    def convert_big(self):
        S = self.S
        wo = self.ext_in("wo", [512 * self.NL, D], F32)
        src_o = S.dram("wo_src", [4 * self.NL, 524288], BF16)
        S.push()
        f = [S.sb([128, D], F32, "bf") for _ in range(2)]
        b = [S.sb([128, D], BF16, "bb") for _ in range(2)]
        eng = RR(["dve", "pool", "act"])
        it = 0
        for i in range(4 * self.NL):
            ff, bb = f[it % 2], b[it % 2]
            it += 1
            S.dma("sp", ff[:], wo.t[i * 128:(i + 1) * 128, :], ff, w=[ff])
            for q in range(4):
                copy_on(S, eng(), bb[:, q * 1024:(q + 1) * 1024], ff[:, q * 1024:(q + 1) * 1024], [ff], [bb])
            S.dma("pool", src_o.t[i].rearrange("(p n) -> p n", p=128), bb[:], bb, r=[bb], w=[src_o])
        self.wo_g = self.gather_chunks(src_o, 4 * self.NL, "wo")
        self.moe_g = {}
        if self.moe_on:
            for nm in ("w1", "w3", "w2"):
                nle = 2 * self.NL
                w = self.ext_in("m" + nm, [nle, D if nm != "w2" else 1024, 1024 if nm != "w2" else D], F32)
                src = S.dram(nm + "_src", [nle * 8, 524288], BF16)
                for i in range(nle):
                    if nm == "w2":
                        for part in range(8):
                            ff, bb = f[it % 2], b[it % 2]
                            it += 1
                            S.dma("sp", ff[:], w.t[i, part * 128:(part + 1) * 128, :], ff, w=[ff])
                            for q in range(4):
                                copy_on(S, eng(), bb[:, q * 1024:(q + 1) * 1024], ff[:, q * 1024:(q + 1) * 1024], [ff], [bb])
                            S.dma("pool", src.t[i * 8 + part].rearrange("(p n) -> p n", p=128), bb[:], bb, r=[bb], w=[src])
                    else:
                        for c4 in range(0, 32, 4):
                            ff, bb = f[it % 2], b[it % 2]
                            it += 1
                            S.dma("sp", ff[:].rearrange("p (c n) -> p c n", c=4),
                                  w.t[i, c4 * 128:(c4 + 4) * 128, :].rearrange("(c p) n -> p c n", p=128), ff, w=[ff])
                            for q in range(4):
                                copy_on(S, eng(), bb[:, q * 1024:(q + 1) * 1024], ff[:, q * 1024:(q + 1) * 1024], [ff], [bb])
                            for dc in range(8):
                                S.dma("pool", src.t[i * 8 + dc].rearrange("(p c n) -> p c n", p=128, c=32)[:, c4:c4 + 4, :],
                                      bb[:].rearrange("p (c d n) -> p c d n", c=4, d=8)[:, :, dc, :], bb, r=[bb], w=[src])
                self.moe_g[nm] = self.gather_chunks(src, nle * 8, nm)
        S.pop()

    def wo_block(self, l, kc):
        per = 4 * self.NL
        g = l * 32 + kc
        r_, ch = g // per, g % per
        return self.wo_g.t[ch // 16][ch % 16, r_ % 4, r_ // 4].rearrange("(p n) -> p n", p=128)

    def moe_chunk(self, nm, l, e, part):
        nle = 2 * self.NL
        le = l * 16 + e
        r_, i = le // nle, le % nle
        ch = i * 8 + part
        return self.moe_g[nm].t[ch // 16][ch % 16, r_ % 4, r_ // 4]

    def bcast_table(self, tab_ap_fn, out_sb, ps):
        S = self.S
        I = S.i
        S.push()
        dg = [S.sb([128, 128], F32, "dg") for _ in range(2)]
        dh = [S.sb([128, 2, 128], BF16, "dh") for _ in range(2)]
        for c in range(32):
            d_, h_ = dg[c % 2], dh[c % 2]
            p = ps[(c // 4) % 2]
            I("dve", "tensor_scalar", out=d_[:], in0=self.identf[:], scalar1=tab_ap_fn(c), scalar2=None, op0=ALU.mult, r=[self.identf] + self.mods, w=[d_])
            I("dve", "tensor_copy", out=h_[:, 0, :], in_=d_[:], r=[d_], w=[h_])
            I("dve", "tensor_tensor", out=d_[:], in0=d_[:], in1=h_[:, 0, :], op=ALU.subtract, r=[d_, h_], w=[d_])
            I("dve", "tensor_copy", out=h_[:, 1, :], in_=d_[:], r=[d_], w=[h_])
            for q in range(2):
                I("pe", "matmul", p[:, (c % 4) * 128:(c % 4 + 1) * 128], lhsT=self.ones_bf[:], rhs=h_[:, q, :], start=(q == 0), stop=(q == 1), r=[h_, self.ones_bf], w=[p])
            if c % 4 == 3:
                I("act", "copy", out=out_sb[:, (c - 3) * 128:(c + 1) * 128], in_=p[:, :], r=[p], w=[out_sb])
        S.pop()

    def outproj_stage(self, l, mix_all, h_src, h_mid):
        S = self.S
        I = S.i
        S.push()
        idx = S.sb([128, 5, 32], I32, "mixidx")
        S.dma("sp", idx[:], self.ext["mixidx"].t, idx, w=[idx])
        gab = S.sb([128, D], F32, "ga1bc")
        mixb = S.sb([128, 32, 512], BF16, "mixb")
        wb = [S.sb([128, 32, 512], BF16, "wob") for _ in range(2)]
        hb = S.sb([128, 4, D], F32, "ohb")
        ps = [S.ps([128, 512], F32, "ops") for _ in range(4)]
        tmp = [S.sb([128, 512], F32, "otmp") for _ in range(2)]
        flat = mix_all.t.rearrange("c r p n -> (c r p) n")
        blocks = [(0, 128, 1)] + [(128 + 512 * q, 512, 0) for q in range(4)]
        cur = None
        pi = 0
        wi = 0
        for bi, (t0, n, which) in enumerate(blocks):
            if cur != which:
                self.bcast_table(lambda c, which=which: self.modcol(which, l, 2, c), gab, ps[0:2])
                cur = which
            for kc in range(32):
                S.gather_rows(mixb[:, kc, :], flat, idx[:, bi, kc:kc + 1], mixb, r=[mix_all, idx], w=[mixb])
            nt = n // 128
            for ti in range(nt):
                S.dma("act", hb[:, ti, :], h_src.t[t0 + ti * 128:t0 + (ti + 1) * 128, :], hb, r=[h_src], w=[hb])
            for dcn in range(8):
                w = wb[wi % 2]
                wi += 1
                for kc in range(32):
                    S.dma("sp" if kc % 2 else "act", w[:, kc, :], self.wo_block(l, kc)[:, dcn * 512:(dcn + 1) * 512], w, r=[self.wo_g], w=[w])
                for ti in range(nt):
                    p = ps[pi % 4]
                    t_ = tmp[pi % 2]
                    pi += 1
                    for kc in range(32):
                        I("pe", "matmul", p[:, :], lhsT=mixb[:, kc, ti * 128:(ti + 1) * 128], rhs=w[:, kc, :], start=(kc == 0), stop=(kc == 31), r=[mixb, w], w=[p])
                    I("dve", "tensor_tensor", out=t_[:], in0=p[:, :], in1=gab[:, dcn * 512:(dcn + 1) * 512], op=ALU.mult, r=[p, gab], w=[t_])
                    I("dve", "tensor_tensor", out=hb[:, ti, dcn * 512:(dcn + 1) * 512], in0=hb[:, ti, dcn * 512:(dcn + 1) * 512], in1=t_[:], op=ALU.add, r=[hb, t_], w=[hb])
            for ti in range(nt):
                S.dma("pool", h_mid.t[t0 + ti * 128:t0 + (ti + 1) * 128, :], hb[:, ti, :], hb, r=[hb], w=[h_mid])
        S.pop()

    def moe_stage(self, l, n2T, h_mid, h_out, final_out=None):
        S = self.S
        I = S.i
        S.push()
        wr = S.sb([128, 32, 16], F32, "wrf")
        wrb = S.sb([128, 32, 16], BF16, "wrb")
        S.dma("sp", wr[:], self.ext["wrT"].t, wr, w=[wr])
        I("dve", "tensor_copy", out=wrb[:], in_=wr[:], r=[wr], w=[wrb])
        rb = S.sb([128, 16], F32, "rbias")
        S.dma("sp", rb[:], self.ext["rbias"].t, rb, w=[rb])
        selEb = S.sb([16, 16, 128], BF16, "selEb")
        I("dve", "tensor_copy", out=selEb[:], in_=self.identf[0:16, 0:16].unsqueeze(2).to_broadcast([16, 16, 128]), r=[self.identf], w=[selEb])
        gab = S.sb([128, D], F32, "ga2bc")
        nb = S.sb([128, 32, 512], BF16, "mnb")
        yacc = S.sb([128, 4, D], F32, "yacc")
        uT = S.sb([128, 8, 512], BF16, "uT")
        w13 = [S.sb([128, 32, 128], BF16, "w13") for _ in range(3)]
        w2b = [S.sb([128, 8, 512], BF16, "w2b") for _ in range(2)]
        gbc = S.sb([128, 512], F32, "gbc")
        sa = S.sb([128, 512], F32, "msa")
        rt = S.sb([128, 64], F32, "rt")
        gate = S.sb([128, 4, 16], F32, "gate")
        ghl = S.sb([128, 2, 16], BF16, "ghl")
        gT = S.sb([16, 2, 512], BF16, "gT")
        ps_a = [S.ps([128, 512], F32, "mpa") for _ in range(2)]
        ps_b = [S.ps([128, 512], F32, "mpb") for _ in range(2)]
        ps_y = [S.ps([128, 512], F32, "mpy") for _ in range(2)]
        ps_r = S.ps([128, 512], F32, "mpr")
        ps_t = S.ps([16, 512], BF16, "mpt")
        blocks = [(0, 128, 1)] + [(128 + 512 * q, 512, 0) for q in range(4)]
        cur = None
        wi = 0
        w2i = 0
        pi = 0
        for bi, (t0, n, which) in enumerate(blocks):
            if cur != which:
                self.bcast_table(lambda c, which=which: self.modcol(which, l, 5, c), gab, ps_y)
                cur = which
            nt = n // 128
            S.dma("sp", nb[:, :, 0:n], n2T.t[:, t0:t0 + n].rearrange("(c p) n -> p c n", p=128), nb, r=[n2T], w=[nb])
            for ti in range(nt):
                for c in range(32):
                    I("pe", "matmul", ps_r[:, 0:16], lhsT=nb[:, c, ti * 128:(ti + 1) * 128], rhs=wrb[:, c, :], start=(c == 0), stop=(c == 31), r=[nb, wrb], w=[ps_r])
                sc = rt[:, 0:16]
                bs = rt[:, 16:32]
                I("act", "activation", out=sc, in_=ps_r[:, 0:16], func=AF.Sigmoid, r=[ps_r], w=[rt])
                I("dve", "tensor_tensor", out=bs, in0=sc, in1=rb[:], op=ALU.add, r=[rt, rb], w=[rt])
                b4 = bs.rearrange("p (g k) -> p g k", k=4)
                pairs = [(0, 1), (0, 2), (0, 3), (1, 2), (1, 3), (2, 3)]
                psum6 = rt[:, 32:56].rearrange("p (g k) -> p g k", k=6)
                for qi, (x, y) in enumerate(pairs):
                    I("dve", "tensor_tensor", out=psum6[:, :, qi:qi + 1], in0=b4[:, :, x:x + 1], in1=b4[:, :, y:y + 1], op=ALU.add, r=[rt], w=[rt])
                gs = rt[:, 56:60]
                I("dve", "tensor_reduce", out=gs, in_=psum6, axis=AX.X, op=ALU.max, r=[rt], w=[rt])
                gm = rt[:, 60:64]
                I("dve", "tensor_reduce", out=gm, in_=b4, axis=AX.X, op=ALU.max, r=[rt], w=[rt])
                eqm = rt[:, 32:48].rearrange("p (g k) -> p g k", k=4)
                I("dve", "tensor_tensor", out=eqm, in0=b4, in1=gm.unsqueeze(2).to_broadcast([128, 4, 4]), op=ALU.is_ge, r=[rt], w=[rt])
                I("dve", "scalar_tensor_tensor", out=eqm, in0=eqm, scalar=-1.0e9, in1=b4, op0=ALU.mult, op1=ALU.add, r=[rt], w=[rt])
                thr = rt[:, 48:52]
                I("dve", "tensor_reduce", out=thr, in_=eqm, axis=AX.X, op=ALU.max, r=[rt], w=[rt])
                gmax = rt[:, 52:53]
                I("dve", "tensor_reduce", out=gmax, in_=gs, axis=AX.X, op=ALU.max, r=[rt], w=[rt])
                ing = rt[:, 56:60]
                I("dve", "tensor_scalar", out=ing, in0=gs, scalar1=gmax, scalar2=None, op0=ALU.is_ge, r=[rt], w=[rt])
                sel = rt[:, 32:48].rearrange("p (g k) -> p g k", k=4)
                I("dve", "tensor_tensor", out=sel, in0=b4, in1=thr.unsqueeze(2).to_broadcast([128, 4, 4]), op=ALU.is_ge, r=[rt], w=[rt])
                I("dve", "tensor_tensor", out=sel, in0=sel, in1=ing.unsqueeze(2).to_broadcast([128, 4, 4]), op=ALU.mult, r=[rt], w=[rt])
                g_ = gate[:, ti, :]
                I("dve", "tensor_tensor", out=g_, in0=rt[:, 32:48], in1=sc, op=ALU.mult, r=[rt], w=[gate])
                den = rt[:, 53:54]
                I("dve", "tensor_reduce", out=den, in_=g_, axis=AX.X, op=ALU.add, r=[gate], w=[rt])
                I("dve", "reciprocal", out=den, in_=den, r=[rt], w=[rt])
                I("dve", "tensor_scalar", out=g_, in0=g_, scalar1=den, scalar2=None, op0=ALU.mult, r=[gate, rt], w=[gate])
                I("dve", "tensor_copy", out=ghl[:, 0, :], in_=g_, r=[gate], w=[ghl])
                I("dve", "tensor_tensor", out=rt[:, 32:48], in0=g_, in1=ghl[:, 0, :], op=ALU.subtract, r=[gate, ghl], w=[rt])
                I("dve", "tensor_copy", out=ghl[:, 1, :], in_=rt[:, 32:48], r=[rt], w=[ghl])
                for q in range(2):
                    I("pe", "transpose", out=ps_t[:, q * 128:(q + 1) * 128], in_=ghl[:, q, :], identity=self.ident[:], r=[ghl, self.ident], w=[ps_t])
                I("act", "copy", out=gT[:, :, ti * 128:(ti + 1) * 128], in_=ps_t[:, 0:256].rearrange("p (q n) -> p q n", q=2), r=[ps_t], w=[gT])
            if "gate" in self.dbg and l == 0:
                S.dma("sp", self.outs["o_gate"].t[t0:t0 + n, :].rearrange("(t p) e -> p t e", p=128), gate[:, 0:nt, :], gate, r=[gate], w=[self.outs["o_gate"]])
            if not self.moe_on:
                continue
            for e in range(16):
                for q in range(2):
                    I("pe", "matmul", ps_r[:, 0:n], lhsT=selEb[:, e, :], rhs=gT[:, q, 0:n], start=(q == 0), stop=(q == 1), r=[selEb, gT], w=[ps_r])
                I("act", "copy", out=gbc[:, 0:n], in_=ps_r[:, 0:n], r=[ps_r], w=[gbc])
                for dc in range(8):
                    wa, wc = w13[wi % 3], w13[(wi + 1) % 3]
                    wi += 2
                    S.dma("sp", wa[:], self.moe_chunk("w1", l, e, dc).rearrange("(p c n) -> p c n", p=128, c=32), wa, r=[self.moe_g["w1"]], w=[wa])
                    S.dma("act", wc[:], self.moe_chunk("w3", l, e, dc).rearrange("(p c n) -> p c n", p=128, c=32), wc, r=[self.moe_g["w3"]], w=[wc])
                    pa, pb_ = ps_a[pi % 2], ps_b[pi % 2]
                    pi += 1
                    for c in range(32):
                        I("pe", "matmul", pa[:, 0:n], lhsT=wa[:, c, :], rhs=nb[:, c, 0:n], start=(c == 0), stop=(c == 31), r=[wa, nb], w=[pa])
                    for c in range(32):
                        I("pe", "matmul", pb_[:, 0:n], lhsT=wc[:, c, :], rhs=nb[:, c, 0:n], start=(c == 0), stop=(c == 31), r=[wc, nb], w=[pb_])
                    I("act", "activation", out=sa[:, 0:n], in_=pa[:, 0:n], func=AF.Silu, r=[pa], w=[sa])
                    I("dve", "tensor_tensor", out=sa[:, 0:n], in0=sa[:, 0:n], in1=pb_[:, 0:n], op=ALU.mult, r=[sa, pb_], w=[sa])
                    I("dve", "tensor_tensor", out=uT[:, dc, 0:n], in0=sa[:, 0:n], in1=gbc[:, 0:n], op=ALU.mult, r=[sa, gbc], w=[uT])
                for dcn in range(8):
                    w2 = w2b[w2i % 2]
                    w2i += 1
                    for dc in range(8):
                        S.dma("sp" if dc % 2 else "act", w2[:, dc, :],
                              self.moe_chunk("w2", l, e, dc).rearrange("(p n) -> p n", p=128)[:, dcn * 512:(dcn + 1) * 512], w2, r=[self.moe_g["w2"]], w=[w2])
                    for ti in range(nt):
                        py = ps_y[(dcn * 4 + ti) % 2]
                        for dc in range(8):
                            I("pe", "matmul", py[:, :], lhsT=uT[:, dc, ti * 128:(ti + 1) * 128], rhs=w2[:, dc, :], start=(dc == 0), stop=(dc == 7), r=[uT, w2], w=[py])
                        ya = yacc[:, ti, dcn * 512:(dcn + 1) * 512]
                        if e == 0:
                            I("act", "copy", out=ya, in_=py[:, :], r=[py], w=[yacc])
                        else:
                            I("dve", "tensor_tensor", out=ya, in0=ya, in1=py[:, :], op=ALU.add, r=[yacc, py], w=[yacc])
            for ti in range(nt):
                r0 = t0 + ti * 128
                I("dve", "tensor_tensor", out=yacc[:, ti, :], in0=yacc[:, ti, :], in1=gab[:], op=ALU.mult, r=[yacc, gab], w=[yacc])
                for dcn in range(8):
                    S.dma("sp", sa[:], h_mid.t[r0:r0 + 128, dcn * 512:(dcn + 1) * 512], sa, r=[h_mid], w=[sa])
                    I("dve", "tensor_tensor", out=yacc[:, ti, dcn * 512:(dcn + 1) * 512], in0=yacc[:, ti, dcn * 512:(dcn + 1) * 512], in1=sa[:], op=ALU.add, r=[yacc, sa], w=[yacc])
                if final_out is not None:
                    if r0 >= 128:
                        S.dma("pool", final_out.t[r0 - 128:r0, :], yacc[:, ti, :], yacc, r=[yacc], w=[final_out])
                else:
                    S.dma("pool", h_out.t[r0:r0 + 128, :], yacc[:, ti, :], yacc, r=[yacc], w=[h_out])
        S.pop()

    def build(self, NL=2, stages=("conv", "mla", "rwkv", "tok", "moe")):
        S = self.S
        self.NL = NL
        self.moe_on = "moe" in stages
        self.ext = {}
        self.consts()
        for nm, shp, dt in (("n1g", [128, 2, 32], F32), ("n2g", [128, 2, 32], F32), ("cw", [128, 2, 4, 31], F32), ("cb", [128, 2, 4, 3], F32),
                            ("mlag", [128, 2, 24], F32), ("ropeT", [64, 2, 4096], F32), ("rotm", [64, 64], F32),
                            ("mixidx", [128, 5, 32], I32), ("wrT", [128, 32, 16], F32), ("rbias", [128, 16], F32),
                            ("rwt", [64, 2, 8, 19], F32), ("rwl", [64, 2, 2, 2, 3], F32), ("rw2", [64, 2, 2, 2, 512], F32),
                            ("rmask", [64, 2, 192], F32), ("rg2", [128, 2, 2, 512], F32), ("rgl", [128, 2, 2, 3], F32)):
            self.ext[nm] = self.ext_in(nm, shp, dt)
        hx = self.ext_in("hx", [MT, D], F32)
        if "gate" in self.dbg:
            self.ext_out("o_gate", [MT, 16], F32)
        self.mod_stage()
        self.convert_win()
        self.convert_mla()
        if "tok" in stages:
            self.convert_big()
        nT_my = S.dram("nT_my", [D, MT], BF16)
        nT_all = S.dram("nT_all", [16, 2, 256, MT], BF16)
        n2T = S.dram("n2T", [D, MT], BF16)
        pT = S.dram("pT", [NCOL, T], F32)
        mixT = S.dram("mixT", [2, 5, 2048, 512], BF16)
        mix_all = S.dram("mix_all", [20, 2, 1024, 512], BF16)
        h_mid = S.dram("h_mid", [MT, D], F32)
        h_nxt = S.dram("h_nxt", [MT, D], F32)
        out = self.ext_out("out", [2048, D], F32) if "tok" in stages else None
        h_cur = hx
        for l in range(NL):
            S.lane = 2 * l
            self.norm_stage(l, h_cur, "n1g", (0, 1), nT_my)
            self.pair_gather(nT_my, nT_all, 16, 256)
            self.inproj_stage(l, nT_all, pT)
            if "conv" in stages:
                self.conv_stage(l, pT, mixT)
            if "rwkv" in stages:
                self.rwkv_stage(l, pT, mixT)
            if "mla" in stages:
                self.mla_stage(l, pT, mixT)
            if "tok" in stages:
                S.lane = 2 * l + 1
                self.pair_gather(mixT, mix_all, 20, 1024, src_ap=mixT.t.rearrange("h b f n -> (h b f) n"))
                self.outproj_stage(l, mix_all, h_cur, h_mid)
                self.norm_stage(l, h_mid, "n2g", (3, 4), n2T)
                self.moe_stage(l, n2T, h_mid, h_nxt, final_out=(out if l == NL - 1 else None))
                h_cur = h_nxt
        if "mixT" in self.dbg:
            o = self.ext_out("o_mixT", [2, 5, 2048, 512], BF16)
            S.dma("sp", o.t, mixT.t, Buf("dd3"), r=[mixT], w=[o])
        if "hmid" in self.dbg:
            o = self.ext_out("o_hmid", [MT, D], F32)
            S.dma("sp", o.t, h_mid.t, Buf("dd4"), r=[h_mid], w=[o])
        if "n2T" in self.dbg:
            o = self.ext_out("o_n2T", [D, MT], BF16)
            S.dma("sp", o.t, n2T.t, Buf("dd5"), r=[n2T], w=[o])
        return S.emit()


OFF_CONV, OFF_MQ, OFF_RR, OFF_RG, OFF_RK, OFF_RV, OFF_RW, OFF_RA, OFF_MKV, OFF_MKR = (
    0, 2048, 3584, 4608, 4768, 5792, 6816, 6944, 7072, 7584)


def my_cols(hf):
    cols = []
    cols += list(range(OFF_CONV + hf * 512, OFF_CONV + hf * 512 + 512))
    cols += list(range(OFF_CONV + 1024 + hf * 512, OFF_CONV + 1024 + hf * 512 + 512))
    cols += list(range(OFF_MQ, OFF_MQ + 1536))
    cols += list(range(OFF_RR + hf * 512, OFF_RR + hf * 512 + 512))
    cols += list(range(OFF_RK + hf * 512, OFF_RK + hf * 512 + 512))
    cols += list(range(OFF_RV + hf * 512, OFF_RV + hf * 512 + 512))
    cols += list(range(OFF_RW, OFF_RW + 128))
    cols += list(range(OFF_RA, OFF_RA + 128))
    cols += list(range(OFF_MKV, OFF_MKV + 512))
    cols += list(range(OFF_RG, OFF_RG + 160))
    cols += list(range(OFF_MKR, OFF_MKR + 64))
    return np.array(cols)


def _rope_tables():
    half = 32
    inv_freq = (10000.0 ** (-np.arange(0, half, 2, dtype=np.float32) / half)).astype(np.float32)
    r = np.repeat(np.arange(64, dtype=np.float32), 64)
    cl = np.tile(np.arange(64, dtype=np.float32), 64)
    ar = r[:, None] * inv_freq
    ac = cl[:, None] * inv_freq
    ang = np.concatenate([ar, ar, ac, ac], -1).astype(np.float32)
    return np.ascontiguousarray(np.stack([np.cos(ang).T, np.sin(ang).T], 1).astype(np.float32))


def _rot_matrix():
    R = np.zeros((64, 64), np.float32)
    for i in range(16):
        R[16 + i, i] = -1.0
        R[i, 16 + i] = 1.0
        R[48 + i, 32 + i] = -1.0
        R[32 + i, 48 + i] = 1.0
    return R


def mix_index_table(hf):
    idx = np.zeros((128, 5, 32), np.int32)
    for blk in range(5):
        for kc in range(32):
            rank, feat0 = kc // 16, (kc % 16) * 128
            R0 = (hf * 5 + blk) * 2048 + feat0
            idx[:, blk, kc] = ((R0 // 1024) * 2 + rank) * 1024 + R0 % 1024 + np.arange(128)
    return idx


MIX_PERM = np.concatenate([np.concatenate([r * 512 + np.arange(512), 1024 + r * 512 + np.arange(512), 2048 + r * 1024 + np.arange(1024)])
                           for r in range(2)])
ROPE_T = _rope_tables()
ROT_M = _rot_matrix()


def pack_inputs(inp, core, NL=2):
    b, hf = core // 2, core % 2
    f = np.float32
    m = {}
    m["hx"] = np.ascontiguousarray(np.concatenate([inp["ctx"][b, hf * 128:(hf + 1) * 128], inp["x"][b, hf * 2048:(hf + 1) * 2048]], 0))
    cv = np.concatenate([inp["c"], inp["c_ctx"][None]], 0)
    m["cvT"] = np.ascontiguousarray(cv.reshape(5, 32, 128).transpose(2, 1, 0))
    m["wmod"] = np.ascontiguousarray(inp["w_mod"][:NL, :, core * 3072:(core + 1) * 3072])
    m["bmodT"] = np.ascontiguousarray(inp["b_mod"][:, core * 3072:(core + 1) * 3072].reshape(2, 24, 128).transpose(2, 0, 1))
    sel = np.zeros((128, 2, 5), f)
    sel[:, 0, b] = 1.0
    sel[:, 1, 4] = 1.0
    m["sel"] = sel
    m["c_ident"] = np.eye(128, dtype=f)
    m["n1g"] = np.ascontiguousarray(inp["norm1_g"].reshape(2, 32, 128).transpose(2, 0, 1))
    m["win"] = np.ascontiguousarray(inp["w_in"][:NL, :, my_cols(hf)])
    ch = hf * 512 + np.arange(512)
    m["cw"] = np.ascontiguousarray(inp["conv_dw"][:, :, ch].reshape(2, 31, 4, 128).transpose(3, 0, 2, 1))
    cb = np.stack([inp["conv_b"][:, ch], inp["conv_ln_g"][:, ch], inp["conv_ln_b"][:, ch]], -1)
    m["cb"] = np.ascontiguousarray(cb.reshape(2, 4, 128, 3).transpose(2, 0, 1, 3))
    mg = np.zeros((128, 2, 24), f)
    mg[:, :, 0:12] = inp["m_cq_g"].reshape(2, 12, 128).transpose(2, 0, 1)
    mg[:, :, 12:16] = inp["m_ckv_g"].reshape(2, 4, 128).transpose(2, 0, 1)
    mg[:, :, 16] = inp["m_qn_g"].T
    mg[:64, :, 17] = inp["m_qr_g"].T
    mg[:, :, 18] = inp["m_kn_g"].T
    mg[:64, :, 19] = inp["m_kr_g"].T
    m["mlag"] = mg
    m["ropeT"] = ROPE_T
    m["rotm"] = ROT_M
    heads = hf * 8 + np.arange(8)
    qc = (heads[:, None] * 192 + np.arange(192)[None]).reshape(-1)
    kc = (heads[:, None] * 256 + np.arange(256)[None]).reshape(-1)
    m["n2g"] = np.ascontiguousarray(inp["norm2_g"].reshape(2, 32, 128).transpose(2, 0, 1))
    chn = hf * 512 + np.arange(512)
    mu = inp["r_mu"]

    def hi(a):
        return a.reshape(2, 8, 64).transpose(2, 0, 1)
    rwt = np.zeros((64, 2, 8, 19), f)
    for qi, off in enumerate((OFF_RR, OFF_RK, OFF_RV)):
        mp = mu[:, 0, off - OFF_RR + chn]
        mn = mu[:, 1, off - OFF_RR + chn]
        rwt[..., qi * 3 + 0] = hi(1.0 - mp - mn)
        rwt[..., qi * 3 + 1] = hi(mp)
        rwt[..., qi * 3 + 2] = hi(mn)
    rwt[..., 9] = hi(inp["r_kk"][:, chn])
    rwt[..., 10] = hi(inp["r_ka"][:, chn])
    rwt[..., 11] = hi(1.0 - inp["r_ka"][:, chn])
    rwt[..., 12] = hi(inp["r_rk"].reshape(2, 1024)[:, chn])
    rwt[..., 13] = hi(inp["r_ln_g"][:, chn])
    rwt[..., 14] = hi(inp["r_ln_b"][:, chn])
    for dd in range(2):
        rwt[..., 15 + dd] = hi(inp["r_w0"][:, dd, chn])
        rwt[..., 17 + dd] = hi(inp["r_a0"][:, dd, chn])
    m["rwt"] = rwt
    rwl = np.zeros((64, 2, 2, 2, 3), f)
    for dd in range(2):
        for qi, off in enumerate((OFF_RW, OFF_RA)):
            cc = off - OFF_RR + dd * 64 + np.arange(64)
            mp, mn = mu[:, 0, cc], mu[:, 1, cc]
            rwl[:, :, dd, qi, 0] = (1.0 - mp - mn).T
            rwl[:, :, dd, qi, 1] = mp.T
            rwl[:, :, dd, qi, 2] = mn.T
    m["rwl"] = rwl
    rw2 = np.zeros((64, 2, 2, 2, 512), f)
    rw2[:, :, :, 0, :] = inp["r_w2"][:, :, :, chn].transpose(2, 0, 1, 3)
    rw2[:, :, :, 1, :] = inp["r_a2"][:, :, :, chn].transpose(2, 0, 1, 3)
    m["rw2"] = rw2
    tri = np.arange(64)
    up_s = (tri[:, None] < tri[None, :]).astype(f)
    up_i = (tri[:, None] <= tri[None, :]).astype(f)
    rmask = np.zeros((64, 2, 192), f)
    rmask[:, 0, 0:64], rmask[:, 0, 64:128], rmask[:, 0, 128:192] = up_s, up_i, up_s.T
    rmask[:, 1, 0:64], rmask[:, 1, 64:128], rmask[:, 1, 128:192] = up_s.T, up_i.T, up_s
    m["rmask"] = rmask
    rg2 = np.zeros((128, 2, 2, 512), f)
    rg2[:, :, 0, :] = inp["r_g2"][:, 0:128, :][:, :, chn].transpose(1, 0, 2)
    rg2[0:32, :, 1, :] = inp["r_g2"][:, 128:160, :][:, :, chn].transpose(1, 0, 2)
    m["rg2"] = rg2
    rgl = np.zeros((128, 2, 2, 3), f)
    cg = OFF_RG - OFF_RR + np.arange(160)
    mp, mn = mu[:, 0, cg], mu[:, 1, cg]
    for qi, (a_, b_, n_) in enumerate(((0, 128, 128), (128, 160, 32))):
        rgl[0:n_, :, qi, 0] = (1.0 - mp[:, a_:b_] - mn[:, a_:b_]).T
        rgl[0:n_, :, qi, 1] = mp[:, a_:b_].T
        rgl[0:n_, :, qi, 2] = mn[:, a_:b_].T
    m["rgl"] = rgl
    m["mixidx"] = mix_index_table(hf)
    m["wrT"] = np.ascontiguousarray(inp["w_router"].reshape(32, 128, 16).transpose(1, 0, 2))
    m["rbias"] = np.ascontiguousarray(np.broadcast_to(inp["router_bias"][None, :], (128, 16))).astype(f)
    if "w_out" in inp:
        wo = inp["w_out"][:NL][:, MIX_PERM, :].reshape(NL * 4096, 4096)
        m["wo"] = np.ascontiguousarray(wo[core * 512 * NL:(core + 1) * 512 * NL])
    if "moe_w1" in inp:
        nle = 2 * NL
        for nm in ("w1", "w3", "w2"):
            w = inp["moe_" + nm][:NL]
            w = w.reshape((NL * 16,) + w.shape[2:])
            m["m" + nm] = np.ascontiguousarray(w[core * nle:(core + 1) * nle])
    m["wuq"] = np.ascontiguousarray(inp["m_w_uq"][:NL, :, qc])
    m["wukv"] = np.ascontiguousarray(inp["m_w_ukv"][:NL, :, kc])
    return m


def kernel(**inputs):
    inp = {k: np.asarray(v) for k, v in inputs.items()}
    nc = bass.Bass("TRN2", target_bir_lowering=False)
    k = K(nc)
    k.build(NL=2)
    need = None
    maps = []
    for c in range(8):
        m = pack_inputs(inp, c, 2)
        maps.append(m)
    res = run_bass_kernel_spmd(nc, maps, core_ids=list(range(8)))
    out = np.zeros((4, 4096, 4096), np.float32)
    for c in range(8):
        b, hf = c // 2, c % 2
        out[b, hf * 2048:(hf + 1) * 2048] = res.results[c]["out"]
    return out
```

```python
import contextlib
import numpy as np
import ml_dtypes
import concourse.bass as bass
import concourse.mybir as mybir
from concourse.bass_utils import run_bass_kernel_spmd

F32 = mybir.dt.float32
BF16 = mybir.dt.bfloat16
I32 = mybir.dt.int32
AF = mybir.ActivationFunctionType
ALU = mybir.AluOpType
AX = mybir.AxisListType

ENGS = ("pe", "act", "dve", "pool", "sp")

D = 4096
T = 4352
MT = 2176
LC = 256
LL = 4096
NCOL = 5088
COLS = {}
_off = 0
for _n, _s in (("ca", 512), ("cg", 512), ("mq", 1536), ("rr", 512), ("rk", 512), ("rv", 512), ("rw", 128),
               ("ra", 128), ("mkv", 512), ("rg", 160), ("mkr", 64)):
    COLS[_n] = (_off, _s)
    _off += _s
assert _off == NCOL
CHUNKS = [(i * 128, 128) for i in range(39)] + [(4992, 32), (5024, 64)]
PAIRS = [[0, 1], [2, 3], [4, 5], [6, 7]]
QUADS = [[0, 1, 2, 3], [4, 5, 6, 7]]
P4 = [[0, 4], [1, 5], [2, 6], [3, 7]]


import os
RW_LEVEL = int(os.environ.get("RW_LEVEL", "9"))
RW_NBLK = int(os.environ.get("RW_NBLK", "99"))
RW_BAR = int(os.environ.get("RW_BAR", "0"))
RW_NFIN = int(os.environ.get("RW_NFIN", "99"))
RW_NLEV = int(os.environ.get("RW_NLEV", "6"))


class Buf:
    __slots__ = ("name", "lw", "rd", "dsem", "dcount", "t", "persist")

    def __init__(self, name, t=None):
        self.name = name
        self.t = t
        self.lw = []
        self.rd = []
        self.dsem = None
        self.dcount = 0
        self.persist = False

    def __getitem__(self, k):
        return self.t[k]


class Sched:
    def __init__(self, nc):
        self.nc = nc
        self.ops = {e: [] for e in ENGS}
        self.ndsem = 0
        self.stacks = [contextlib.ExitStack()]
        self.n_tiles = 0
        self.dbufs = []
        self.free_sems = []
        self.scope_sems = [[]]
        self.lane = 0

    def sb(self, shape, dtype, name="t"):
        self.n_tiles += 1
        t = self.stacks[-1].enter_context(self.nc.sbuf_tensor(f"{name}_{self.n_tiles}", list(shape), dtype))
        return Buf(name, t)

    def ps(self, shape, dtype, name="p"):
        self.n_tiles += 1
        t = self.stacks[-1].enter_context(self.nc.psum_tensor(f"{name}_{self.n_tiles}", list(shape), dtype))
        return Buf(name, t)

    def dram(self, name, shape, dtype, kind="Internal"):
        t = self.nc.dram_tensor(name, list(shape), dtype, kind=kind).ap()
        return Buf(name, t)

    def push(self):
        self.stacks.append(contextlib.ExitStack())
        self.scope_sems.append([])

    def pop(self):
        self.barrier()
        self.stacks.pop().close()
        for b in self.scope_sems.pop():
            if b.dsem is not None:
                self.free_sems.append((b.dsem, b.dcount))
                self.dbufs.remove(b)
                b.dsem = None

    def _events_for(self, r, w):
        ev = []
        for b in r:
            ev.extend(b.lw)
        for b in w:
            ev.extend(b.lw)
            ev.extend(b.rd)
        return ev

    def op(self, eng, fn, r=(), w=()):
        idx = len(self.ops[eng])
        ev = self._events_for(r, w)
        self.ops[eng].append({"fn": fn, "ev": ev, "dma": None, "lane": self.lane})
        me = ("e", eng, idx)
        for b in r:
            b.rd = [e for e in b.rd if not (e[0] == "e" and e[1] == eng)] + [me]
        for b in w:
            b.lw = [me]
            b.rd = []
        return idx

    def i(self, eng, meth, *a, r=(), w=(), **kw):
        return self.op(eng, lambda e: getattr(e, meth)(*a, **kw), r=r, w=w)

    def _async(self, eng, fn, sem_buf, inc, r, w):
        idx = len(self.ops[eng])
        ev = self._events_for(r, w)
        b = sem_buf
        if b.dsem is None:
            if self.free_sems and self.ndsem >= 72:
                j = min(range(len(self.free_sems)), key=lambda q: self.free_sems[q][1])
                b.dsem, b.dcount = self.free_sems.pop(j)
            else:
                b.dsem = self.ndsem
                self.ndsem += 1
                b.dcount = 0
            self.dbufs.append(b)
            self.scope_sems[0 if b.persist else -1].append(b)
        b.dcount += inc
        me = ("d", b.dsem, b.dcount)
        self.ops[eng].append({"fn": fn, "ev": ev, "dma": b.dsem, "inc": inc, "lane": self.lane})
        for x in r:
            x.rd = [e for e in x.rd if not (e[0] == "d" and e[1] == b.dsem)] + [me]
        for x in w:
            x.lw = [me]
            x.rd = []
        return idx

    def dma(self, eng, out, in_, sem_buf, r=(), w=(), **kw):
        return self._async(eng, lambda e: e.dma_start(out=out, in_=in_, **kw), sem_buf, 16, r, w)

    def gather_rows(self, out, in_, idx_ap, sem_buf, r=(), w=()):
        return self._async("pool", lambda e: e.indirect_dma_start(
            out=out, out_offset=None, in_=in_, in_offset=bass.IndirectOffsetOnAxis(ap=idx_ap, axis=0)),
            sem_buf, 16, r, w)

    def coll(self, kind, ins_ap, outs_ap, groups, sem_buf, r=(), w=()):
        return self._async("pool", lambda e: e.collective_compute(
            kind, ALU.bypass, replica_groups=groups, ins=[ins_ap], outs=[outs_ap]), sem_buf, 1, r, w)

    def barrier(self, final=False):
        ev = []
        for e in ENGS:
            for i in range(len(self.ops[e]) - 1, -1, -1):
                o = self.ops[e][i]
                if o["fn"] is not None and o["dma"] is None:
                    ev.append(("e", e, i))
                    break
        for b in self.dbufs:
            if b.persist and not final:
                continue
            ev.append(("d", b.dsem, b.dcount))
        for e in ENGS:
            self.ops[e].append({"fn": None, "ev": list(ev), "dma": None, "lane": self.lane})

    def emit(self):
        nc = self.nc
        self.barrier(final=True)
        NLANE = 4
        marked = {e: set() for e in ENGS}
        lane_of = {e: [o["lane"] % NLANE for o in self.ops[e]] for e in ENGS}
        for eng in ENGS:
            seen_e = {}
            seen_d = {}
            for i, o in enumerate(self.ops[eng]):
                need_e = {}
                need_d = {}
                for ev in o["ev"]:
                    if ev[0] == "e":
                        _, e2, i2 = ev
                        if e2 == eng and eng == "pe":
                            continue
                        key = (e2, lane_of[e2][i2])
                        if seen_e.get(key, -1) >= i2:
                            continue
                        need_e[key] = max(need_e.get(key, -1), i2)
                    else:
                        _, s, c = ev
                        if seen_d.get(s, 0) >= c:
                            continue
                        need_d[s] = max(need_d.get(s, 0), c)
                for key, i2 in need_e.items():
                    seen_e[key] = i2
                    marked[key[0]].add(i2)
                for s, c in need_d.items():
                    seen_d[s] = c
                o["need_e"] = need_e
                o["need_d"] = need_d
        cum = {}
        for eng in ENGS:
            c = [0] * NLANE
            arr = []
            for i in range(len(self.ops[eng])):
                ln = lane_of[eng][i]
                if i in marked[eng]:
                    c[ln] += 1
                arr.append(c[ln])
            cum[eng] = arr
        with contextlib.ExitStack() as st:
            esem = {(e, ln): st.enter_context(nc.semaphore(f"es_{e}{ln}")) for e in ENGS for ln in range(NLANE)}
            dsem = [st.enter_context(nc.semaphore(f"ds_{i}")) for i in range(self.ndsem)]
            block = st.enter_context(nc.Block())

            def body(eng):
                def run(engine):
                    for i, o in enumerate(self.ops[eng]):
                        for (e2, ln), i2 in o["need_e"].items():
                            engine.wait_ge(esem[(e2, ln)], cum[e2][i2])
                        for s, c in o["need_d"].items():
                            engine.wait_ge(dsem[s], c)
                        if o["fn"] is None:
                            continue
                        ins = o["fn"](engine)
                        if o["dma"] is not None:
                            ins.then_inc(dsem[o["dma"]], o["inc"])
                        elif i in marked[eng]:
                            ins.then_inc(esem[(eng, lane_of[eng][i])], 1)
                return run

            block.tensor(body("pe"))
            block.scalar(body("act"))
            block.vector(body("dve"))
            block.gpsimd(body("pool"))
            block.sync(body("sp"))
        while self.stacks:
            self.stacks.pop().close()
        return {e: len(self.ops[e]) for e in ENGS}


class _View:
    def __init__(self, buf, ap):
        self.buf = buf
        self.ap = ap


def xin_view(xin, n):
    return xin


class RR:
    def __init__(self, items):
        self.items = list(items)
        self.i = 0

    def __call__(self):
        x = self.items[self.i % len(self.items)]
        self.i += 1
        return x


def copy_on(S, eng, out_ap, in_ap, r, w):
    if eng == "act":
        S.op("act", lambda e: e.copy(out=out_ap, in_=in_ap), r=r, w=w)
    else:
        S.op(eng, lambda e: e.tensor_copy(out=out_ap, in_=in_ap), r=r, w=w)


class K:
    def __init__(self, nc, dbg=()):
        self.nc = nc
        self.S = Sched(nc)
        self.dbg = set(dbg)
        self.outs = {}
        self.pending_g = []

    def ext_in(self, name, shape, dtype):
        return Buf(name, self.nc.dram_tensor(name, list(shape), dtype, kind="ExternalInput").ap())

    def ext_out(self, name, shape, dtype):
        b = Buf(name, self.nc.dram_tensor(name, list(shape), dtype, kind="ExternalOutput").ap())
        self.outs[name] = b
        return b

    def consts(self):
        S = self.S
        self.ident = S.sb([128, 128], BF16, "ident")
        identf = S.sb([128, 128], F32, "identf")
        self.ones_bf = S.sb([128, 128], BF16, "ones")
        cid = self.ext_in("c_ident", [128, 128], F32)
        S.dma("sp", identf[:], cid.t, identf, w=[identf])
        S.op("dve", lambda e: e.tensor_copy(out=self.ident[:], in_=identf[:]), r=[identf], w=[self.ident])
        S.op("dve", lambda e: e.memset(self.ones_bf[:], 1.0), w=[self.ones_bf])
        self.identf = identf

    def gather8(self, src, rows, cols, dtype, name):
        S = self.S
        mid = S.dram(name + "_q", [4 * rows, cols], dtype)
        dst = S.dram(name + "_g", [8 * rows, cols], dtype)
        sem = Buf(name + "_sem")
        S.coll("AllGather", src.t, mid.t, QUADS, sem, r=[src], w=[mid])
        S.coll("AllGather", mid.t, dst.t, P4, sem, r=[mid], w=[dst])
        return dst

    def pair_gather(self, src, dst, nch, rows, src_ap=None):
        sem = Buf(src.name + "_pgsem")
        ap = src.t if src_ap is None else src_ap
        for i in range(nch):
            self.S.coll("AllGather", ap[i * rows:(i + 1) * rows, :], dst.t[i].rearrange("r p n -> (r p) n"), PAIRS, sem,
                        r=[src], w=[dst])

    def mod_stage(self):
        S = self.S
        cvT = self.ext_in("cvT", [128, 32, 5], F32)
        wmod = self.ext_in("wmod", [self.NL, D, 3072], F32)
        bmodT = self.ext_in("bmodT", [128, 2, 24], F32)
        sel = self.ext_in("sel", [128, 2, 5], F32)
        S.push()
        cv = S.sb([128, 32, 5], F32, "cv")
        cvb = S.sb([128, 32, 5], BF16, "cvb")
        bm = S.sb([128, 2, 24], F32, "bm")
        part = S.sb([128, 2, 24, 5], F32, "modpart")
        S.dma("sp", cv[:], cvT.t, cv, w=[cv])
        S.dma("sp", bm[:], bmodT.t, bm, w=[bm])
        S.op("act", lambda e: e.activation(out=cvb[:], in_=cv[:], func=AF.Silu), r=[cv], w=[cvb])
        wf = [S.sb([128, 32, 128], F32, "wf") for _ in range(2)]
        wb = [S.sb([128, 32, 128], BF16, "wb") for _ in range(2)]
        pp = [S.ps([128, 8], F32, "modps") for _ in range(2)]
        cv_eng = RR(["dve", "pool", "act"])
        it = 0
        S.op("dve", lambda e: e.memset(part[:], 0.0), w=[part])
        for l in range(self.NL):
            for lc in range(24):
                f = wf[it % 2]
                b = wb[it % 2]
                p = pp[it % 2]
                src = wmod.t[l, :, lc * 128:(lc + 1) * 128].rearrange("(c p) n -> p c n", p=128)
                S.dma("sp" if it % 2 == 0 else "act", f[:], src, f, w=[f])
                for q in range(4):
                    copy_on(S, cv_eng(), b[:, q * 8:(q + 1) * 8, :], f[:, q * 8:(q + 1) * 8, :], [f], [b])
                for c in range(32):
                    S.op("pe", lambda e, p=p, b=b, c=c: e.matmul(p[:, 0:5], lhsT=b[:, c, :], rhs=cvb[:, c, :],
                                                               start=(c == 0), stop=(c == 31)), r=[b, cvb], w=[p])
                S.op("dve", lambda e, p=p, l=l, lc=lc: e.tensor_scalar(
                    out=part[:, l, lc, :], in0=p[:, 0:5], scalar1=bm[:, l, lc:lc + 1], scalar2=None, op0=ALU.add),
                    r=[p, bm], w=[part])
                it += 1
        msrc = S.dram("modsrc", [128, 240], F32)
        S.dma("pool", msrc.t, part[:].rearrange("p l c v -> p (l c v)"), part, r=[part], w=[msrc])
        mall = self.gather8(msrc, 128, 240, F32, "modg")
        S.pop()
        tab = S.sb([128, 8, 2, 24, 5], F32, "modtab")
        selt = S.sb([128, 2, 5], F32, "selt")
        S.dma("sp", tab[:].rearrange("p r l c v -> p r (l c v)"), mall.t.rearrange("(r p) n -> p r n", p=128), tab,
              r=[mall], w=[tab])
        S.dma("sp", selt[:], sel.t, selt, w=[selt])
        self.mods = [S.sb([128, 2, 192], F32, f"modsel{w_}") for w_ in range(2)]
        S.push()
        tmp = S.sb([128, 8 * 2 * 24, 5], F32, "modtmp")
        for wsel in range(2):
            m = self.mods[wsel]
            S.op("dve", lambda e, wsel=wsel: e.tensor_tensor(
                out=tmp[:], in0=tab[:].rearrange("p r l c v -> p (r l c) v"),
                in1=selt[:, wsel:wsel + 1, :].to_broadcast([128, 384, 5]), op=ALU.mult), r=[tab, selt], w=[tmp])
            S.op("dve", lambda e, m=m: e.tensor_reduce(
                out=m[:].rearrange("p l (r c) -> p r l c", r=8),
                in_=tmp[:].rearrange("p (r l c) v -> p r l c v", r=8, l=2), axis=AX.X, op=ALU.add), r=[tmp], w=[m])
        S.pop()

    def modcol(self, which, l, k, c):
        return self.mods[which][:, l, k * 32 + c:k * 32 + c + 1]

    def convert_win(self):
        S = self.S
        self.win = self.ext_in("win", [self.NL, D, NCOL], F32)
        self.winb = S.dram("winb", [self.NL, 41, 128, 32, 128], BF16)
        S.push()
        wf = [S.sb([128, NCOL], F32, "cwf") for _ in range(2)]
        wb = [S.sb([128, NCOL], BF16, "cwb") for _ in range(2)]
        eng = RR(["dve", "pool", "act"])
        it = 0
        for l in range(self.NL):
            for c in range(32):
                f, b = wf[it % 2], wb[it % 2]
                S.dma("sp", f[:], self.win.t[l, c * 128:(c + 1) * 128, :], f, w=[f])
                for q in range(6):
                    copy_on(S, eng(), b[:, q * 848:(q + 1) * 848], f[:, q * 848:(q + 1) * 848], [f], [b])
                S.dma("pool", self.winb.t[l, 0:39, :, c, :].rearrange("j p n -> p j n"),
                      b[:, 0:4992].rearrange("p (j n) -> p j n", n=128), b, r=[b], w=[self.winb])
                S.dma("pool", self.winb.t[l, 39, :, c, 0:32], b[:, 4992:5024], b, r=[b], w=[self.winb])
                S.dma("pool", self.winb.t[l, 40, :, c, 0:64], b[:, 5024:5088], b, r=[b], w=[self.winb])
                it += 1
        S.pop()

    def norm_stage(self, l, h_src, gname, ktypes, out_nT):
        S = self.S
        k_sh, k_sc = ktypes
        S.push()
        g = S.sb([128, 2, 32], F32, "ng")
        S.dma("sp", g[:], self.ext[gname].t, g, w=[g])
        gm = [S.sb([128, 32], F32, "gm") for _ in range(2)]
        for w_ in range(2):
            S.op("dve", lambda e, w_=w_: e.scalar_tensor_tensor(
                out=gm[w_][:], in0=self.mods[w_][:, l, k_sc * 32:(k_sc + 1) * 32], scalar=1.0, in1=g[:, l, :],
                op0=ALU.add, op1=ALU.mult), r=[self.mods[w_], g], w=[gm[w_]])
        hb = [S.sb([128, D], F32, "hb") for _ in range(2)]
        junk = S.sb([128, D], BF16, "junk")
        hs = [S.sb([128, D], BF16, "hs") for _ in range(2)]
        st = [S.sb([128, 4], F32, "nst") for _ in range(2)]
        tp = [S.ps([128, 1024], BF16, "ntp") for _ in range(4)]
        nblk = [S.sb([128, 32, 512], BF16, "nblk") for _ in range(2)]
        ev = RR(["dve", "act"])
        tiles = [(0, 1, 0)] + [(128 + 512 * q, 4, 0) for q in range(4)]
        tiles[0] = (0, 1, 1)
        tcount = 0
        for bi, (t0, nt, which) in enumerate(tiles):
            nb = nblk[bi % 2]
            for ti in range(nt):
                h = hb[tcount % 2]
                hsb = hs[tcount % 2]
                s_ = st[tcount % 2]
                r0 = t0 + ti * 128
                S.dma("sp", h[:], h_src.t[r0:r0 + 128, :], h, r=[h_src], w=[h])
                S.op("act", lambda e, h=h, s_=s_: e.activation(out=junk[:], in_=h[:], func=AF.Square, accum_out=s_[:, 0:1]),
                     r=[h], w=[junk, s_])
                S.op("dve", lambda e, s_=s_: e.tensor_scalar(out=s_[:, 1:2], in0=s_[:, 0:1], scalar1=1.0 / D, scalar2=1e-6,
                                                            op0=ALU.mult, op1=ALU.add), r=[s_], w=[s_])
                S.op("act", lambda e, s_=s_: e.activation(out=s_[:, 2:3], in_=s_[:, 1:2], func=AF.Sqrt), r=[s_], w=[s_])
                S.op("dve", lambda e, s_=s_: e.reciprocal(out=s_[:, 3:4], in_=s_[:, 2:3]), r=[s_], w=[s_])
                S.op("act", lambda e, h=h, hsb=hsb, s_=s_: e.activation(out=hsb[:], in_=h[:], func=AF.Identity, scale=s_[:, 3:4]),
                     r=[h, s_], w=[hsb])
                for q in range(4):
                    p = tp[q]
                    for j in range(8):
                        c = q * 8 + j
                        S.op("pe", lambda e, p=p, hsb=hsb, c=c, j=j: e.transpose(
                            out=p[:, j * 128:(j + 1) * 128], in_=hsb[:, c * 128:(c + 1) * 128], identity=self.ident[:]),
                            r=[hsb, self.ident], w=[p])
                    for j in range(8):
                        c = q * 8 + j
                        en = ev()
                        o_ap = nb[:, c, ti * 128:(ti + 1) * 128]
                        i_ap = p[:, j * 128:(j + 1) * 128]
                        sc_ap = gm[which][:, c:c + 1]
                        sh_ap = self.modcol(which, l, k_sh, c)
                        if en == "dve":
                            S.op("dve", lambda e, o_ap=o_ap, i_ap=i_ap, sc_ap=sc_ap, sh_ap=sh_ap: e.tensor_scalar(
                                out=o_ap, in0=i_ap, scalar1=sc_ap, scalar2=sh_ap, op0=ALU.mult, op1=ALU.add),
                                r=[p, gm[which], self.mods[which]], w=[nb])
                        else:
                            S.op("act", lambda e, o_ap=o_ap, i_ap=i_ap, sc_ap=sc_ap, sh_ap=sh_ap: e.activation(
                                out=o_ap, in_=i_ap, func=AF.Identity, scale=sc_ap, bias=sh_ap),
                                r=[p, gm[which], self.mods[which]], w=[nb])
                tcount += 1
            n = nt * 128
            S.dma("pool", out_nT.t[:, t0:t0 + n].rearrange("(c p) n -> p c n", p=128), nb[:, :, 0:n], nb,
                  r=[nb], w=[out_nT])
        S.pop()

    def inproj_stage(self, l, nT_all, pT):
        S = self.S
        S.push()
        nt = S.sb([128, 32, 1024], BF16, "pnT")
        wr = [S.sb([128, 32, 128], BF16, "pw") for _ in range(3)]
        pp = [S.ps([128, 512], F32, "pps") for _ in range(4)]
        ob = [S.sb([128, 512], F32, "pob") for _ in range(4)]
        ev = RR(["act", "dve"])
        sbs = [(0, [(0, 0, 128), (1, 0, 128)])]
        for q in range(4):
            sbs.append((256 + q * 1024, [(q // 2, 128 + (q % 2) * 1024, 1024)]))
        wi = 0
        oi = 0
        for s0, parts in sbs:
            off = 0
            for (rk, o, n) in parts:
                for ci in range(2):
                    S.dma("sp", nt[:, :, off:off + n].rearrange("p (ch ci) n -> p ch ci n", ci=2)[:, :, ci, :],
                          nT_all.t[:, rk, ci * 128:(ci + 1) * 128, o:o + n].rearrange("ch p n -> p ch n"), nt,
                          r=[nT_all], w=[nt])
                off += n
            ntok = off
            for j, (c0, m) in enumerate(CHUNKS):
                w = wr[wi % 3]
                wi += 1
                S.dma("act" if wi % 2 else "sp", w[:, :, 0:m], self.winb.t[l, j, :, :, 0:m], w, r=[self.winb], w=[w])
                for hf in range((ntok + 511) // 512):
                    nn = min(512, ntok - hf * 512)
                    p = pp[oi % 4]
                    o_ = ob[oi % 4]
                    oi += 1
                    for c in range(32):
                        S.op("pe", lambda e, p=p, w=w, c=c, m=m, hf=hf, nn=nn: e.matmul(
                            p[0:m, 0:nn], lhsT=w[:, c, 0:m], rhs=nt[:, c, hf * 512:hf * 512 + nn],
                            start=(c == 0), stop=(c == 31)), r=[w, nt], w=[p])
                    copy_on(S, ev(), o_[0:m, 0:nn], p[0:m, 0:nn], [p], [o_])
                    S.dma("pool", pT.t[c0:c0 + m, s0 + hf * 512:s0 + hf * 512 + nn], o_[0:m, 0:nn], o_, r=[o_], w=[pT])
        S.pop()


    def mix_dst(self, mixT, row0, nrows, s0, n):
        out = []
        segs = [(0, 128, 0, 0), (128, 256, 1, 0)]
        for half in range(2):
            for bq in range(4):
                a0 = 256 + half * 2048 + bq * 512
                segs.append((a0, a0 + 512, half, 1 + bq))
        for (a, b, half, blk) in segs:
            lo, hi = max(a, s0), min(b, s0 + n)
            if lo < hi:
                out.append((mixT.t[half, blk, row0:row0 + nrows, lo - a:hi - a], lo - s0, hi - lo))
        return out

    def bcast_row(self, ps_ap, row_hi, row_lo, n, r):
        S = self.S
        S.op("pe", lambda e: e.matmul(ps_ap, lhsT=self.ones_bf[0:1, :], rhs=row_hi, start=True, stop=False), r=r + [self.ones_bf], w=[])
        return None

    def conv_stage(self, l, pT, mixT):
        S = self.S
        NP = 4397
        NA = 4367
        S.push()
        cw = S.sb([128, 2, 4, 31], F32, "cw")
        cb = S.sb([128, 2, 4, 3], F32, "cb")
        S.dma("sp", cw[:], self.ext["cw"].t, cw, w=[cw])
        S.dma("sp", cb[:], self.ext["cb"].t, cb, w=[cb])
        acc = S.sb([128, 4, NA], F32, "cacc")
        xb = S.sb([128, 2, 512], BF16, "cxb")
        ca0, cg0 = COLS["ca"][0], COLS["cg"][0]
        S.push()
        hp = S.sb([128, NP], F32, "chp")
        gt = S.sb([128, T], F32, "cgt")
        S.op("pool", lambda e: e.memset(hp[:], 0.0), w=[hp])
        for cc in range(4):
            S.dma("sp", gt[:], pT.t[cg0 + cc * 128:cg0 + (cc + 1) * 128, :], gt, r=[pT], w=[gt])
            S.op("act", lambda e: e.activation(out=gt[:], in_=gt[:], func=AF.Sigmoid), r=[gt], w=[gt])
            S.dma("act", hp[:, 15:271], pT.t[ca0 + cc * 128:ca0 + (cc + 1) * 128, 0:256], hp, r=[pT], w=[hp])
            S.dma("act", hp[:, 286:4382], pT.t[ca0 + cc * 128:ca0 + (cc + 1) * 128, 256:T], hp, r=[pT], w=[hp])
            S.op("dve", lambda e: e.tensor_tensor(out=hp[:, 15:271], in0=hp[:, 15:271], in1=gt[:, 0:256], op=ALU.mult), r=[hp, gt], w=[hp])
            S.op("dve", lambda e: e.tensor_tensor(out=hp[:, 286:4382], in0=hp[:, 286:4382], in1=gt[:, 256:T], op=ALU.mult), r=[hp, gt], w=[hp])
            S.op("dve", lambda e, cc=cc: e.tensor_scalar(out=acc[:, cc, :], in0=hp[:, 0:NA], scalar1=cw[:, l, cc, 0:1],
                                                          scalar2=cb[:, l, cc, 0:1], op0=ALU.mult, op1=ALU.add), r=[hp, cw, cb], w=[acc])
            for j in range(1, 31):
                S.op("dve", lambda e, cc=cc, j=j: e.scalar_tensor_tensor(
                    out=acc[:, cc, :], in0=hp[:, j:j + NA], scalar=cw[:, l, cc, j:j + 1], in1=acc[:, cc, :],
                    op0=ALU.mult, op1=ALU.add), r=[hp, cw, acc], w=[acc])
        S.pop()
        strow = [S.sb([1, 2, 512], F32, "cstrow") for _ in range(2)]
        pst = [S.ps([128, 512], F32, "cps") for _ in range(4)]
        nblk = (NA + 511) // 512
        cst = S.dram(f"cst{l}", [2, NA], F32)
        cstg = S.dram(f"cstg{l}", [4, NA], F32)
        for bi in range(nblk):
            c0 = bi * 512
            n = min(512, NA - c0)
            p1, p2 = pst[(bi % 2) * 2], pst[(bi % 2) * 2 + 1]
            sr = strow[bi % 2]
            for cc in range(4):
                S.op("act", lambda e, cc=cc, c0=c0, n=n: e.copy(out=xb[:, 0, 0:n], in_=acc[:, cc, c0:c0 + n]), r=[acc], w=[xb])
                S.op("act", lambda e, cc=cc, c0=c0, n=n: e.activation(out=xb[:, 1, 0:n], in_=acc[:, cc, c0:c0 + n], func=AF.Square), r=[acc], w=[xb])
                S.op("pe", lambda e, p1=p1, n=n, cc=cc: e.matmul(p1[0:1, 0:n], lhsT=self.ones_bf[:, 0:1], rhs=xb[:, 0, 0:n],
                                                                 start=(cc == 0), stop=(cc == 3)), r=[xb, self.ones_bf], w=[p1])
                S.op("pe", lambda e, p2=p2, n=n, cc=cc: e.matmul(p2[0:1, 0:n], lhsT=self.ones_bf[:, 0:1], rhs=xb[:, 1, 0:n],
                                                                 start=(cc == 0), stop=(cc == 3)), r=[xb, self.ones_bf], w=[p2])
            S.op("dve", lambda e, p1=p1, sr=sr, n=n: e.tensor_copy(out=sr[0:1, 0, 0:n], in_=p1[0:1, 0:n]), r=[p1], w=[sr])
            S.op("dve", lambda e, p2=p2, sr=sr, n=n: e.tensor_copy(out=sr[0:1, 1, 0:n], in_=p2[0:1, 0:n]), r=[p2], w=[sr])
            S.dma("sp", cst.t[:, c0:c0 + n].rearrange("(o a) n -> o a n", o=1), sr[0:1, :, 0:n], sr, r=[sr], w=[cst])
        S.coll("AllGather", cst.t, cstg.t, PAIRS, Buf("cstsem"), r=[cst], w=[cstg])
        both = [S.sb([1, 4, 512], F32, "cboth") for _ in range(2)]
        mrs = [S.sb([1, 4, 512], F32, "cmr") for _ in range(2)]
        hls = [S.sb([1, 2, 2, 512], BF16, "chl") for _ in range(2)]
        ot = [S.sb([128, 512], F32, "cot") for _ in range(2)]
        ob = [S.sb([128, 4, 512], BF16, "cob") for _ in range(2)]
        for bi in range(nblk):
            c0 = bi * 512
            n = min(512, NA - c0)
            p1, p2 = pst[(bi % 2) * 2], pst[(bi % 2) * 2 + 1]
            bo, mr, hl = both[bi % 2], mrs[bi % 2], hls[bi % 2]
            S.dma("sp", bo[0:1, :, 0:n], cstg.t[:, c0:c0 + n].rearrange("(o a) n -> o a n", o=1), bo, r=[cstg], w=[bo])
            S.op("dve", lambda e, bo=bo, mr=mr, n=n: e.tensor_tensor(out=mr[0:1, 0:2, 0:n], in0=bo[0:1, 0:2, 0:n], in1=bo[0:1, 2:4, 0:n], op=ALU.add), r=[bo], w=[mr])
            S.op("dve", lambda e, mr=mr, n=n: e.tensor_scalar(out=mr[0:1, 0:2, 0:n], in0=mr[0:1, 0:2, 0:n], scalar1=1.0 / 1024, scalar2=None, op0=ALU.mult), r=[mr], w=[mr])
            S.op("dve", lambda e, mr=mr, n=n: e.tensor_tensor(out=mr[0:1, 3, 0:n], in0=mr[0:1, 0, 0:n], in1=mr[0:1, 0, 0:n], op=ALU.mult), r=[mr], w=[mr])
            S.op("dve", lambda e, mr=mr, n=n: e.tensor_tensor(out=mr[0:1, 1, 0:n], in0=mr[0:1, 1, 0:n], in1=mr[0:1, 3, 0:n], op=ALU.subtract), r=[mr], w=[mr])
            S.op("dve", lambda e, mr=mr, n=n: e.tensor_scalar(out=mr[0:1, 1, 0:n], in0=mr[0:1, 1, 0:n], scalar1=1e-5, scalar2=None, op0=ALU.add), r=[mr], w=[mr])
            S.op("act", lambda e, mr=mr, n=n: e.activation(out=mr[0:1, 1, 0:n], in_=mr[0:1, 1, 0:n], func=AF.Sqrt), r=[mr], w=[mr])
            S.op("dve", lambda e, mr=mr, n=n: e.reciprocal(out=mr[0:1, 1, 0:n], in_=mr[0:1, 1, 0:n]), r=[mr], w=[mr])
            S.op("dve", lambda e, mr=mr, n=n: e.scalar_tensor_tensor(out=mr[0:1, 2, 0:n], in0=mr[0:1, 0, 0:n], scalar=-1.0, in1=mr[0:1, 1, 0:n],
                                                                     op0=ALU.mult, op1=ALU.mult), r=[mr], w=[mr])
            S.op("dve", lambda e, mr=mr, hl=hl, n=n: e.tensor_copy(out=hl[0:1, 0, :, 0:n], in_=mr[0:1, 1:3, 0:n]), r=[mr], w=[hl])
            S.op("dve", lambda e, mr=mr, hl=hl, n=n: e.tensor_tensor(out=mr[0:1, 1:3, 0:n], in0=mr[0:1, 1:3, 0:n], in1=hl[0:1, 0, :, 0:n], op=ALU.subtract), r=[mr, hl], w=[mr])
            S.op("dve", lambda e, mr=mr, hl=hl, n=n: e.tensor_copy(out=hl[0:1, 1, :, 0:n], in_=mr[0:1, 1:3, 0:n]), r=[mr], w=[hl])
            for which, p in ((0, p1), (1, p2)):
                for hq in range(2):
                    S.op("pe", lambda e, p=p, which=which, hq=hq, hl=hl, n=n: e.matmul(
                        p[:, 0:n], lhsT=self.ones_bf[0:1, :], rhs=hl[0:1, hq, which, 0:n], start=(hq == 0), stop=(hq == 1)),
                        r=[hl, self.ones_bf], w=[p])
            o_b = ob[bi % 2]
            for cc in range(4):
                o_ = ot[cc % 2]
                S.op("dve", lambda e, o_=o_, cc=cc, c0=c0, n=n, p1=p1: e.tensor_tensor(out=o_[:, 0:n], in0=acc[:, cc, c0:c0 + n], in1=p1[:, 0:n], op=ALU.mult), r=[acc, p1], w=[o_])
                S.op("dve", lambda e, o_=o_, n=n, p2=p2: e.tensor_tensor(out=o_[:, 0:n], in0=o_[:, 0:n], in1=p2[:, 0:n], op=ALU.add), r=[o_, p2], w=[o_])
                S.op("act", lambda e, o_=o_, o_b=o_b, cc=cc, n=n: e.activation(out=o_b[:, cc, 0:n], in_=o_[:, 0:n], func=AF.Silu,
                                                                              scale=cb[:, l, cc, 1:2], bias=cb[:, l, cc, 2:3]), r=[o_, cb], w=[o_b])
            for (a_lo, a_hi, s_lo) in ((0, 256, 0), (271, NA, 256)):
                lo, hi = max(a_lo, c0), min(a_hi, c0 + n)
                if lo >= hi:
                    continue
                for (ap, off, ln) in self.mix_dst(mixT, 0, 512, s_lo + lo - a_lo, hi - lo):
                    S.dma("pool", ap.rearrange("(cc p) n -> p cc n", p=128), o_b[:, :, lo - c0 + off:lo - c0 + off + ln], o_b, r=[o_b], w=[mixT])
        S.pop()


    def rwkv_stage(self, l, pT, mixT):
        S = self.S
        I = S.i
        NB = 256
        ones = self.ones_bf
        yd = [S.dram(f"rw_y{d}_{l}", [512, T], F32) for d in range(2)]
        bon = [S.dram(f"rw_b{d}_{l}", [512, T], F32) for d in range(2)]
        c_r, c_k, c_v, c_w, c_a, c_g = (COLS[n][0] for n in ("rr", "rk", "rv", "rw", "ra", "rg"))
        S.push()
        rwt = S.sb([64, 8, 19], F32, "rwt")
        S.dma("sp", rwt[:], self.ext["rwt"].t[:, l], rwt, w=[rwt])
        rwl = S.sb([64, 2, 2, 3], F32, "rwl")
        S.dma("sp", rwl[:], self.ext["rwl"].t[:, l], rwl, w=[rwl])
        w2f = S.sb([64, 2, 2, 512], F32, "w2f")
        w2b = S.sb([64, 2, 2, 512], BF16, "w2b")
        S.dma("sp", w2f[:], self.ext["rw2"].t[:, l], w2f, w=[w2f])
        I("dve", "tensor_copy", out=w2b[:], in_=w2f[:], r=[w2f], w=[w2b])
        msk = S.sb([64, 2, 192], F32, "rmask")
        S.dma("sp", msk[:], self.ext["rmask"].t, msk, w=[msk])

        def tb(col):
            return rwt[:, :, col:col + 1]

        for d in range(2):
            S.push()
            ST = S.sb([64, 8, 64], F32, "ST")
            STb = S.sb([64, 8, 64], BF16, "STb")
            I("dve", "memset", ST[:], 0.0, w=[ST])
            I("dve", "memset", STb[:], 0.0, w=[STb])
            xr = S.sb([64, 8, NB + 2], F32, "xr")
            xk = S.sb([64, 8, NB + 2], F32, "xk")
            xv = S.sb([64, 8, NB + 2], F32, "xv")
            xl = S.sb([64, 2, NB + 2], F32, "xl")
            rs_ = S.sb([64, 8, NB], F32, "rs_")
            ks_ = S.sb([64, 8, NB], F32, "ks_")
            vs_ = S.sb([64, 8, NB], F32, "vs_")
            ls_ = S.sb([64, 2, NB], F32, "ls_")
            lb_ = S.sb([64, 2, NB], BF16, "lb_")
            t1 = S.sb([64, 8, NB], F32, "t1")
            t2 = S.sb([64, 8, NB], F32, "t2")
            tbf = S.sb([64, 8, NB], BF16, "tbf")
            lw = S.sb([64, 8, NB], F32, "lw")
            av = S.sb([64, 8, NB], F32, "av")
            kk = S.sb([64, 8, NB], F32, "kk")
            Lp = S.sb([64, 64 + 8 * NB], F32, "Lp")
            onesf = S.sb([64, 8 * NB], BF16, "onesf")
            tot = S.sb([64, 32], F32, "tot")
            wcc = S.sb([64, 32], F32, "wcc")
            AR = S.sb([64, 8, 4, 2, 64], BF16, "AR")
            KT = S.sb([64, 8, 4, 64], BF16, "KT")
            BT = S.sb([64, 8, 4, 64], BF16, "BT")
            vb = S.sb([64, 8, NB], BF16, "vb")
            vtok = S.sb([64, 8, 64], BF16, "vtok")
            kbtok = S.sb([64, 2, 8, 64], BF16, "kbtok")
            Mkb = S.sb([64, 4, 128], BF16, "Mkb")
            Mbb = S.sb([64, 4, 128], BF16, "Mbb")
            Pk = [S.sb([64, 2, 4, 64], BF16, "Pk") for _ in range(2)]
            U = S.sb([64, 4, 64], F32, "U")
            Ub = S.sb([64, 4, 64], BF16, "Ub")
            yblk = S.sb([64, 8, NB], F32, "yblk")
            tS = S.sb([64, 4, 64], F32, "tS")
            psA = S.ps([64, 512], F32, "rpsA")
            psB = S.ps([64, 512], F32, "rpsB")
            psP = S.ps([64, 512], F32, "rpsP")
            psQ = S.ps([64, 512], F32, "rpsQ")
            psX = S.ps([64, 512], F32, "rpsX")
            psT = S.ps([64, 1024], BF16, "rpsT")
            psT2 = S.ps([64, 1024], BF16, "rpsT2")
            I("dve", "memset", Lp[:, 0:64], 0.0, w=[Lp])
            I("dve", "memset", onesf[:], 1.0, w=[onesf])
            blks = [(0, 0, 256)] + [(256 + NB * q, 256, T) for q in range(16)]
            if d == 1:
                blks = [blks[0]] + blks[:0:-1]
            blks = blks[:RW_NBLK]
            for (s0, lo, hi) in blks:
                a0, a1 = max(lo, s0 - 1), min(hi, s0 + NB + 1)
                c0_, c1_ = a0 - (s0 - 1), a1 - (s0 - 1)
                for (xt, col) in ((xr, c_r), (xk, c_k), (xv, c_v)):
                    I("pool", "memset", xt[:], 0.0, w=[xt])
                    S.dma("sp", xt[:, :, c0_:c1_], pT.t[col:col + 512, a0:a1].rearrange("(h i) t -> i h t", i=64), xt, r=[pT], w=[xt])
                I("pool", "memset", xl[:], 0.0, w=[xl])
                S.dma("act", xl[:, 0, c0_:c1_], pT.t[c_w + d * 64:c_w + d * 64 + 64, a0:a1], xl, r=[pT], w=[xl])
                S.dma("act", xl[:, 1, c0_:c1_], pT.t[c_a + d * 64:c_a + d * 64 + 64, a0:a1], xl, r=[pT], w=[xl])
                for (xt, out_, tcol) in ((xr, rs_, 0), (xk, ks_, 3), (xv, vs_, 6)):
                    I("dve", "tensor_tensor", out=out_[:], in0=xt[:, :, 1:NB + 1], in1=tb(tcol).to_broadcast([64, 8, NB]), op=ALU.mult, r=[xt, rwt], w=[out_])
                    I("dve", "tensor_tensor", out=t1[:], in0=xt[:, :, 0:NB], in1=tb(tcol + 1).to_broadcast([64, 8, NB]), op=ALU.mult, r=[xt, rwt], w=[t1])
                    I("dve", "tensor_tensor", out=out_[:], in0=out_[:], in1=t1[:], op=ALU.add, r=[out_, t1], w=[out_])
                    I("dve", "tensor_tensor", out=t1[:], in0=xt[:, :, 2:NB + 2], in1=tb(tcol + 2).to_broadcast([64, 8, NB]), op=ALU.mult, r=[xt, rwt], w=[t1])
                    I("dve", "tensor_tensor", out=out_[:], in0=out_[:], in1=t1[:], op=ALU.add, r=[out_, t1], w=[out_])
                for q in range(2):
                    I("dve", "tensor_scalar", out=ls_[:, q, :], in0=xl[:, q, 1:NB + 1], scalar1=rwl[:, d, q, 0:1], scalar2=None, op0=ALU.mult, r=[xl, rwl], w=[ls_])
                    I("dve", "scalar_tensor_tensor", out=ls_[:, q, :], in0=xl[:, q, 0:NB], scalar=rwl[:, d, q, 1:2], in1=ls_[:, q, :], op0=ALU.mult, op1=ALU.add, r=[xl, rwl, ls_], w=[ls_])
                    I("dve", "scalar_tensor_tensor", out=ls_[:, q, :], in0=xl[:, q, 2:NB + 2], scalar=rwl[:, d, q, 2:3], in1=ls_[:, q, :], op0=ALU.mult, op1=ALU.add, r=[xl, rwl, ls_], w=[ls_])
                I("act", "activation", out=lb_[:, 0, :], in_=ls_[:, 0, :], func=AF.Tanh, r=[ls_], w=[lb_])
                I("act", "copy", out=lb_[:, 1, :], in_=ls_[:, 1, :], r=[ls_], w=[lb_])
                for (q, dst, bcol) in ((0, lw, 15 + d), (1, av, 17 + d)):
                    for hp in range(4):
                        ps = psA if hp % 2 == 0 else psB
                        for hh in range(2):
                            h = hp * 2 + hh
                            I("pe", "matmul", ps[:, hh * NB:(hh + 1) * NB], lhsT=w2b[:, d, q, h * 64:(h + 1) * 64], rhs=lb_[:, q, :], start=True, stop=True, r=[w2b, lb_], w=[ps])
                        I("dve", "tensor_tensor", out=dst[:, hp * 2:hp * 2 + 2, :], in0=ps[:, 0:2 * NB].rearrange("p (h n) -> p h n", h=2),
                          in1=rwt[:, hp * 2:hp * 2 + 2, bcol:bcol + 1].to_broadcast([64, 2, NB]), op=ALU.add, r=[ps, rwt], w=[dst])
                    I("act", "activation", out=dst[:], in_=dst[:], func=AF.Sigmoid, r=[dst], w=[dst])
                I("dve", "tensor_scalar", out=lw[:], in0=lw[:], scalar1=-float(np.exp(-0.5)), scalar2=None, op0=ALU.mult, r=[lw], w=[lw])
                I("dve", "tensor_tensor", out=kk[:], in0=ks_[:], in1=tb(9).to_broadcast([64, 8, NB]), op=ALU.mult, r=[ks_, rwt], w=[kk])
                I("act", "activation", out=tbf[:], in_=kk[:], func=AF.Square, r=[kk], w=[tbf])
                for hp in range(4):
                    ps = psA if hp % 2 == 0 else psB
                    I("pe", "matmul", ps[:, 0:2 * NB], lhsT=ones[0:64, 0:64], rhs=tbf[:, hp * 2:hp * 2 + 2, :], start=True, stop=True, r=[ones, tbf], w=[ps])
                    I("act", "activation", out=t1[:, hp * 2:hp * 2 + 2, :], in_=ps[:, 0:2 * NB].rearrange("p (h n) -> p h n", h=2), func=AF.Sqrt, r=[ps], w=[t1])
                I("dve", "tensor_scalar", out=t1[:], in0=t1[:], scalar1=1e-12, scalar2=None, op0=ALU.max, r=[t1], w=[t1])
                I("dve", "reciprocal", out=t1[:], in_=t1[:], r=[t1], w=[t1])
                I("dve", "tensor_tensor", out=kk[:], in0=kk[:], in1=t1[:], op=ALU.mult, r=[kk, t1], w=[kk])
                I("dve", "tensor_tensor", out=t1[:], in0=av[:], in1=tb(10).to_broadcast([64, 8, NB]), op=ALU.mult, r=[av, rwt], w=[t1])
                I("dve", "tensor_tensor", out=t1[:], in0=t1[:], in1=tb(11).to_broadcast([64, 8, NB]), op=ALU.add, r=[t1, rwt], w=[t1])
                I("dve", "tensor_tensor", out=ks_[:], in0=ks_[:], in1=t1[:], op=ALU.mult, r=[ks_, t1], w=[ks_])
                I("dve", "tensor_tensor", out=av[:], in0=av[:], in1=kk[:], op=ALU.mult, r=[av, kk], w=[av])
                I("dve", "tensor_tensor", out=t1[:], in0=rs_[:], in1=ks_[:], op=ALU.mult, r=[rs_, ks_], w=[t1])
                I("dve", "tensor_tensor", out=tbf[:], in0=t1[:], in1=tb(12).to_broadcast([64, 8, NB]), op=ALU.mult, r=[t1, rwt], w=[tbf])
                for hp in range(4):
                    ps = psA if hp % 2 == 0 else psB
                    I("pe", "matmul", ps[:, 0:2 * NB], lhsT=ones[0:64, 0:64], rhs=tbf[:, hp * 2:hp * 2 + 2, :], start=True, stop=True, r=[ones, tbf], w=[ps])
                    I("dve", "tensor_tensor", out=t2[:, hp * 2:hp * 2 + 2, :], in0=ps[:, 0:2 * NB].rearrange("p (h n) -> p h n", h=2), in1=vs_[:, hp * 2:hp * 2 + 2, :], op=ALU.mult, r=[ps, vs_], w=[t2])
                S.dma("act", bon[d].t[:, s0:s0 + NB].rearrange("(h i) t -> i h t", i=64), t2[:], t2, r=[t2], w=[bon[d]])
                I("dve", "tensor_tensor_scan", out=Lp[:, 64:], data0=onesf[:], data1=lw[:].rearrange("p h n -> p (h n)"), initial=0.0, op0=ALU.mult, op1=ALU.add, r=[onesf, lw], w=[Lp])
                Lc = Lp[:, 64:].rearrange("p (m n) -> p m n", n=64)
                Lprev = Lp[:, 0:8 * NB].rearrange("p (m n) -> p m n", n=64)[:, :, 63:64]
                t1v = t1[:].rearrange("p h (c n) -> p (h c) n", n=64)
                t2v = t2[:].rearrange("p h (c n) -> p (h c) n", n=64)
                lwv = lw[:].rearrange("p h (c n) -> p (h c) n", n=64)
                I("dve", "tensor_tensor", out=t1v, in0=Lc, in1=Lprev.to_broadcast([64, 32, 64]), op=ALU.subtract, r=[Lp], w=[t1])
                I("dve", "tensor_copy", out=tot[:].unsqueeze(2), in_=t1v[:, :, 63:64], r=[t1], w=[tot])
                I("act", "activation", out=wcc[:], in_=tot[:], func=AF.Exp, r=[tot], w=[wcc])
                I("dve", "tensor_tensor", out=t2v, in0=t1v, in1=lwv, op=ALU.subtract, r=[t1, lw], w=[t2])
                if d == 1:
                    I("dve", "tensor_tensor", out=lwv, in0=tot[:].unsqueeze(2).to_broadcast([64, 32, 64]), in1=t2v, op=ALU.subtract, r=[tot, t2], w=[lw])
                    I("dve", "tensor_tensor", out=t2v, in0=tot[:].unsqueeze(2).to_broadcast([64, 32, 64]), in1=t1v, op=ALU.subtract, r=[tot, t1], w=[t2])
                    I("dve", "tensor_copy", out=t1[:], in_=lw[:], r=[lw], w=[t1])
                ARv = AR[:].rearrange("p h c a n -> p (h c) a n")
                I("act", "activation", out=lw[:], in_=t2[:], func=AF.Exp, r=[t2], w=[lw])
                I("dve", "scalar_tensor_tensor", out=ARv[:, :, 0, :], in0=kk[:].rearrange("p h (c n) -> p (h c) n", n=64), scalar=-1.0, in1=lwv, op0=ALU.mult, op1=ALU.mult, r=[kk, lw], w=[AR])
                I("act", "activation", out=lw[:], in_=t1[:], func=AF.Exp, r=[t1], w=[lw])
                I("dve", "tensor_tensor", out=ARv[:, :, 1, :], in0=rs_[:].rearrange("p h (c n) -> p (h c) n", n=64), in1=lwv, op=ALU.mult, r=[rs_, lw], w=[AR])
                I("act", "activation", out=lw[:], in_=t1[:], func=AF.Exp, scale=-1.0, r=[t1], w=[lw])
                I("dve", "tensor_tensor", out=KT[:].rearrange("p h c n -> p h (c n)"), in0=ks_[:], in1=lw[:], op=ALU.mult, r=[ks_, lw], w=[KT])
                I("dve", "tensor_tensor", out=BT[:].rearrange("p h c n -> p h (c n)"), in0=av[:], in1=lw[:], op=ALU.mult, r=[av, lw], w=[BT])
                I("act", "copy", out=vb[:], in_=vs_[:], r=[vs_], w=[vb])
                chunks = range(4) if d == 0 else range(3, -1, -1)
                if RW_LEVEL < 2:
                    chunks = []
                for q in chunks:
                    for h in range(8):
                        I("pe", "transpose", out=psT[:, h * 64:(h + 1) * 64], in_=vb[:, h, q * 64:(q + 1) * 64], identity=self.ident[0:64, 0:64], r=[vb, self.ident], w=[psT])
                    I("act", "copy", out=vtok[:].rearrange("p h n -> p (h n)"), in_=psT[:, 0:512], r=[psT], w=[vtok])
                    for h in range(8):
                        I("pe", "transpose", out=psT2[:, h * 64:(h + 1) * 64], in_=KT[:, h, q, :], identity=self.ident[0:64, 0:64], r=[KT, self.ident], w=[psT2])
                        I("pe", "transpose", out=psT2[:, 512 + h * 64:512 + (h + 1) * 64], in_=BT[:, h, q, :], identity=self.ident[0:64, 0:64], r=[BT, self.ident], w=[psT2])
                    I("act", "copy", out=kbtok[:].rearrange("p a h n -> p (a h n)"), in_=psT2[:, 0:1024], r=[psT2], w=[kbtok])
                    for hg in (range(2) if RW_LEVEL >= 3 else []):
                        hs = [hg * 4 + x for x in range(4)]
                        for x, h in enumerate(hs):
                            I("pe", "matmul", psA[:, x * 128:(x + 1) * 128], lhsT=KT[:, h, q, :], rhs=AR[:, h, q, :, :], start=True, stop=True, r=[KT, AR], w=[psA])
                        for x, h in enumerate(hs):
                            I("pe", "matmul", psB[:, x * 128:(x + 1) * 128], lhsT=BT[:, h, q, :], rhs=AR[:, h, q, :, :], start=True, stop=True, r=[BT, AR], w=[psB])
                        for x, h in enumerate(hs):
                            I("pe", "matmul", psP[:, x * 64:(x + 1) * 64], lhsT=AR[:, h, q, 0, :], rhs=BT[:, h, q, :], start=True, stop=True, r=[AR, BT], w=[psP])
                        I("dve", "tensor_tensor", out=Mkb[:], in0=psA[:, 0:512].rearrange("p (x n) -> p x n", x=4), in1=msk[:, d:d + 1, 0:128].to_broadcast([64, 4, 128]), op=ALU.mult, r=[psA, msk], w=[Mkb])
                        I("dve", "tensor_tensor", out=Mbb[:], in0=psB[:, 0:512].rearrange("p (x n) -> p x n", x=4), in1=msk[:, d:d + 1, 0:128].to_broadcast([64, 4, 128]), op=ALU.mult, r=[psB, msk], w=[Mbb])
                        pk = Pk[0]
                        I("dve", "tensor_tensor", out=pk[:, 0, :, :], in0=psP[:, 0:256].rearrange("p (x n) -> p x n", x=4), in1=msk[:, d:d + 1, 128:192].to_broadcast([64, 4, 64]), op=ALU.mult, r=[psP, msk], w=[pk])
                        I("act", "copy", out=pk[:, 1, :, :], in_=Mbb[:, :, 0:64], r=[Mbb], w=[pk])
                        if RW_LEVEL < 4:
                            continue
                        for x, h in enumerate(hs):
                            I("pe", "matmul", psX[:, x * 64:(x + 1) * 64], lhsT=Mkb[:, x, 0:64], rhs=vtok[:, h, :], start=True, stop=False, r=[Mkb, vtok], w=[psX])
                            I("pe", "matmul", psX[:, x * 64:(x + 1) * 64], lhsT=AR[:, h, q, 0, :], rhs=STb[:, h, :], start=False, stop=True, r=[AR, STb], w=[psX])
                        I("dve", "tensor_copy", out=U[:].rearrange("p x n -> p (x n)"), in_=psX[:, 0:256], r=[psX], w=[U])
                        I("dve", "tensor_copy", out=Ub[:], in_=U[:], r=[U], w=[Ub])
                        for lev in range(RW_NLEV):
                            pk = Pk[lev % 2]
                            for x in range(4):
                                I("pe", "matmul", psX[:, x * 64:(x + 1) * 64], lhsT=pk[:, 1, x, :], rhs=Ub[:, x, :], start=True, stop=True, r=[pk, Ub], w=[psX])
                            if lev < 5:
                                pn = Pk[(lev + 1) % 2]
                                for x in range(4):
                                    I("pe", "matmul", psQ[:, x * 64:(x + 1) * 64], lhsT=pk[:, 1, x, :], rhs=pk[:, 0, x, :], start=True, stop=True, r=[pk], w=[psQ])
                                    I("pe", "matmul", psQ[:, 256 + x * 64:256 + (x + 1) * 64], lhsT=pk[:, 0, x, :], rhs=pk[:, 1, x, :], start=True, stop=True, r=[pk], w=[psQ])
                                I("act", "copy", out=pn[:].rearrange("p a x n -> p (a x n)"), in_=psQ[:, 0:512], r=[psQ], w=[pn])
                            I("dve", "tensor_tensor", out=U[:].rearrange("p x n -> p (x n)"), in0=U[:].rearrange("p x n -> p (x n)"), in1=psX[:, 0:256], op=ALU.add, r=[U, psX], w=[U])
                            I("dve", "tensor_copy", out=Ub[:], in_=U[:], r=[U], w=[Ub])
                        if RW_LEVEL < 5:
                            continue
                        for x, h in enumerate(hs):
                            I("pe", "matmul", psP[:, 256 + x * 64:256 + (x + 1) * 64], lhsT=vtok[:, h, :], rhs=Mkb[:, x, 64:128], start=True, stop=False, r=[vtok, Mkb], w=[psP])
                            I("pe", "matmul", psP[:, 256 + x * 64:256 + (x + 1) * 64], lhsT=Ub[:, x, :], rhs=Mbb[:, x, 64:128], start=False, stop=False, r=[Ub, Mbb], w=[psP])
                            I("pe", "matmul", psP[:, 256 + x * 64:256 + (x + 1) * 64], lhsT=STb[:, h, :], rhs=AR[:, h, q, 1, :], start=False, stop=True, r=[STb, AR], w=[psP])
                        I("act", "copy", out=yblk[:, hg * 4:hg * 4 + 4, q * 64:(q + 1) * 64], in_=psP[:, 256:512].rearrange("p (x n) -> p x n", x=4), r=[psP], w=[yblk])
                        if RW_LEVEL < 6:
                            continue
                        for x, h in enumerate(hs):
                            I("pe", "matmul", psQ[:, x * 64:(x + 1) * 64], lhsT=kbtok[:, 0, h, :], rhs=vtok[:, h, :], start=True, stop=False, r=[kbtok, vtok], w=[psQ])
                            I("pe", "matmul", psQ[:, x * 64:(x + 1) * 64], lhsT=kbtok[:, 1, h, :], rhs=Ub[:, x, :], start=False, stop=True, r=[kbtok, Ub], w=[psQ])
                        I("dve", "tensor_tensor", out=tS[:], in0=ST[:, hg * 4:hg * 4 + 4, :], in1=psQ[:, 0:256].rearrange("p (x n) -> p x n", x=4), op=ALU.add, r=[ST, psQ], w=[tS])
                        wv = wcc[:].rearrange("p (h c) -> p h c", c=4)[:, hg * 4:hg * 4 + 4, q:q + 1]
                        I("dve", "tensor_tensor", out=ST[:, hg * 4:hg * 4 + 4, :], in0=tS[:], in1=wv.to_broadcast([64, 4, 64]), op=ALU.mult, r=[tS, wcc], w=[ST])
                        I("act", "copy", out=STb[:, hg * 4:hg * 4 + 4, :], in_=ST[:, hg * 4:hg * 4 + 4, :], r=[ST], w=[STb])
                        if RW_BAR:
                            S.barrier()
                S.dma("act", yd[d].t[:, s0:s0 + NB].rearrange("(h i) t -> i h t", i=64), yblk[:], yblk, r=[yblk], w=[yd[d]])
            S.pop()
        g2f = S.sb([128, 2, 512], F32, "g2f")
        g2b = S.sb([128, 2, 512], BF16, "g2b")
        S.dma("sp", g2f[:], self.ext["rg2"].t[:, l], g2f, w=[g2f])
        I("dve", "tensor_copy", out=g2b[:], in_=g2f[:], r=[g2f], w=[g2b])
        rgl = S.sb([128, 2, 3], F32, "rgl")
        S.dma("sp", rgl[:], self.ext["rgl"].t[:, l], rgl, w=[rgl])
        NF = 256
        y0 = S.sb([64, 8, NF], F32, "fy0")
        y1 = S.sb([64, 8, NF], F32, "fy1")
        b0 = S.sb([64, 8, NF], F32, "fb0")
        b1 = S.sb([64, 8, NF], F32, "fb1")
        yb = S.sb([64, 8, NF], BF16, "fyb")
        mu = S.sb([64, 8, NF], F32, "fmu")
        va = S.sb([64, 8, NF], F32, "fva")
        xg = S.sb([128, 2, NF + 2], F32, "fxg")
        sg = S.sb([128, 2, NF], F32, "fsg")
        sgb = S.sb([128, 2, NF], BF16, "fsgb")
        ob = S.sb([64, 8, NF], BF16, "fob")
        psA = S.ps([64, 512], F32, "fpsA")
        psB = S.ps([64, 512], F32, "fpsB")
        psG = S.ps([64, 512], F32, "fpsG")
        for (s0, lo, hi) in ([(0, 0, 256)] + [(256 + NF * q, 256, T) for q in range(16)])[:RW_NFIN]:
            for (tl, src) in ((y0, yd[0]), (y1, yd[1]), (b0, bon[0]), (b1, bon[1])):
                S.dma("sp", tl[:], src.t[:, s0:s0 + NF].rearrange("(h i) t -> i h t", i=64), tl, r=[src], w=[tl])
            I("dve", "tensor_tensor", out=y0[:], in0=y0[:], in1=y1[:], op=ALU.add, r=[y0, y1], w=[y0])
            I("dve", "tensor_tensor", out=b0[:], in0=b0[:], in1=b1[:], op=ALU.add, r=[b0, b1], w=[b0])
            I("act", "copy", out=yb[:], in_=y0[:], r=[y0], w=[yb])
            for hp in range(4):
                ps = psA if hp % 2 == 0 else psB
                I("pe", "matmul", ps[:, 0:2 * NF], lhsT=ones[0:64, 0:64], rhs=yb[:, hp * 2:hp * 2 + 2, :], start=True, stop=True, r=[ones, yb], w=[ps])
                I("act", "activation", out=mu[:, hp * 2:hp * 2 + 2, :], in_=ps[:, 0:2 * NF].rearrange("p (h n) -> p h n", h=2), func=AF.Identity, scale=1.0 / 64, r=[ps], w=[mu])
            I("dve", "tensor_tensor", out=y0[:], in0=y0[:], in1=mu[:], op=ALU.subtract, r=[y0, mu], w=[y0])
            I("act", "activation", out=yb[:], in_=y0[:], func=AF.Square, r=[y0], w=[yb])
            for hp in range(4):
                ps = psA if hp % 2 == 0 else psB
                I("pe", "matmul", ps[:, 0:2 * NF], lhsT=ones[0:64, 0:64], rhs=yb[:, hp * 2:hp * 2 + 2, :], start=True, stop=True, r=[ones, yb], w=[ps])
                I("dve", "tensor_scalar", out=va[:, hp * 2:hp * 2 + 2, :], in0=ps[:, 0:2 * NF].rearrange("p (h n) -> p h n", h=2), scalar1=1.0 / 64, scalar2=64e-5, op0=ALU.mult, op1=ALU.add, r=[ps], w=[va])
            I("act", "activation", out=va[:], in_=va[:], func=AF.Sqrt, r=[va], w=[va])
            I("dve", "reciprocal", out=va[:], in_=va[:], r=[va], w=[va])
            I("dve", "tensor_tensor", out=y0[:], in0=y0[:], in1=va[:], op=ALU.mult, r=[y0, va], w=[y0])
            I("dve", "tensor_tensor", out=y0[:], in0=y0[:], in1=rwt[:, :, 13:14].to_broadcast([64, 8, NF]), op=ALU.mult, r=[y0, rwt], w=[y0])
            I("dve", "tensor_tensor", out=y0[:], in0=y0[:], in1=rwt[:, :, 14:15].to_broadcast([64, 8, NF]), op=ALU.add, r=[y0, rwt], w=[y0])
            I("dve", "tensor_tensor", out=y0[:], in0=y0[:], in1=b0[:], op=ALU.add, r=[y0, b0], w=[y0])
            a0, a1 = max(lo, s0 - 1), min(hi, s0 + NF + 1)
            c0_, c1_ = a0 - (s0 - 1), a1 - (s0 - 1)
            I("pool", "memset", xg[:], 0.0, w=[xg])
            S.dma("act", xg[:, 0, c0_:c1_], pT.t[c_g:c_g + 128, a0:a1], xg, r=[pT], w=[xg])
            S.dma("act", xg[0:32, 1, c0_:c1_], pT.t[c_g + 128:c_g + 160, a0:a1], xg, r=[pT], w=[xg])
            for q in range(2):
                I("dve", "tensor_scalar", out=sg[:, q, :], in0=xg[:, q, 1:NF + 1], scalar1=rgl[:, q, 0:1], scalar2=None, op0=ALU.mult, r=[xg, rgl], w=[sg])
                I("dve", "scalar_tensor_tensor", out=sg[:, q, :], in0=xg[:, q, 0:NF], scalar=rgl[:, q, 1:2], in1=sg[:, q, :], op0=ALU.mult, op1=ALU.add, r=[xg, rgl, sg], w=[sg])
                I("dve", "scalar_tensor_tensor", out=sg[:, q, :], in0=xg[:, q, 2:NF + 2], scalar=rgl[:, q, 2:3], in1=sg[:, q, :], op0=ALU.mult, op1=ALU.add, r=[xg, rgl, sg], w=[sg])
            I("act", "activation", out=sgb[:], in_=sg[:], func=AF.Sigmoid, r=[sg], w=[sgb])
            for hp in range(4):
                for hh in range(2):
                    h = hp * 2 + hh
                    I("pe", "matmul", psG[:, hh * NF:(hh + 1) * NF], lhsT=g2b[:, 0, h * 64:(h + 1) * 64], rhs=sgb[:, 0, :], start=True, stop=False, r=[g2b, sgb], w=[psG])
                    I("pe", "matmul", psG[:, hh * NF:(hh + 1) * NF], lhsT=g2b[0:32, 1, h * 64:(h + 1) * 64], rhs=sgb[0:32, 1, :], start=False, stop=True, r=[g2b, sgb], w=[psG])
                I("dve", "tensor_tensor", out=ob[:, hp * 2:hp * 2 + 2, :], in0=y0[:, hp * 2:hp * 2 + 2, :], in1=psG[:, 0:2 * NF].rearrange("p (h n) -> p h n", h=2), op=ALU.mult, r=[y0, psG], w=[ob])
            for (ap, off, ln) in self.mix_dst(mixT, 512, 512, s0, NF):
                S.dma("pool", ap.rearrange("(h i) n -> i h n", i=64), ob[:, :, off:off + ln], ob, r=[ob], w=[mixT])
        S.pop()

    def convert_mla(self):
        S = self.S
        NL = self.NL
        wuq = self.ext_in("wuq", [NL, 1536, 1536], F32)
        wukv = self.ext_in("wukv", [NL, 512, 2048], F32)
        self.wuqb = S.dram("wuqb", [NL, 8, 128, 12, 192], BF16)
        self.wukvb = S.dram("wukvb", [NL, 128, 4, 2048], BF16)
        S.push()
        f = [S.sb([128, 2048], F32, "mcf") for _ in range(2)]
        b = [S.sb([128, 2048], BF16, "mcb") for _ in range(2)]
        eng = RR(["dve", "pool", "act"])
        it = 0
        for l in range(NL):
            for c in range(12):
                ff, bb = f[it % 2], b[it % 2]
                it += 1
                S.dma("sp", ff[:, 0:1536], wuq.t[l, c * 128:(c + 1) * 128, :], ff, w=[ff])
                for q in range(2):
                    copy_on(S, eng(), bb[:, q * 768:(q + 1) * 768], ff[:, q * 768:(q + 1) * 768], [ff], [bb])
                S.dma("pool", self.wuqb.t[l, :, :, c, :].rearrange("h p x -> p h x"),
                      bb[:, 0:1536].rearrange("p (h x) -> p h x", x=192), bb, r=[bb], w=[self.wuqb])
            for c in range(4):
                ff, bb = f[it % 2], b[it % 2]
                it += 1
                S.dma("sp", ff[:], wukv.t[l, c * 128:(c + 1) * 128, :], ff, w=[ff])
                for q in range(2):
                    copy_on(S, eng(), bb[:, q * 1024:(q + 1) * 1024], ff[:, q * 1024:(q + 1) * 1024], [ff], [bb])
                S.dma("pool", self.wukvb.t[l, :, c, :], bb[:], bb, r=[bb], w=[self.wukvb])
        S.pop()

    def rstd_from_ps(self, ps, out_sb, rows, n, inv_n, eps):
        S = self.S
        S.op("dve", lambda e: e.tensor_scalar(out=out_sb[0:rows, 0:n], in0=ps[0:rows, 0:n], scalar1=inv_n, scalar2=eps,
                                              op0=ALU.mult, op1=ALU.add), r=[ps], w=[out_sb])
        S.op("act", lambda e: e.activation(out=out_sb[0:rows, 0:n], in_=out_sb[0:rows, 0:n], func=AF.Sqrt), r=[out_sb], w=[out_sb])
        S.op("dve", lambda e: e.reciprocal(out=out_sb[0:rows, 0:n], in_=out_sb[0:rows, 0:n]), r=[out_sb], w=[out_sb])

    def mla_stage(self, l, pT, mixT):
        S = self.S
        I = S.i
        SCALE = float((128 + 64) ** -0.5)
        blocks = [(0, 256)] + [(256 + 512 * q, 512) for q in range(8)]
        S.push()
        mg = S.sb([128, 2, 24], F32, "mg")
        S.dma("sp", mg[:], self.ext["mlag"].t, mg, w=[mg])
        qsc = S.sb([128, 2], F32, "qsc")
        I("dve", "tensor_scalar", out=qsc[:], in0=mg[:, l, 16:18], scalar1=SCALE, scalar2=None, op0=ALU.mult, r=[mg], w=[qsc])
        rotf = S.sb([64, 64], F32, "rotf")
        rotb = S.sb([64, 64], BF16, "rotb")
        S.dma("sp", rotf[:], self.ext["rotm"].t, rotf, w=[rotf])
        I("dve", "tensor_copy", out=rotb[:], in_=rotf[:], r=[rotf], w=[rotb])
        nbias = S.sb([128, 1], F32, "nbias")
        I("dve", "memset", nbias[:], -6.0, w=[nbias])
        wkv = S.sb([128, 4, 2048], BF16, "wkv")
        S.dma("sp", wkv[:], self.wukvb.t[l], wkv, r=[self.wukvb], w=[wkv])
        krT = S.sb([64, T], BF16, "krT")
        kT = S.sb([128, 4, T], BF16, "kT")
        vt = S.sb([128, 34, 512], BF16, "vt")
        wq = [S.sb([128, 12, 192], BF16, "wq") for _ in range(2)]
        xin = S.sb([128, 4, 512], F32, "xin")
        krin = S.sb([64, 512], F32, "krin")
        ropeb = S.sb([64, 2, 512], F32, "ropeb")
        xsq = S.sb([128, 512], BF16, "xsq")
        xnb = S.sb([128, 12, 512], BF16, "xnb")
        rs = S.sb([128, 512], F32, "rs")
        rs2 = S.sb([128, 512], F32, "rs2")
        tmp = S.sb([128, 512], F32, "mtmp")
        tmp2 = S.sb([64, 512], F32, "mtmp2")
        xrb = S.sb([64, 512], BF16, "xrb")
        qnb = S.sb([128, 512], BF16, "qnb")
        qrb = S.sb([64, 512], BF16, "qrb")
        pb = [S.sb([128, 512], BF16, "pb") for _ in range(2)]
        ob = S.sb([128, 512], BF16, "mob")
        ps_s = [S.ps([128, 512], F32, "ps_s") for _ in range(2)]
        ps_o = S.ps([128, 512], F32, "ps_o")
        ps_d = S.ps([128, 512], F32, "ps_d")
        ps_a = S.ps([128, 512], F32, "ps_a")
        ps_b = S.ps([128, 512], F32, "ps_b")
        ps_n = S.ps([128, 512], F32, "ps_n")
        c_kv, c_kr, c_q = COLS["mkv"][0], COLS["mkr"][0], COLS["mq"][0]
        ones = self.ones_bf

        def norm_in(col0, nch, s0, n, gcol0, inv_n):
            for c4 in range(0, nch, 4):
                S.dma("sp", xin[:, :, 0:n], pT.t[col0 + c4 * 128:col0 + (c4 + 4) * 128, s0:s0 + n].rearrange("(c p) n -> p c n", p=128), xin, r=[pT], w=[xin])
                for cc in range(4):
                    c = c4 + cc
                    I("act", "activation", out=xsq[:, 0:n], in_=xin[:, cc, 0:n], func=AF.Square, r=[xin], w=[xsq])
                    I("pe", "matmul", ps_n[:, 0:n], lhsT=ones[:], rhs=xsq[:, 0:n], start=(c == 0), stop=(c == nch - 1), r=[xsq, ones], w=[ps_n])
            self.rstd_from_ps(ps_n, rs, 128, n, inv_n, 1e-6)
            for c4 in range(0, nch, 4):
                S.dma("sp", xin[:, :, 0:n], pT.t[col0 + c4 * 128:col0 + (c4 + 4) * 128, s0:s0 + n].rearrange("(c p) n -> p c n", p=128), xin, r=[pT], w=[xin])
                for cc in range(4):
                    c = c4 + cc
                    I("dve", "tensor_tensor", out=xin[:, cc, 0:n], in0=xin[:, cc, 0:n], in1=rs[:, 0:n], op=ALU.mult, r=[xin, rs], w=[xin])
                    I("act", "activation", out=xnb[:, c, 0:n], in_=xin[:, cc, 0:n], func=AF.Identity, scale=mg[:, l, gcol0 + c:gcol0 + c + 1], r=[xin, mg], w=[xnb])

        def head_norm(src, rows, n, gain_ap, out_ap, out_buf):
            I("act", "activation", out=xsq[0:rows, 0:n], in_=src[0:rows, 0:n], func=AF.Square, r=[src], w=[xsq])
            I("pe", "matmul", ps_n[0:rows, 0:n], lhsT=ones[0:rows, 0:rows], rhs=xsq[0:rows, 0:n], start=True, stop=True, r=[xsq, ones], w=[ps_n])
            self.rstd_from_ps(ps_n, rs2, rows, n, 1.0 / rows, 1e-6)
            I("dve", "tensor_tensor", out=tmp[0:rows, 0:n], in0=src[0:rows, 0:n], in1=rs2[0:rows, 0:n], op=ALU.mult, r=[src, rs2], w=[tmp])
            I("act", "activation", out=out_ap, in_=tmp[0:rows, 0:n], func=AF.Identity, scale=gain_ap, r=[tmp, mg, qsc], w=[out_buf])

        def rope_apply(n, out_ap, out_buf):
            I("dve", "tensor_copy", out=xrb[:, 0:n], in_=tmp2[:, 0:n], r=[tmp2], w=[xrb])
            I("pe", "matmul", ps_n[0:64, 0:n], lhsT=rotb[:], rhs=xrb[:, 0:n], start=True, stop=True, r=[rotb, xrb], w=[ps_n])
            I("dve", "tensor_tensor", out=tmp[0:64, 0:n], in0=ps_n[0:64, 0:n], in1=ropeb[:, 1, 0:n], op=ALU.mult, r=[ps_n, ropeb], w=[tmp])
            I("dve", "tensor_tensor", out=tmp2[:, 0:n], in0=tmp2[:, 0:n], in1=ropeb[:, 0, 0:n], op=ALU.mult, r=[tmp2, ropeb], w=[tmp2])
            I("dve", "tensor_tensor", out=out_ap, in0=tmp2[:, 0:n], in1=tmp[0:64, 0:n], op=ALU.add, r=[tmp2, tmp], w=[out_buf])

        def load_rope(s0, n):
            if s0 >= 256:
                S.dma("act", ropeb[:, :, 0:n], self.ext["ropeT"].t[:, :, s0 - 256:s0 - 256 + n], ropeb, w=[ropeb])

        wqi = 0
        for g in range(2):
            for bi, (s0, n) in enumerate(blocks):
                norm_in(c_kv, 4, s0, n, 12, 1.0 / 512)
                for hh in range(4):
                    h = g * 4 + hh
                    for c in range(4):
                        I("pe", "matmul", ps_a[:, 0:n], lhsT=wkv[:, c, h * 256:h * 256 + 128], rhs=xnb[:, c, 0:n], start=(c == 0), stop=(c == 3), r=[wkv, xnb], w=[ps_a])
                    head_norm(ps_a, 128, n, mg[:, l, 18:19], kT[:, hh, s0:s0 + n], kT)
                for tt in range(n // 128):
                    for hh in range(4):
                        h = g * 4 + hh
                        for c in range(4):
                            I("pe", "matmul", ps_b[:, hh * 128:(hh + 1) * 128], lhsT=xnb[:, c, tt * 128:(tt + 1) * 128],
                              rhs=wkv[:, c, h * 256 + 128:h * 256 + 256], start=(c == 0), stop=(c == 3), r=[wkv, xnb], w=[ps_b])
                    ti = s0 // 128 + tt
                    I("act", "copy", out=vt[:, ti, :], in_=ps_b[:, 0:512], r=[ps_b], w=[vt])
                if g == 0:
                    load_rope(s0, n)
                    S.dma("sp", krin[:, 0:n], pT.t[c_kr:c_kr + 64, s0:s0 + n], krin, r=[pT], w=[krin])
                    head_norm(krin, 64, n, mg[0:64, l, 19:20], tmp2[:, 0:n], tmp2)
                    if s0 >= 256:
                        rope_apply(n, krT[:, s0:s0 + n], krT)
                    else:
                        I("dve", "tensor_copy", out=krT[:, s0:s0 + n], in_=tmp2[:, 0:n], r=[tmp2], w=[krT])
            for bi, (s0, n) in enumerate(blocks):
                norm_in(c_q, 12, s0, n, 0, 1.0 / 1536)
                load_rope(s0, n)
                kcs = list(range(2)) if bi == 0 else list(range(34))
                for hh in range(4):
                    h = g * 4 + hh
                    wqh = wq[wqi % 2]
                    wqi += 1
                    S.dma("act", wqh[:], self.wuqb.t[l, h], wqh, r=[self.wuqb], w=[wqh])
                    for c in range(12):
                        I("pe", "matmul", ps_a[:, 0:n], lhsT=wqh[:, c, 0:128], rhs=xnb[:, c, 0:n], start=(c == 0), stop=(c == 11), r=[wqh, xnb], w=[ps_a])
                    for c in range(12):
                        I("pe", "matmul", ps_b[0:64, 0:n], lhsT=wqh[:, c, 128:192], rhs=xnb[:, c, 0:n], start=(c == 0), stop=(c == 11), r=[wqh, xnb], w=[ps_b])
                    head_norm(ps_a, 128, n, qsc[:, 0:1], qnb[:, 0:n], qnb)
                    head_norm(ps_b, 64, n, qsc[0:64, 1:2], tmp2[:, 0:n], tmp2)
                    if s0 >= 256:
                        rope_apply(n, qrb[:, 0:n], qrb)
                    else:
                        I("dve", "tensor_copy", out=qrb[:, 0:n], in_=tmp2[:, 0:n], r=[tmp2], w=[qrb])
                    for ki, kc in enumerate(kcs):
                        ps = ps_s[ki % 2]
                        pbb = pb[ki % 2]
                        I("pe", "matmul", ps[:, 0:n], lhsT=kT[:, hh, kc * 128:(kc + 1) * 128], rhs=qnb[:, 0:n], start=True, stop=False, r=[kT, qnb], w=[ps])
                        I("pe", "matmul", ps[:, 0:n], lhsT=krT[:, kc * 128:(kc + 1) * 128], rhs=qrb[:, 0:n], start=False, stop=True, r=[krT, qrb], w=[ps])
                        I("act", "activation", out=pbb[:, 0:n], in_=ps[:, 0:n], func=AF.Exp, bias=nbias[:, 0:1], r=[ps, nbias], w=[pbb])
                        first, last = (ki == 0), (ki == len(kcs) - 1)
                        I("pe", "matmul", ps_o[:, 0:n], lhsT=vt[:, kc, hh * 128:(hh + 1) * 128], rhs=pbb[:, 0:n], start=first, stop=last, r=[vt, pbb], w=[ps_o])
                        I("pe", "matmul", ps_d[:, 0:n], lhsT=ones[:], rhs=pbb[:, 0:n], start=first, stop=last, r=[ones, pbb], w=[ps_d])
                    I("dve", "reciprocal", out=rs2[:, 0:n], in_=ps_d[:, 0:n], r=[ps_d], w=[rs2])
                    I("dve", "tensor_tensor", out=ob[:, 0:n], in0=ps_o[:, 0:n], in1=rs2[:, 0:n], op=ALU.mult, r=[ps_o, rs2], w=[ob])
                    for (ap, off, ln) in self.mix_dst(mixT, 1024 + h * 128, 128, s0, n):
                        S.dma("pool", ap, ob[:, off:off + ln], ob, r=[ob], w=[mixT])
        S.pop()


    def gather_chunks(self, src, nch, name):
        S = self.S
        mid = S.dram(name + "_q", [nch, 4, 524288], BF16)
        dst = Buf(name + "_g")
        dst.t = [self.nc.dram_tensor(f"{name}_g{j}", [min(16, nch - 16 * j), 4, 2, 524288], BF16).ap() for j in range((nch + 15) // 16)]
        sem = Buf(name + "_sem")
        sem.persist = True
        for i in range(nch):
            S.coll("AllGather", src.t[i:i + 1, :], mid.t[i], QUADS, sem, r=[src], w=[mid])
        self.pending_g.append((mid, dst, sem, nch))
        return dst

    def gather_finish(self, n=1):
        S = self.S
        for _ in range(n):
            if not self.pending_g:
                return
            mid, dst, sem, nch = self.pending_g.pop(0)
            for i in range(nch):
                for q in range(4):
                    S.coll("AllGather", mid.t[i, q:q + 1, :], dst.t[i // 16][i % 16, q], P4, sem, r=[mid], w=[dst])

    def convert_big(self):
        S = self.S
        wo = self.ext_in("wo", [512 * self.NL, D], F32)
        src_o = S.dram("wo_src", [4 * self.NL, 524288], BF16)
        S.push()
        f = [S.sb([128, D], F32, "bf") for _ in range(2)]
        b = [S.sb([128, D], BF16, "bb") for _ in range(2)]
        eng = RR(["dve", "pool", "act"])
        it = 0
        for i in range(4 * self.NL):
            ff, bb = f[it % 2], b[it % 2]
            it += 1
            S.dma("sp", ff[:], wo.t[i * 128:(i + 1) * 128, :], ff, w=[ff])
            for q in range(4):
                copy_on(S, eng(), bb[:, q * 1024:(q + 1) * 1024], ff[:, q * 1024:(q + 1) * 1024], [ff], [bb])
            S.dma("pool", src_o.t[i].rearrange("(p n) -> p n", p=128), bb[:], bb, r=[bb], w=[src_o])
        self.wo_g = self.gather_chunks(src_o, 4 * self.NL, "wo")
        self.moe_g = {}
        if self.moe_on:
            for nm in ("w1", "w3", "w2"):
                nle = 2 * self.NL
                w = self.ext_in("m" + nm, [nle, D if nm != "w2" else 1024, 1024 if nm != "w2" else D], F32)
                src = S.dram(nm + "_src", [nle * 8, 524288], BF16)
                for i in range(nle):
                    if nm == "w2":
                        for part in range(8):
                            ff, bb = f[it % 2], b[it % 2]
                            it += 1
                            S.dma("sp", ff[:], w.t[i, part * 128:(part + 1) * 128, :], ff, w=[ff])
                            for q in range(4):
                                copy_on(S, eng(), bb[:, q * 1024:(q + 1) * 1024], ff[:, q * 1024:(q + 1) * 1024], [ff], [bb])
                            S.dma("pool", src.t[i * 8 + part].rearrange("(p n) -> p n", p=128), bb[:], bb, r=[bb], w=[src])
                    else:
                        for c4 in range(0, 32, 4):
                            ff, bb = f[it % 2], b[it % 2]
                            it += 1
                            S.dma("sp", ff[:].rearrange("p (c n) -> p c n", c=4),
                                  w.t[i, c4 * 128:(c4 + 4) * 128, :].rearrange("(c p) n -> p c n", p=128), ff, w=[ff])
                            for q in range(4):
                                copy_on(S, eng(), bb[:, q * 1024:(q + 1) * 1024], ff[:, q * 1024:(q + 1) * 1024], [ff], [bb])
                            for dc in range(8):
                                S.dma("pool", src.t[i * 8 + dc].rearrange("(p c n) -> p c n", p=128, c=32)[:, c4:c4 + 4, :],
                                      bb[:].rearrange("p (c d n) -> p c d n", c=4, d=8)[:, :, dc, :], bb, r=[bb], w=[src])
                self.moe_g[nm] = self.gather_chunks(src, nle * 8, nm)
        S.pop()

    def wo_block(self, l, kc):
        per = 4 * self.NL
        g = l * 32 + kc
        r_, ch = g // per, g % per
        return self.wo_g.t[ch // 16][ch % 16, r_ % 4, r_ // 4].rearrange("(p n) -> p n", p=128)

    def moe_chunk(self, nm, l, e, part):
        nle = 2 * self.NL
        le = l * 16 + e
        r_, i = le // nle, le % nle
        ch = i * 8 + part
        return self.moe_g[nm].t[ch // 16][ch % 16, r_ % 4, r_ // 4]

    def bcast_table(self, tab_ap_fn, out_sb, ps):
        S = self.S
        I = S.i
        S.push()
        dg = [S.sb([128, 128], F32, "dg") for _ in range(2)]
        dh = [S.sb([128, 2, 128], BF16, "dh") for _ in range(2)]
        for c in range(32):
            d_, h_ = dg[c % 2], dh[c % 2]
            p = ps[(c // 4) % 2]
            I("dve", "tensor_scalar", out=d_[:], in0=self.identf[:], scalar1=tab_ap_fn(c), scalar2=None, op0=ALU.mult, r=[self.identf] + self.mods, w=[d_])
            I("dve", "tensor_copy", out=h_[:, 0, :], in_=d_[:], r=[d_], w=[h_])
            I("dve", "tensor_tensor", out=d_[:], in0=d_[:], in1=h_[:, 0, :], op=ALU.subtract, r=[d_, h_], w=[d_])
            I("dve", "tensor_copy", out=h_[:, 1, :], in_=d_[:], r=[d_], w=[h_])
            for q in range(2):
                I("pe", "matmul", p[:, (c % 4) * 128:(c % 4 + 1) * 128], lhsT=self.ones_bf[:], rhs=h_[:, q, :], start=(q == 0), stop=(q == 1), r=[h_, self.ones_bf], w=[p])
            if c % 4 == 3:
                I("act", "copy", out=out_sb[:, (c - 3) * 128:(c + 1) * 128], in_=p[:, :], r=[p], w=[out_sb])
        S.pop()

    def outproj_stage(self, l, mix_all, h_src, h_mid):
        S = self.S
        I = S.i
        S.push()
        idx = S.sb([128, 5, 32], I32, "mixidx")
        S.dma("sp", idx[:], self.ext["mixidx"].t, idx, w=[idx])
        gab = S.sb([128, D], F32, "ga1bc")
        mixb = S.sb([128, 32, 512], BF16, "mixb")
        wb = [S.sb([128, 32, 512], BF16, "wob") for _ in range(2)]
        hb = S.sb([128, 4, D], F32, "ohb")
        ps = [S.ps([128, 512], F32, "ops") for _ in range(4)]
        tmp = [S.sb([128, 512], F32, "otmp") for _ in range(2)]
        flat = mix_all.t.rearrange("c r p n -> (c r p) n")
        blocks = [(0, 128, 1)] + [(128 + 512 * q, 512, 0) for q in range(4)]
        cur = None
        pi = 0
        wi = 0
        for bi, (t0, n, which) in enumerate(blocks):
            if cur != which:
                self.bcast_table(lambda c, which=which: self.modcol(which, l, 2, c), gab, ps[0:2])
                cur = which
            for kc in range(32):
                S.gather_rows(mixb[:, kc, :], flat, idx[:, bi, kc:kc + 1], mixb, r=[mix_all, idx], w=[mixb])
            nt = n // 128
            for ti in range(nt):
                S.dma("act", hb[:, ti, :], h_src.t[t0 + ti * 128:t0 + (ti + 1) * 128, :], hb, r=[h_src], w=[hb])
            for dcn in range(8):
                w = wb[wi % 2]
                wi += 1
                for kc in range(32):
                    S.dma("sp" if kc % 2 else "act", w[:, kc, :], self.wo_block(l, kc)[:, dcn * 512:(dcn + 1) * 512], w, r=[self.wo_g], w=[w])
                for ti in range(nt):
                    p = ps[pi % 4]
                    t_ = tmp[pi % 2]
                    pi += 1
                    for kc in range(32):
                        I("pe", "matmul", p[:, :], lhsT=mixb[:, kc, ti * 128:(ti + 1) * 128], rhs=w[:, kc, :], start=(kc == 0), stop=(kc == 31), r=[mixb, w], w=[p])
                    I("dve", "tensor_tensor", out=t_[:], in0=p[:, :], in1=gab[:, dcn * 512:(dcn + 1) * 512], op=ALU.mult, r=[p, gab], w=[t_])
                    I("dve", "tensor_tensor", out=hb[:, ti, dcn * 512:(dcn + 1) * 512], in0=hb[:, ti, dcn * 512:(dcn + 1) * 512], in1=t_[:], op=ALU.add, r=[hb, t_], w=[hb])
            for ti in range(nt):
                S.dma("pool", h_mid.t[t0 + ti * 128:t0 + (ti + 1) * 128, :], hb[:, ti, :], hb, r=[hb], w=[h_mid])
        S.pop()

    def moe_stage(self, l, n2T, h_mid, h_out, final_out=None):
        S = self.S
        I = S.i
        S.push()
        wr = S.sb([128, 32, 16], F32, "wrf")
        wrb = S.sb([128, 32, 16], BF16, "wrb")
        S.dma("sp", wr[:], self.ext["wrT"].t, wr, w=[wr])
        I("dve", "tensor_copy", out=wrb[:], in_=wr[:], r=[wr], w=[wrb])
        rb = S.sb([128, 16], F32, "rbias")
        S.dma("sp", rb[:], self.ext["rbias"].t, rb, w=[rb])
        selEb = S.sb([16, 16, 128], BF16, "selEb")
        I("dve", "tensor_copy", out=selEb[:], in_=self.identf[0:16, 0:16].unsqueeze(2).to_broadcast([16, 16, 128]), r=[self.identf], w=[selEb])
        gab = S.sb([128, D], F32, "ga2bc")
        nb = S.sb([128, 32, 512], BF16, "mnb")
        yacc = S.sb([128, 4, D], F32, "yacc")
        uT = S.sb([128, 8, 512], BF16, "uT")
        w13 = [S.sb([128, 32, 128], BF16, "w13") for _ in range(3)]
        w2b = [S.sb([128, 8, 512], BF16, "w2b") for _ in range(2)]
        gbc = S.sb([128, 512], F32, "gbc")
        sa = S.sb([128, 512], F32, "msa")
        rt = S.sb([128, 64], F32, "rt")
        gate = S.sb([128, 4, 16], F32, "gate")
        ghl = S.sb([128, 2, 16], BF16, "ghl")
        gT = S.sb([16, 2, 512], BF16, "gT")
        ps_a = [S.ps([128, 512], F32, "mpa") for _ in range(2)]
        ps_b = [S.ps([128, 512], F32, "mpb") for _ in range(2)]
        ps_y = [S.ps([128, 512], F32, "mpy") for _ in range(2)]
        ps_r = S.ps([128, 512], F32, "mpr")
        ps_t = S.ps([16, 512], BF16, "mpt")
        blocks = [(0, 128, 1)] + [(128 + 512 * q, 512, 0) for q in range(4)]
        cur = None
        wi = 0
        w2i = 0
        pi = 0
        for bi, (t0, n, which) in enumerate(blocks):
            if cur != which:
                self.bcast_table(lambda c, which=which: self.modcol(which, l, 5, c), gab, ps_y)
                cur = which
            nt = n // 128
            S.dma("sp", nb[:, :, 0:n], n2T.t[:, t0:t0 + n].rearrange("(c p) n -> p c n", p=128), nb, r=[n2T], w=[nb])
            for ti in range(nt):
                for c in range(32):
                    I("pe", "matmul", ps_r[:, 0:16], lhsT=nb[:, c, ti * 128:(ti + 1) * 128], rhs=wrb[:, c, :], start=(c == 0), stop=(c == 31), r=[nb, wrb], w=[ps_r])
                sc = rt[:, 0:16]
                bs = rt[:, 16:32]
                I("act", "activation", out=sc, in_=ps_r[:, 0:16], func=AF.Sigmoid, r=[ps_r], w=[rt])
                I("dve", "tensor_tensor", out=bs, in0=sc, in1=rb[:], op=ALU.add, r=[rt, rb], w=[rt])
                b4 = bs.rearrange("p (g k) -> p g k", k=4)
                pairs = [(0, 1), (0, 2), (0, 3), (1, 2), (1, 3), (2, 3)]
                psum6 = rt[:, 32:56].rearrange("p (g k) -> p g k", k=6)
                for qi, (x, y) in enumerate(pairs):
                    I("dve", "tensor_tensor", out=psum6[:, :, qi:qi + 1], in0=b4[:, :, x:x + 1], in1=b4[:, :, y:y + 1], op=ALU.add, r=[rt], w=[rt])
                gs = rt[:, 56:60]
                I("dve", "tensor_reduce", out=gs, in_=psum6, axis=AX.X, op=ALU.max, r=[rt], w=[rt])
                gm = rt[:, 60:64]
                I("dve", "tensor_reduce", out=gm, in_=b4, axis=AX.X, op=ALU.max, r=[rt], w=[rt])
                eqm = rt[:, 32:48].rearrange("p (g k) -> p g k", k=4)
                I("dve", "tensor_tensor", out=eqm, in0=b4, in1=gm.unsqueeze(2).to_broadcast([128, 4, 4]), op=ALU.is_ge, r=[rt], w=[rt])
                I("dve", "scalar_tensor_tensor", out=eqm, in0=eqm, scalar=-1.0e9, in1=b4, op0=ALU.mult, op1=ALU.add, r=[rt], w=[rt])
                thr = rt[:, 48:52]
                I("dve", "tensor_reduce", out=thr, in_=eqm, axis=AX.X, op=ALU.max, r=[rt], w=[rt])
                gmax = rt[:, 52:53]
                I("dve", "tensor_reduce", out=gmax, in_=gs, axis=AX.X, op=ALU.max, r=[rt], w=[rt])
                ing = rt[:, 56:60]
                I("dve", "tensor_scalar", out=ing, in0=gs, scalar1=gmax, scalar2=None, op0=ALU.is_ge, r=[rt], w=[rt])
                sel = rt[:, 32:48].rearrange("p (g k) -> p g k", k=4)
                I("dve", "tensor_tensor", out=sel, in0=b4, in1=thr.unsqueeze(2).to_broadcast([128, 4, 4]), op=ALU.is_ge, r=[rt], w=[rt])
                I("dve", "tensor_tensor", out=sel, in0=sel, in1=ing.unsqueeze(2).to_broadcast([128, 4, 4]), op=ALU.mult, r=[rt], w=[rt])
                g_ = gate[:, ti, :]
                I("dve", "tensor_tensor", out=g_, in0=rt[:, 32:48], in1=sc, op=ALU.mult, r=[rt], w=[gate])
                den = rt[:, 53:54]
                I("dve", "tensor_reduce", out=den, in_=g_, axis=AX.X, op=ALU.add, r=[gate], w=[rt])
                I("dve", "reciprocal", out=den, in_=den, r=[rt], w=[rt])
                I("dve", "tensor_scalar", out=g_, in0=g_, scalar1=den, scalar2=None, op0=ALU.mult, r=[gate, rt], w=[gate])
                I("dve", "tensor_copy", out=ghl[:, 0, :], in_=g_, r=[gate], w=[ghl])
                I("dve", "tensor_tensor", out=rt[:, 32:48], in0=g_, in1=ghl[:, 0, :], op=ALU.subtract, r=[gate, ghl], w=[rt])
                I("dve", "tensor_copy", out=ghl[:, 1, :], in_=rt[:, 32:48], r=[rt], w=[ghl])
                for q in range(2):
                    I("pe", "transpose", out=ps_t[:, q * 128:(q + 1) * 128], in_=ghl[:, q, :], identity=self.ident[:], r=[ghl, self.ident], w=[ps_t])
                I("act", "copy", out=gT[:, :, ti * 128:(ti + 1) * 128], in_=ps_t[:, 0:256].rearrange("p (q n) -> p q n", q=2), r=[ps_t], w=[gT])
            if "gate" in self.dbg and l == 0:
                S.dma("sp", self.outs["o_gate"].t[t0:t0 + n, :].rearrange("(t p) e -> p t e", p=128), gate[:, 0:nt, :], gate, r=[gate], w=[self.outs["o_gate"]])
            if not self.moe_on:
                continue
            for e in range(16):
                for q in range(2):
                    I("pe", "matmul", ps_r[:, 0:n], lhsT=selEb[:, e, :], rhs=gT[:, q, 0:n], start=(q == 0), stop=(q == 1), r=[selEb, gT], w=[ps_r])
                I("act", "copy", out=gbc[:, 0:n], in_=ps_r[:, 0:n], r=[ps_r], w=[gbc])
                for dc in range(8):
                    wa, wc = w13[wi % 3], w13[(wi + 1) % 3]
                    wi += 2
                    S.dma("sp", wa[:], self.moe_chunk("w1", l, e, dc).rearrange("(p c n) -> p c n", p=128, c=32), wa, r=[self.moe_g["w1"]], w=[wa])
                    S.dma("act", wc[:], self.moe_chunk("w3", l, e, dc).rearrange("(p c n) -> p c n", p=128, c=32), wc, r=[self.moe_g["w3"]], w=[wc])
                    pa, pb_ = ps_a[pi % 2], ps_b[pi % 2]
                    pi += 1
                    for c in range(32):
                        I("pe", "matmul", pa[:, 0:n], lhsT=wa[:, c, :], rhs=nb[:, c, 0:n], start=(c == 0), stop=(c == 31), r=[wa, nb], w=[pa])
                    for c in range(32):
                        I("pe", "matmul", pb_[:, 0:n], lhsT=wc[:, c, :], rhs=nb[:, c, 0:n], start=(c == 0), stop=(c == 31), r=[wc, nb], w=[pb_])
                    I("act", "activation", out=sa[:, 0:n], in_=pa[:, 0:n], func=AF.Silu, r=[pa], w=[sa])
                    I("dve", "tensor_tensor", out=sa[:, 0:n], in0=sa[:, 0:n], in1=pb_[:, 0:n], op=ALU.mult, r=[sa, pb_], w=[sa])
                    I("dve", "tensor_tensor", out=uT[:, dc, 0:n], in0=sa[:, 0:n], in1=gbc[:, 0:n], op=ALU.mult, r=[sa, gbc], w=[uT])
                for dcn in range(8):
                    w2 = w2b[w2i % 2]
                    w2i += 1
                    for dc in range(8):
                        S.dma("sp" if dc % 2 else "act", w2[:, dc, :],
                              self.moe_chunk("w2", l, e, dc).rearrange("(p n) -> p n", p=128)[:, dcn * 512:(dcn + 1) * 512], w2, r=[self.moe_g["w2"]], w=[w2])
                    for ti in range(nt):
                        py = ps_y[(dcn * 4 + ti) % 2]
                        for dc in range(8):
                            I("pe", "matmul", py[:, :], lhsT=uT[:, dc, ti * 128:(ti + 1) * 128], rhs=w2[:, dc, :], start=(dc == 0), stop=(dc == 7), r=[uT, w2], w=[py])
                        ya = yacc[:, ti, dcn * 512:(dcn + 1) * 512]
                        if e == 0:
                            I("act", "copy", out=ya, in_=py[:, :], r=[py], w=[yacc])
                        else:
                            I("dve", "tensor_tensor", out=ya, in0=ya, in1=py[:, :], op=ALU.add, r=[yacc, py], w=[yacc])
            for ti in range(nt):
                r0 = t0 + ti * 128
                I("dve", "tensor_tensor", out=yacc[:, ti, :], in0=yacc[:, ti, :], in1=gab[:], op=ALU.mult, r=[yacc, gab], w=[yacc])
                for dcn in range(8):
                    S.dma("sp", sa[:], h_mid.t[r0:r0 + 128, dcn * 512:(dcn + 1) * 512], sa, r=[h_mid], w=[sa])
                    I("dve", "tensor_tensor", out=yacc[:, ti, dcn * 512:(dcn + 1) * 512], in0=yacc[:, ti, dcn * 512:(dcn + 1) * 512], in1=sa[:], op=ALU.add, r=[yacc, sa], w=[yacc])
                if final_out is not None:
                    if r0 >= 128:
                        S.dma("pool", final_out.t[r0 - 128:r0, :], yacc[:, ti, :], yacc, r=[yacc], w=[final_out])
                else:
                    S.dma("pool", h_out.t[r0:r0 + 128, :], yacc[:, ti, :], yacc, r=[yacc], w=[h_out])
        S.pop()

    def build(self, NL=2, stages=("conv", "mla", "rwkv", "tok", "moe")):
        S = self.S
        self.NL = NL
        self.moe_on = "moe" in stages
        self.ext = {}
        self.consts()
        for nm, shp, dt in (("n1g", [128, 2, 32], F32), ("n2g", [128, 2, 32], F32), ("cw", [128, 2, 4, 31], F32), ("cb", [128, 2, 4, 3], F32),
                            ("mlag", [128, 2, 24], F32), ("ropeT", [64, 2, 4096], F32), ("rotm", [64, 64], F32),
                            ("mixidx", [128, 5, 32], I32), ("wrT", [128, 32, 16], F32), ("rbias", [128, 16], F32),
                            ("rwt", [64, 2, 8, 19], F32), ("rwl", [64, 2, 2, 2, 3], F32), ("rw2", [64, 2, 2, 2, 512], F32),
                            ("rmask", [64, 2, 192], F32), ("rg2", [128, 2, 2, 512], F32), ("rgl", [128, 2, 2, 3], F32)):
            self.ext[nm] = self.ext_in(nm, shp, dt)
        hx = self.ext_in("hx", [MT, D], F32)
        if "gate" in self.dbg:
            self.ext_out("o_gate", [MT, 16], F32)
        self.mod_stage()
        self.convert_win()
        self.convert_mla()
        if "tok" in stages:
            self.convert_big()
        nT_my = S.dram("nT_my", [D, MT], BF16)
        nT_all = S.dram("nT_all", [16, 2, 256, MT], BF16)
        n2T = S.dram("n2T", [D, MT], BF16)
        pT = S.dram("pT", [NCOL, T], F32)
        mixT = S.dram("mixT", [2, 5, 2048, 512], BF16)
        mix_all = S.dram("mix_all", [20, 2, 1024, 512], BF16)
        h_mid = S.dram("h_mid", [MT, D], F32)
        h_nxt = S.dram("h_nxt", [MT, D], F32)
        out = self.ext_out("out", [2048, D], F32) if "tok" in stages else None
        h_cur = hx
        for l in range(NL):
            S.lane = 2 * l
            self.norm_stage(l, h_cur, "n1g", (0, 1), nT_my)
            self.pair_gather(nT_my, nT_all, 16, 256)
            self.inproj_stage(l, nT_all, pT)
            self.gather_finish(1)
            if "conv" in stages:
                self.conv_stage(l, pT, mixT)
            self.gather_finish(1)
            if "rwkv" in stages:
                self.rwkv_stage(l, pT, mixT)
            self.gather_finish(1)
            if "mla" in stages:
                self.mla_stage(l, pT, mixT)
            self.gather_finish(9)
            if "tok" in stages:
                S.lane = 2 * l + 1
                self.pair_gather(mixT, mix_all, 20, 1024, src_ap=mixT.t.rearrange("h b f n -> (h b f) n"))
                self.outproj_stage(l, mix_all, h_cur, h_mid)
                self.norm_stage(l, h_mid, "n2g", (3, 4), n2T)
                self.moe_stage(l, n2T, h_mid, h_nxt, final_out=(out if l == NL - 1 else None))
                h_cur = h_nxt
        if "mixT" in self.dbg:
            o = self.ext_out("o_mixT", [2, 5, 2048, 512], BF16)
            S.dma("sp", o.t, mixT.t, Buf("dd3"), r=[mixT], w=[o])
        if "hmid" in self.dbg:
            o = self.ext_out("o_hmid", [MT, D], F32)
            S.dma("sp", o.t, h_mid.t, Buf("dd4"), r=[h_mid], w=[o])
        if "n2T" in self.dbg:
            o = self.ext_out("o_n2T", [D, MT], BF16)
            S.dma("sp", o.t, n2T.t, Buf("dd5"), r=[n2T], w=[o])
        return S.emit()


OFF_CONV, OFF_MQ, OFF_RR, OFF_RG, OFF_RK, OFF_RV, OFF_RW, OFF_RA, OFF_MKV, OFF_MKR = (
    0, 2048, 3584, 4608, 4768, 5792, 6816, 6944, 7072, 7584)


def my_cols(hf):
    cols = []
    cols += list(range(OFF_CONV + hf * 512, OFF_CONV + hf * 512 + 512))
    cols += list(range(OFF_CONV + 1024 + hf * 512, OFF_CONV + 1024 + hf * 512 + 512))
    cols += list(range(OFF_MQ, OFF_MQ + 1536))
    cols += list(range(OFF_RR + hf * 512, OFF_RR + hf * 512 + 512))
    cols += list(range(OFF_RK + hf * 512, OFF_RK + hf * 512 + 512))
    cols += list(range(OFF_RV + hf * 512, OFF_RV + hf * 512 + 512))
    cols += list(range(OFF_RW, OFF_RW + 128))
    cols += list(range(OFF_RA, OFF_RA + 128))
    cols += list(range(OFF_MKV, OFF_MKV + 512))
    cols += list(range(OFF_RG, OFF_RG + 160))
    cols += list(range(OFF_MKR, OFF_MKR + 64))
    return np.array(cols)


def _rope_tables():
    half = 32
    inv_freq = (10000.0 ** (-np.arange(0, half, 2, dtype=np.float32) / half)).astype(np.float32)
    r = np.repeat(np.arange(64, dtype=np.float32), 64)
    cl = np.tile(np.arange(64, dtype=np.float32), 64)
    ar = r[:, None] * inv_freq
    ac = cl[:, None] * inv_freq
    ang = np.concatenate([ar, ar, ac, ac], -1).astype(np.float32)
    return np.ascontiguousarray(np.stack([np.cos(ang).T, np.sin(ang).T], 1).astype(np.float32))


def _rot_matrix():
    R = np.zeros((64, 64), np.float32)
    for i in range(16):
        R[16 + i, i] = -1.0
        R[i, 16 + i] = 1.0
        R[48 + i, 32 + i] = -1.0
        R[32 + i, 48 + i] = 1.0
    return R


def mix_index_table(hf):
    idx = np.zeros((128, 5, 32), np.int32)
    for blk in range(5):
        for kc in range(32):
            rank, feat0 = kc // 16, (kc % 16) * 128
            R0 = (hf * 5 + blk) * 2048 + feat0
            idx[:, blk, kc] = ((R0 // 1024) * 2 + rank) * 1024 + R0 % 1024 + np.arange(128)
    return idx


MIX_PERM = np.concatenate([np.concatenate([r * 512 + np.arange(512), 1024 + r * 512 + np.arange(512), 2048 + r * 1024 + np.arange(1024)])
                           for r in range(2)])
ROPE_T = _rope_tables()
ROT_M = _rot_matrix()


def pack_inputs(inp, core, NL=2):
    b, hf = core // 2, core % 2
    f = np.float32
    m = {}
    m["hx"] = np.ascontiguousarray(np.concatenate([inp["ctx"][b, hf * 128:(hf + 1) * 128], inp["x"][b, hf * 2048:(hf + 1) * 2048]], 0))
    cv = np.concatenate([inp["c"], inp["c_ctx"][None]], 0)
    m["cvT"] = np.ascontiguousarray(cv.reshape(5, 32, 128).transpose(2, 1, 0))
    m["wmod"] = np.ascontiguousarray(inp["w_mod"][:NL, :, core * 3072:(core + 1) * 3072])
    m["bmodT"] = np.ascontiguousarray(inp["b_mod"][:, core * 3072:(core + 1) * 3072].reshape(2, 24, 128).transpose(2, 0, 1))
    sel = np.zeros((128, 2, 5), f)
    sel[:, 0, b] = 1.0
    sel[:, 1, 4] = 1.0
    m["sel"] = sel
    m["c_ident"] = np.eye(128, dtype=f)
    m["n1g"] = np.ascontiguousarray(inp["norm1_g"].reshape(2, 32, 128).transpose(2, 0, 1))
    m["win"] = np.ascontiguousarray(inp["w_in"][:NL, :, my_cols(hf)])
    ch = hf * 512 + np.arange(512)
    m["cw"] = np.ascontiguousarray(inp["conv_dw"][:, :, ch].reshape(2, 31, 4, 128).transpose(3, 0, 2, 1))
    cb = np.stack([inp["conv_b"][:, ch], inp["conv_ln_g"][:, ch], inp["conv_ln_b"][:, ch]], -1)
    m["cb"] = np.ascontiguousarray(cb.reshape(2, 4, 128, 3).transpose(2, 0, 1, 3))
    mg = np.zeros((128, 2, 24), f)
    mg[:, :, 0:12] = inp["m_cq_g"].reshape(2, 12, 128).transpose(2, 0, 1)
    mg[:, :, 12:16] = inp["m_ckv_g"].reshape(2, 4, 128).transpose(2, 0, 1)
    mg[:, :, 16] = inp["m_qn_g"].T
    mg[:64, :, 17] = inp["m_qr_g"].T
    mg[:, :, 18] = inp["m_kn_g"].T
    mg[:64, :, 19] = inp["m_kr_g"].T
    m["mlag"] = mg
    m["ropeT"] = ROPE_T
    m["rotm"] = ROT_M
    heads = hf * 8 + np.arange(8)
    qc = (heads[:, None] * 192 + np.arange(192)[None]).reshape(-1)
    kc = (heads[:, None] * 256 + np.arange(256)[None]).reshape(-1)
    m["n2g"] = np.ascontiguousarray(inp["norm2_g"].reshape(2, 32, 128).transpose(2, 0, 1))
    chn = hf * 512 + np.arange(512)
    mu = inp["r_mu"]

    def hi(a):
        return a.reshape(2, 8, 64).transpose(2, 0, 1)
    rwt = np.zeros((64, 2, 8, 19), f)
    for qi, off in enumerate((OFF_RR, OFF_RK, OFF_RV)):
        mp = mu[:, 0, off - OFF_RR + chn]
        mn = mu[:, 1, off - OFF_RR + chn]
        rwt[..., qi * 3 + 0] = hi(1.0 - mp - mn)
        rwt[..., qi * 3 + 1] = hi(mp)
        rwt[..., qi * 3 + 2] = hi(mn)
    rwt[..., 9] = hi(inp["r_kk"][:, chn])
    rwt[..., 10] = hi(inp["r_ka"][:, chn])
    rwt[..., 11] = hi(1.0 - inp["r_ka"][:, chn])
    rwt[..., 12] = hi(inp["r_rk"].reshape(2, 1024)[:, chn])
    rwt[..., 13] = hi(inp["r_ln_g"][:, chn])
    rwt[..., 14] = hi(inp["r_ln_b"][:, chn])
    for dd in range(2):
        rwt[..., 15 + dd] = hi(inp["r_w0"][:, dd, chn])
        rwt[..., 17 + dd] = hi(inp["r_a0"][:, dd, chn])
    m["rwt"] = rwt
    rwl = np.zeros((64, 2, 2, 2, 3), f)
    for dd in range(2):
        for qi, off in enumerate((OFF_RW, OFF_RA)):
            cc = off - OFF_RR + dd * 64 + np.arange(64)
            mp, mn = mu[:, 0, cc], mu[:, 1, cc]
            rwl[:, :, dd, qi, 0] = (1.0 - mp - mn).T
            rwl[:, :, dd, qi, 1] = mp.T
            rwl[:, :, dd, qi, 2] = mn.T
    m["rwl"] = rwl
    rw2 = np.zeros((64, 2, 2, 2, 512), f)
    rw2[:, :, :, 0, :] = inp["r_w2"][:, :, :, chn].transpose(2, 0, 1, 3)
    rw2[:, :, :, 1, :] = inp["r_a2"][:, :, :, chn].transpose(2, 0, 1, 3)
    m["rw2"] = rw2
    tri = np.arange(64)
    up_s = (tri[:, None] < tri[None, :]).astype(f)
    up_i = (tri[:, None] <= tri[None, :]).astype(f)
    rmask = np.zeros((64, 2, 192), f)
    rmask[:, 0, 0:64], rmask[:, 0, 64:128], rmask[:, 0, 128:192] = up_s, up_i, up_s.T
    rmask[:, 1, 0:64], rmask[:, 1, 64:128], rmask[:, 1, 128:192] = up_s.T, up_i.T, up_s
    m["rmask"] = rmask
    rg2 = np.zeros((128, 2, 2, 512), f)
    rg2[:, :, 0, :] = inp["r_g2"][:, 0:128, :][:, :, chn].transpose(1, 0, 2)
    rg2[0:32, :, 1, :] = inp["r_g2"][:, 128:160, :][:, :, chn].transpose(1, 0, 2)
    m["rg2"] = rg2
    rgl = np.zeros((128, 2, 2, 3), f)
    cg = OFF_RG - OFF_RR + np.arange(160)
    mp, mn = mu[:, 0, cg], mu[:, 1, cg]
    for qi, (a_, b_, n_) in enumerate(((0, 128, 128), (128, 160, 32))):
        rgl[0:n_, :, qi, 0] = (1.0 - mp[:, a_:b_] - mn[:, a_:b_]).T
        rgl[0:n_, :, qi, 1] = mp[:, a_:b_].T
        rgl[0:n_, :, qi, 2] = mn[:, a_:b_].T
    m["rgl"] = rgl
    m["mixidx"] = mix_index_table(hf)
    m["wrT"] = np.ascontiguousarray(inp["w_router"].reshape(32, 128, 16).transpose(1, 0, 2))
    m["rbias"] = np.ascontiguousarray(np.broadcast_to(inp["router_bias"][None, :], (128, 16))).astype(f)
    if "w_out" in inp:
        wo = inp["w_out"][:NL][:, MIX_PERM, :].reshape(NL * 4096, 4096)
        m["wo"] = np.ascontiguousarray(wo[core * 512 * NL:(core + 1) * 512 * NL])
    if "moe_w1" in inp:
        nle = 2 * NL
        for nm in ("w1", "w3", "w2"):
            w = inp["moe_" + nm][:NL]
            w = w.reshape((NL * 16,) + w.shape[2:])
            m["m" + nm] = np.ascontiguousarray(w[core * nle:(core + 1) * nle])
    m["wuq"] = np.ascontiguousarray(inp["m_w_uq"][:NL, :, qc])
    m["wukv"] = np.ascontiguousarray(inp["m_w_ukv"][:NL, :, kc])
    return m


def kernel(**inputs):
    inp = {k: np.asarray(v) for k, v in inputs.items()}
    nc = bass.Bass("TRN2", target_bir_lowering=False)
    k = K(nc)
    k.build(NL=2)
    need = None
    maps = []
    for c in range(8):
        m = pack_inputs(inp, c, 2)
        maps.append(m)
    res = run_bass_kernel_spmd(nc, maps, core_ids=list(range(8)))
    out = np.zeros((4, 4096, 4096), np.float32)
    for c in range(8):
        b, hf = c // 2, c % 2
        out[b, hf * 2048:(hf + 1) * 2048] = res.results[c]["out"]
    return out
```

```python
import contextlib
import numpy as np
import ml_dtypes
import concourse.bass as bass
import concourse.mybir as mybir
from concourse.bass_utils import run_bass_kernel_spmd

F32 = mybir.dt.float32
BF16 = mybir.dt.bfloat16
I32 = mybir.dt.int32
AF = mybir.ActivationFunctionType
ALU = mybir.AluOpType
AX = mybir.AxisListType

ENGS = ("pe", "act", "dve", "pool", "sp")

D = 4096
T = 4352
MT = 2176
LC = 256
LL = 4096
NCOL = 5088
COLS = {}
_off = 0
for _n, _s in (("ca", 512), ("cg", 512), ("mq", 1536), ("rr", 512), ("rk", 512), ("rv", 512), ("rw", 128),
               ("ra", 128), ("mkv", 512), ("rg", 160), ("mkr", 64)):
    COLS[_n] = (_off, _s)
    _off += _s
assert _off == NCOL
CHUNKS = [(i * 128, 128) for i in range(39)] + [(4992, 32), (5024, 64)]
PAIRS = [[0, 1], [2, 3], [4, 5], [6, 7]]
QUADS = [[0, 1, 2, 3], [4, 5, 6, 7]]
P4 = [[0, 4], [1, 5], [2, 6], [3, 7]]


import os
RW_LEVEL = int(os.environ.get("RW_LEVEL", "9"))
RW_NBLK = int(os.environ.get("RW_NBLK", "99"))
RW_BAR = int(os.environ.get("RW_BAR", "0"))
RW_NFIN = int(os.environ.get("RW_NFIN", "99"))
RW_NLEV = int(os.environ.get("RW_NLEV", "6"))


class Buf:
    __slots__ = ("name", "lw", "rd", "dsem", "dcount", "t", "persist")

    def __init__(self, name, t=None):
        self.name = name
        self.t = t
        self.lw = []
        self.rd = []
        self.dsem = None
        self.dcount = 0
        self.persist = False

    def __getitem__(self, k):
        return self.t[k]


class Sched:
    def __init__(self, nc):
        self.nc = nc
        self.ops = {e: [] for e in ENGS}
        self.ndsem = 0
        self.stacks = [contextlib.ExitStack()]
        self.n_tiles = 0
        self.dbufs = []
        self.free_sems = []
        self.scope_sems = [[]]
        self.lane = 0

    def sb(self, shape, dtype, name="t"):
        self.n_tiles += 1
        t = self.stacks[-1].enter_context(self.nc.sbuf_tensor(f"{name}_{self.n_tiles}", list(shape), dtype))
        return Buf(name, t)

    def ps(self, shape, dtype, name="p"):
        self.n_tiles += 1
        t = self.stacks[-1].enter_context(self.nc.psum_tensor(f"{name}_{self.n_tiles}", list(shape), dtype))
        return Buf(name, t)

    def dram(self, name, shape, dtype, kind="Internal"):
        t = self.nc.dram_tensor(name, list(shape), dtype, kind=kind).ap()
        return Buf(name, t)

    def push(self):
        self.stacks.append(contextlib.ExitStack())
        self.scope_sems.append([])

    def pop(self):
        self.barrier()
        self.stacks.pop().close()
        for b in self.scope_sems.pop():
            if b.dsem is not None:
                self.free_sems.append((b.dsem, b.dcount))
                self.dbufs.remove(b)
                b.dsem = None

    def _events_for(self, r, w):
        ev = []
        for b in r:
            ev.extend(b.lw)
        for b in w:
            ev.extend(b.lw)
            ev.extend(b.rd)
        return ev

    def op(self, eng, fn, r=(), w=()):
        idx = len(self.ops[eng])
        ev = self._events_for(r, w)
        self.ops[eng].append({"fn": fn, "ev": ev, "dma": None, "lane": self.lane})
        me = ("e", eng, idx)
        for b in r:
            b.rd = [e for e in b.rd if not (e[0] == "e" and e[1] == eng)] + [me]
        for b in w:
            b.lw = [me]
            b.rd = []
        return idx

    def i(self, eng, meth, *a, r=(), w=(), **kw):
        return self.op(eng, lambda e: getattr(e, meth)(*a, **kw), r=r, w=w)

    def _async(self, eng, fn, sem_buf, inc, r, w):
        idx = len(self.ops[eng])
        ev = self._events_for(r, w)
        b = sem_buf
        if b.dsem is None:
            if self.free_sems and self.ndsem >= 72:
                j = min(range(len(self.free_sems)), key=lambda q: self.free_sems[q][1])
                b.dsem, b.dcount = self.free_sems.pop(j)
            else:
                b.dsem = self.ndsem
                self.ndsem += 1
                b.dcount = 0
            self.dbufs.append(b)
            self.scope_sems[0 if b.persist else -1].append(b)
        b.dcount += inc
        me = ("d", b.dsem, b.dcount)
        self.ops[eng].append({"fn": fn, "ev": ev, "dma": b.dsem, "inc": inc, "lane": self.lane})
        for x in r:
            x.rd = [e for e in x.rd if not (e[0] == "d" and e[1] == b.dsem)] + [me]
        for x in w:
            x.lw = [me]
            x.rd = []
        return idx

    def dma(self, eng, out, in_, sem_buf, r=(), w=(), **kw):
        return self._async(eng, lambda e: e.dma_start(out=out, in_=in_, **kw), sem_buf, 16, r, w)

    def gather_rows(self, out, in_, idx_ap, sem_buf, r=(), w=()):
        return self._async("pool", lambda e: e.indirect_dma_start(
            out=out, out_offset=None, in_=in_, in_offset=bass.IndirectOffsetOnAxis(ap=idx_ap, axis=0)),
            sem_buf, 16, r, w)

    def coll(self, kind, ins_ap, outs_ap, groups, sem_buf, r=(), w=()):
        return self._async("pool", lambda e: e.collective_compute(
            kind, ALU.bypass, replica_groups=groups, ins=[ins_ap], outs=[outs_ap]), sem_buf, 1, r, w)

    def barrier(self, final=False):
        ev = []
        for e in ENGS:
            for i in range(len(self.ops[e]) - 1, -1, -1):
                o = self.ops[e][i]
                if o["fn"] is not None and o["dma"] is None:
                    ev.append(("e", e, i))
                    break
        for b in self.dbufs:
            if b.persist and not final:
                continue
            ev.append(("d", b.dsem, b.dcount))
        for e in ENGS:
            self.ops[e].append({"fn": None, "ev": list(ev), "dma": None, "lane": self.lane})

    def emit(self):
        nc = self.nc
        self.barrier(final=True)
        NLANE = 4
        marked = {e: set() for e in ENGS}
        lane_of = {e: [o["lane"] % NLANE for o in self.ops[e]] for e in ENGS}
        for eng in ENGS:
            seen_e = {}
            seen_d = {}
            for i, o in enumerate(self.ops[eng]):
                need_e = {}
                need_d = {}
                for ev in o["ev"]:
                    if ev[0] == "e":
                        _, e2, i2 = ev
                        if e2 == eng and eng == "pe":
                            continue
                        key = (e2, lane_of[e2][i2])
                        if seen_e.get(key, -1) >= i2:
                            continue
                        need_e[key] = max(need_e.get(key, -1), i2)
                    else:
                        _, s, c = ev
                        if seen_d.get(s, 0) >= c:
                            continue
                        need_d[s] = max(need_d.get(s, 0), c)
                for key, i2 in need_e.items():
                    seen_e[key] = i2
                    marked[key[0]].add(i2)
                for s, c in need_d.items():
                    seen_d[s] = c
                o["need_e"] = need_e
                o["need_d"] = need_d
        cum = {}
        for eng in ENGS:
            c = [0] * NLANE
            arr = []
            for i in range(len(self.ops[eng])):
                ln = lane_of[eng][i]
                if i in marked[eng]:
                    c[ln] += 1
                arr.append(c[ln])
            cum[eng] = arr
        with contextlib.ExitStack() as st:
            esem = {(e, ln): st.enter_context(nc.semaphore(f"es_{e}{ln}")) for e in ENGS for ln in range(NLANE)}
            dsem = [st.enter_context(nc.semaphore(f"ds_{i}")) for i in range(self.ndsem)]
            block = st.enter_context(nc.Block())

            def body(eng):
                def run(engine):
                    for i, o in enumerate(self.ops[eng]):
                        for (e2, ln), i2 in o["need_e"].items():
                            engine.wait_ge(esem[(e2, ln)], cum[e2][i2])
                        for s, c in o["need_d"].items():
                            engine.wait_ge(dsem[s], c)
                        if o["fn"] is None:
                            continue
                        ins = o["fn"](engine)
                        if o["dma"] is not None:
                            ins.then_inc(dsem[o["dma"]], o["inc"])
                        elif i in marked[eng]:
                            ins.then_inc(esem[(eng, lane_of[eng][i])], 1)
                return run

            block.tensor(body("pe"))
            block.scalar(body("act"))
            block.vector(body("dve"))
            block.gpsimd(body("pool"))
            block.sync(body("sp"))
        while self.stacks:
            self.stacks.pop().close()
        return {e: len(self.ops[e]) for e in ENGS}


class _View:
    def __init__(self, buf, ap):
        self.buf = buf
        self.ap = ap


def xin_view(xin, n):
    return xin


class RR:
    def __init__(self, items):
        self.items = list(items)
        self.i = 0

    def __call__(self):
        x = self.items[self.i % len(self.items)]
        self.i += 1
        return x


def copy_on(S, eng, out_ap, in_ap, r, w):
    if eng == "act":
        S.op("act", lambda e: e.copy(out=out_ap, in_=in_ap), r=r, w=w)
    else:
        S.op(eng, lambda e: e.tensor_copy(out=out_ap, in_=in_ap), r=r, w=w)


class K:
    def __init__(self, nc, dbg=()):
        self.nc = nc
        self.S = Sched(nc)
        self.dbg = set(dbg)
        self.outs = {}
        self.pending_g = []

    def ext_in(self, name, shape, dtype):
        return Buf(name, self.nc.dram_tensor(name, list(shape), dtype, kind="ExternalInput").ap())

    def ext_out(self, name, shape, dtype):
        b = Buf(name, self.nc.dram_tensor(name, list(shape), dtype, kind="ExternalOutput").ap())
        self.outs[name] = b
        return b

    def consts(self):
        S = self.S
        self.ident = S.sb([128, 128], BF16, "ident")
        identf = S.sb([128, 128], F32, "identf")
        self.ones_bf = S.sb([128, 128], BF16, "ones")
        cid = self.ext_in("c_ident", [128, 128], F32)
        S.dma("sp", identf[:], cid.t, identf, w=[identf])
        S.op("dve", lambda e: e.tensor_copy(out=self.ident[:], in_=identf[:]), r=[identf], w=[self.ident])
        S.op("dve", lambda e: e.memset(self.ones_bf[:], 1.0), w=[self.ones_bf])
        self.identf = identf

    def gather8(self, src, rows, cols, dtype, name):
        S = self.S
        mid = S.dram(name + "_q", [4 * rows, cols], dtype)
        dst = S.dram(name + "_g", [8 * rows, cols], dtype)
        sem = Buf(name + "_sem")
        S.coll("AllGather", src.t, mid.t, QUADS, sem, r=[src], w=[mid])
        S.coll("AllGather", mid.t, dst.t, P4, sem, r=[mid], w=[dst])
        return dst

    def pair_gather(self, src, dst, nch, rows, src_ap=None):
        sem = Buf(src.name + "_pgsem")
        ap = src.t if src_ap is None else src_ap
        for i in range(nch):
            self.S.coll("AllGather", ap[i * rows:(i + 1) * rows, :], dst.t[i].rearrange("r p n -> (r p) n"), PAIRS, sem,
                        r=[src], w=[dst])

    def mod_stage(self):
        S = self.S
        cvT = self.ext_in("cvT", [128, 32, 5], F32)
        wmod = self.ext_in("wmod", [self.NL, D, 3072], F32)
        bmodT = self.ext_in("bmodT", [128, 2, 24], F32)
        sel = self.ext_in("sel", [128, 2, 5], F32)
        S.push()
        cv = S.sb([128, 32, 5], F32, "cv")
        cvb = S.sb([128, 32, 5], BF16, "cvb")
        bm = S.sb([128, 2, 24], F32, "bm")
        part = S.sb([128, 2, 24, 5], F32, "modpart")
        S.dma("sp", cv[:], cvT.t, cv, w=[cv])
        S.dma("sp", bm[:], bmodT.t, bm, w=[bm])
        S.op("act", lambda e: e.activation(out=cvb[:], in_=cv[:], func=AF.Silu), r=[cv], w=[cvb])
        wf = [S.sb([128, 32, 128], F32, "wf") for _ in range(2)]
        wb = [S.sb([128, 32, 128], BF16, "wb") for _ in range(2)]
        pp = [S.ps([128, 8], F32, "modps") for _ in range(2)]
        cv_eng = RR(["dve", "pool", "act"])
        it = 0
        S.op("dve", lambda e: e.memset(part[:], 0.0), w=[part])
        for l in range(self.NL):
            for lc in range(24):
                f = wf[it % 2]
                b = wb[it % 2]
                p = pp[it % 2]
                src = wmod.t[l, :, lc * 128:(lc + 1) * 128].rearrange("(c p) n -> p c n", p=128)
                S.dma("sp" if it % 2 == 0 else "act", f[:], src, f, w=[f])
                for q in range(4):
                    copy_on(S, cv_eng(), b[:, q * 8:(q + 1) * 8, :], f[:, q * 8:(q + 1) * 8, :], [f], [b])
                for c in range(32):
                    S.op("pe", lambda e, p=p, b=b, c=c: e.matmul(p[:, 0:5], lhsT=b[:, c, :], rhs=cvb[:, c, :],
                                                               start=(c == 0), stop=(c == 31)), r=[b, cvb], w=[p])
                S.op("dve", lambda e, p=p, l=l, lc=lc: e.tensor_scalar(
                    out=part[:, l, lc, :], in0=p[:, 0:5], scalar1=bm[:, l, lc:lc + 1], scalar2=None, op0=ALU.add),
                    r=[p, bm], w=[part])
                it += 1
        msrc = S.dram("modsrc", [128, 240], F32)
        S.dma("pool", msrc.t, part[:].rearrange("p l c v -> p (l c v)"), part, r=[part], w=[msrc])
        mall = self.gather8(msrc, 128, 240, F32, "modg")
        S.pop()
        tab = S.sb([128, 8, 2, 24, 5], F32, "modtab")
        selt = S.sb([128, 2, 5], F32, "selt")
        S.dma("sp", tab[:].rearrange("p r l c v -> p r (l c v)"), mall.t.rearrange("(r p) n -> p r n", p=128), tab,
              r=[mall], w=[tab])
        S.dma("sp", selt[:], sel.t, selt, w=[selt])
        self.mods = [S.sb([128, 2, 192], F32, f"modsel{w_}") for w_ in range(2)]
        S.push()
        tmp = S.sb([128, 8 * 2 * 24, 5], F32, "modtmp")
        for wsel in range(2):
            m = self.mods[wsel]
            S.op("dve", lambda e, wsel=wsel: e.tensor_tensor(
                out=tmp[:], in0=tab[:].rearrange("p r l c v -> p (r l c) v"),
                in1=selt[:, wsel:wsel + 1, :].to_broadcast([128, 384, 5]), op=ALU.mult), r=[tab, selt], w=[tmp])
            S.op("dve", lambda e, m=m: e.tensor_reduce(
                out=m[:].rearrange("p l (r c) -> p r l c", r=8),
                in_=tmp[:].rearrange("p (r l c) v -> p r l c v", r=8, l=2), axis=AX.X, op=ALU.add), r=[tmp], w=[m])
        S.pop()

    def modcol(self, which, l, k, c):
        return self.mods[which][:, l, k * 32 + c:k * 32 + c + 1]

    def convert_win(self):
        S = self.S
        self.win = self.ext_in("win", [self.NL, D, NCOL], F32)
        self.winb = S.dram("winb", [self.NL, 41, 128, 32, 128], BF16)
        S.push()
        wf = [S.sb([128, NCOL], F32, "cwf") for _ in range(2)]
        wb = [S.sb([128, NCOL], BF16, "cwb") for _ in range(2)]
        eng = RR(["dve", "pool", "act"])
        it = 0
        for l in range(self.NL):
            for c in range(32):
                f, b = wf[it % 2], wb[it % 2]
                S.dma("sp", f[:], self.win.t[l, c * 128:(c + 1) * 128, :], f, w=[f])
                for q in range(6):
                    copy_on(S, eng(), b[:, q * 848:(q + 1) * 848], f[:, q * 848:(q + 1) * 848], [f], [b])
                S.dma("pool", self.winb.t[l, 0:39, :, c, :].rearrange("j p n -> p j n"),
                      b[:, 0:4992].rearrange("p (j n) -> p j n", n=128), b, r=[b], w=[self.winb])
                S.dma("pool", self.winb.t[l, 39, :, c, 0:32], b[:, 4992:5024], b, r=[b], w=[self.winb])
                S.dma("pool", self.winb.t[l, 40, :, c, 0:64], b[:, 5024:5088], b, r=[b], w=[self.winb])
                it += 1
        S.pop()

    def norm_stage(self, l, h_src, gname, ktypes, out_nT):
        S = self.S
        k_sh, k_sc = ktypes
        S.push()
        g = S.sb([128, 2, 32], F32, "ng")
        S.dma("sp", g[:], self.ext[gname].t, g, w=[g])
        gm = [S.sb([128, 32], F32, "gm") for _ in range(2)]
        for w_ in range(2):
            S.op("dve", lambda e, w_=w_: e.scalar_tensor_tensor(
                out=gm[w_][:], in0=self.mods[w_][:, l, k_sc * 32:(k_sc + 1) * 32], scalar=1.0, in1=g[:, l, :],
                op0=ALU.add, op1=ALU.mult), r=[self.mods[w_], g], w=[gm[w_]])
        hb = [S.sb([128, D], F32, "hb") for _ in range(2)]
        junk = S.sb([128, D], BF16, "junk")
        hs = [S.sb([128, D], BF16, "hs") for _ in range(2)]
        st = [S.sb([128, 4], F32, "nst") for _ in range(2)]
        tp = [S.ps([128, 1024], BF16, "ntp") for _ in range(4)]
        nblk = [S.sb([128, 32, 512], BF16, "nblk") for _ in range(2)]
        ev = RR(["dve", "act"])
        tiles = [(0, 1, 0)] + [(128 + 512 * q, 4, 0) for q in range(4)]
        tiles[0] = (0, 1, 1)
        tcount = 0
        for bi, (t0, nt, which) in enumerate(tiles):
            nb = nblk[bi % 2]
            for ti in range(nt):
                h = hb[tcount % 2]
                hsb = hs[tcount % 2]
                s_ = st[tcount % 2]
                r0 = t0 + ti * 128
                S.dma("sp", h[:], h_src.t[r0:r0 + 128, :], h, r=[h_src], w=[h])
                S.op("act", lambda e, h=h, s_=s_: e.activation(out=junk[:], in_=h[:], func=AF.Square, accum_out=s_[:, 0:1]),
                     r=[h], w=[junk, s_])
                S.op("dve", lambda e, s_=s_: e.tensor_scalar(out=s_[:, 1:2], in0=s_[:, 0:1], scalar1=1.0 / D, scalar2=1e-6,
                                                            op0=ALU.mult, op1=ALU.add), r=[s_], w=[s_])
                S.op("act", lambda e, s_=s_: e.activation(out=s_[:, 2:3], in_=s_[:, 1:2], func=AF.Sqrt), r=[s_], w=[s_])
                S.op("dve", lambda e, s_=s_: e.reciprocal(out=s_[:, 3:4], in_=s_[:, 2:3]), r=[s_], w=[s_])
                S.op("act", lambda e, h=h, hsb=hsb, s_=s_: e.activation(out=hsb[:], in_=h[:], func=AF.Identity, scale=s_[:, 3:4]),
                     r=[h, s_], w=[hsb])
                for q in range(4):
                    p = tp[q]
                    for j in range(8):
                        c = q * 8 + j
                        S.op("pe", lambda e, p=p, hsb=hsb, c=c, j=j: e.transpose(
                            out=p[:, j * 128:(j + 1) * 128], in_=hsb[:, c * 128:(c + 1) * 128], identity=self.ident[:]),
                            r=[hsb, self.ident], w=[p])
                    for j in range(8):
                        c = q * 8 + j
                        en = ev()
                        o_ap = nb[:, c, ti * 128:(ti + 1) * 128]
                        i_ap = p[:, j * 128:(j + 1) * 128]
                        sc_ap = gm[which][:, c:c + 1]
                        sh_ap = self.modcol(which, l, k_sh, c)
                        if en == "dve":
                            S.op("dve", lambda e, o_ap=o_ap, i_ap=i_ap, sc_ap=sc_ap, sh_ap=sh_ap: e.tensor_scalar(
                                out=o_ap, in0=i_ap, scalar1=sc_ap, scalar2=sh_ap, op0=ALU.mult, op1=ALU.add),
                                r=[p, gm[which], self.mods[which]], w=[nb])
                        else:
                            S.op("act", lambda e, o_ap=o_ap, i_ap=i_ap, sc_ap=sc_ap, sh_ap=sh_ap: e.activation(
                                out=o_ap, in_=i_ap, func=AF.Identity, scale=sc_ap, bias=sh_ap),
                                r=[p, gm[which], self.mods[which]], w=[nb])
                tcount += 1
            n = nt * 128
            S.dma("pool", out_nT.t[:, t0:t0 + n].rearrange("(c p) n -> p c n", p=128), nb[:, :, 0:n], nb,
                  r=[nb], w=[out_nT])
        S.pop()

    def inproj_stage(self, l, nT_all, pT):
        S = self.S
        S.push()
        nt = S.sb([128, 32, 1024], BF16, "pnT")
        wr = [S.sb([128, 32, 128], BF16, "pw") for _ in range(3)]
        pp = [S.ps([128, 512], F32, "pps") for _ in range(4)]
        ob = [S.sb([128, 512], F32, "pob") for _ in range(4)]
        ev = RR(["act", "dve"])
        sbs = [(0, [(0, 0, 128), (1, 0, 128)])]
        for q in range(4):
            sbs.append((256 + q * 1024, [(q // 2, 128 + (q % 2) * 1024, 1024)]))
        wi = 0
        oi = 0
        for s0, parts in sbs:
            off = 0
            for (rk, o, n) in parts:
                for ci in range(2):
                    S.dma("sp", nt[:, :, off:off + n].rearrange("p (ch ci) n -> p ch ci n", ci=2)[:, :, ci, :],
                          nT_all.t[:, rk, ci * 128:(ci + 1) * 128, o:o + n].rearrange("ch p n -> p ch n"), nt,
                          r=[nT_all], w=[nt])
                off += n
            ntok = off
            for j, (c0, m) in enumerate(CHUNKS):
                w = wr[wi % 3]
                wi += 1
                S.dma("act" if wi % 2 else "sp", w[:, :, 0:m], self.winb.t[l, j, :, :, 0:m], w, r=[self.winb], w=[w])
                for hf in range((ntok + 511) // 512):
                    nn = min(512, ntok - hf * 512)
                    p = pp[oi % 4]
                    o_ = ob[oi % 4]
                    oi += 1
                    for c in range(32):
                        S.op("pe", lambda e, p=p, w=w, c=c, m=m, hf=hf, nn=nn: e.matmul(
                            p[0:m, 0:nn], lhsT=w[:, c, 0:m], rhs=nt[:, c, hf * 512:hf * 512 + nn],
                            start=(c == 0), stop=(c == 31)), r=[w, nt], w=[p])
                    copy_on(S, ev(), o_[0:m, 0:nn], p[0:m, 0:nn], [p], [o_])
                    S.dma("pool", pT.t[c0:c0 + m, s0 + hf * 512:s0 + hf * 512 + nn], o_[0:m, 0:nn], o_, r=[o_], w=[pT])
        S.pop()


    def mix_dst(self, mixT, row0, nrows, s0, n):
        out = []
        segs = [(0, 128, 0, 0), (128, 256, 1, 0)]
        for half in range(2):
            for bq in range(4):
                a0 = 256 + half * 2048 + bq * 512
                segs.append((a0, a0 + 512, half, 1 + bq))
        for (a, b, half, blk) in segs:
            lo, hi = max(a, s0), min(b, s0 + n)
            if lo < hi:
                out.append((mixT.t[half, blk, row0:row0 + nrows, lo - a:hi - a], lo - s0, hi - lo))
        return out

    def bcast_row(self, ps_ap, row_hi, row_lo, n, r):
        S = self.S
        S.op("pe", lambda e: e.matmul(ps_ap, lhsT=self.ones_bf[0:1, :], rhs=row_hi, start=True, stop=False), r=r + [self.ones_bf], w=[])
        return None

    def conv_stage(self, l, pT, mixT):
        S = self.S
        NP = 4397
        NA = 4367
        S.push()
        cw = S.sb([128, 2, 4, 31], F32, "cw")
        cb = S.sb([128, 2, 4, 3], F32, "cb")
        S.dma("sp", cw[:], self.ext["cw"].t, cw, w=[cw])
        S.dma("sp", cb[:], self.ext["cb"].t, cb, w=[cb])
        acc = S.sb([128, 4, NA], F32, "cacc")
        xb = S.sb([128, 2, 512], BF16, "cxb")
        ca0, cg0 = COLS["ca"][0], COLS["cg"][0]
        S.push()
        hp = S.sb([128, NP], F32, "chp")
        gt = S.sb([128, T], F32, "cgt")
        S.op("pool", lambda e: e.memset(hp[:], 0.0), w=[hp])
        for cc in range(4):
            S.dma("sp", gt[:], pT.t[cg0 + cc * 128:cg0 + (cc + 1) * 128, :], gt, r=[pT], w=[gt])
            S.op("act", lambda e: e.activation(out=gt[:], in_=gt[:], func=AF.Sigmoid), r=[gt], w=[gt])
            S.dma("act", hp[:, 15:271], pT.t[ca0 + cc * 128:ca0 + (cc + 1) * 128, 0:256], hp, r=[pT], w=[hp])
            S.dma("act", hp[:, 286:4382], pT.t[ca0 + cc * 128:ca0 + (cc + 1) * 128, 256:T], hp, r=[pT], w=[hp])
            S.op("dve", lambda e: e.tensor_tensor(out=hp[:, 15:271], in0=hp[:, 15:271], in1=gt[:, 0:256], op=ALU.mult), r=[hp, gt], w=[hp])
            S.op("dve", lambda e: e.tensor_tensor(out=hp[:, 286:4382], in0=hp[:, 286:4382], in1=gt[:, 256:T], op=ALU.mult), r=[hp, gt], w=[hp])
            S.op("dve", lambda e, cc=cc: e.tensor_scalar(out=acc[:, cc, :], in0=hp[:, 0:NA], scalar1=cw[:, l, cc, 0:1],
                                                          scalar2=cb[:, l, cc, 0:1], op0=ALU.mult, op1=ALU.add), r=[hp, cw, cb], w=[acc])
            for j in range(1, 31):
                S.op("dve", lambda e, cc=cc, j=j: e.scalar_tensor_tensor(
                    out=acc[:, cc, :], in0=hp[:, j:j + NA], scalar=cw[:, l, cc, j:j + 1], in1=acc[:, cc, :],
                    op0=ALU.mult, op1=ALU.add), r=[hp, cw, acc], w=[acc])
        S.pop()
        strow = [S.sb([1, 2, 512], F32, "cstrow") for _ in range(2)]
        pst = [S.ps([128, 512], F32, "cps") for _ in range(4)]
        nblk = (NA + 511) // 512
        cst = S.dram(f"cst{l}", [2, NA], F32)
        cstg = S.dram(f"cstg{l}", [4, NA], F32)
        for bi in range(nblk):
            c0 = bi * 512
            n = min(512, NA - c0)
            p1, p2 = pst[(bi % 2) * 2], pst[(bi % 2) * 2 + 1]
            sr = strow[bi % 2]
            for cc in range(4):
                S.op("act", lambda e, cc=cc, c0=c0, n=n: e.copy(out=xb[:, 0, 0:n], in_=acc[:, cc, c0:c0 + n]), r=[acc], w=[xb])
                S.op("act", lambda e, cc=cc, c0=c0, n=n: e.activation(out=xb[:, 1, 0:n], in_=acc[:, cc, c0:c0 + n], func=AF.Square), r=[acc], w=[xb])
                S.op("pe", lambda e, p1=p1, n=n, cc=cc: e.matmul(p1[0:1, 0:n], lhsT=self.ones_bf[:, 0:1], rhs=xb[:, 0, 0:n],
                                                                 start=(cc == 0), stop=(cc == 3)), r=[xb, self.ones_bf], w=[p1])
                S.op("pe", lambda e, p2=p2, n=n, cc=cc: e.matmul(p2[0:1, 0:n], lhsT=self.ones_bf[:, 0:1], rhs=xb[:, 1, 0:n],
                                                                 start=(cc == 0), stop=(cc == 3)), r=[xb, self.ones_bf], w=[p2])
            S.op("dve", lambda e, p1=p1, sr=sr, n=n: e.tensor_copy(out=sr[0:1, 0, 0:n], in_=p1[0:1, 0:n]), r=[p1], w=[sr])
            S.op("dve", lambda e, p2=p2, sr=sr, n=n: e.tensor_copy(out=sr[0:1, 1, 0:n], in_=p2[0:1, 0:n]), r=[p2], w=[sr])
            S.dma("sp", cst.t[:, c0:c0 + n].rearrange("(o a) n -> o a n", o=1), sr[0:1, :, 0:n], sr, r=[sr], w=[cst])
        S.coll("AllGather", cst.t, cstg.t, PAIRS, Buf("cstsem"), r=[cst], w=[cstg])
        both = [S.sb([1, 4, 512], F32, "cboth") for _ in range(2)]
        mrs = [S.sb([1, 4, 512], F32, "cmr") for _ in range(2)]
        hls = [S.sb([1, 2, 2, 512], BF16, "chl") for _ in range(2)]
        ot = [S.sb([128, 512], F32, "cot") for _ in range(2)]
        ob = [S.sb([128, 4, 512], BF16, "cob") for _ in range(2)]
        for bi in range(nblk):
            c0 = bi * 512
            n = min(512, NA - c0)
            p1, p2 = pst[(bi % 2) * 2], pst[(bi % 2) * 2 + 1]
            bo, mr, hl = both[bi % 2], mrs[bi % 2], hls[bi % 2]
            S.dma("sp", bo[0:1, :, 0:n], cstg.t[:, c0:c0 + n].rearrange("(o a) n -> o a n", o=1), bo, r=[cstg], w=[bo])
            S.op("dve", lambda e, bo=bo, mr=mr, n=n: e.tensor_tensor(out=mr[0:1, 0:2, 0:n], in0=bo[0:1, 0:2, 0:n], in1=bo[0:1, 2:4, 0:n], op=ALU.add), r=[bo], w=[mr])
            S.op("dve", lambda e, mr=mr, n=n: e.tensor_scalar(out=mr[0:1, 0:2, 0:n], in0=mr[0:1, 0:2, 0:n], scalar1=1.0 / 1024, scalar2=None, op0=ALU.mult), r=[mr], w=[mr])
            S.op("dve", lambda e, mr=mr, n=n: e.tensor_tensor(out=mr[0:1, 3, 0:n], in0=mr[0:1, 0, 0:n], in1=mr[0:1, 0, 0:n], op=ALU.mult), r=[mr], w=[mr])
            S.op("dve", lambda e, mr=mr, n=n: e.tensor_tensor(out=mr[0:1, 1, 0:n], in0=mr[0:1, 1, 0:n], in1=mr[0:1, 3, 0:n], op=ALU.subtract), r=[mr], w=[mr])
            S.op("dve", lambda e, mr=mr, n=n: e.tensor_scalar(out=mr[0:1, 1, 0:n], in0=mr[0:1, 1, 0:n], scalar1=1e-5, scalar2=None, op0=ALU.add), r=[mr], w=[mr])
            S.op("act", lambda e, mr=mr, n=n: e.activation(out=mr[0:1, 1, 0:n], in_=mr[0:1, 1, 0:n], func=AF.Sqrt), r=[mr], w=[mr])
            S.op("dve", lambda e, mr=mr, n=n: e.reciprocal(out=mr[0:1, 1, 0:n], in_=mr[0:1, 1, 0:n]), r=[mr], w=[mr])
            S.op("dve", lambda e, mr=mr, n=n: e.scalar_tensor_tensor(out=mr[0:1, 2, 0:n], in0=mr[0:1, 0, 0:n], scalar=-1.0, in1=mr[0:1, 1, 0:n],
                                                                     op0=ALU.mult, op1=ALU.mult), r=[mr], w=[mr])
            S.op("dve", lambda e, mr=mr, hl=hl, n=n: e.tensor_copy(out=hl[0:1, 0, :, 0:n], in_=mr[0:1, 1:3, 0:n]), r=[mr], w=[hl])
            S.op("dve", lambda e, mr=mr, hl=hl, n=n: e.tensor_tensor(out=mr[0:1, 1:3, 0:n], in0=mr[0:1, 1:3, 0:n], in1=hl[0:1, 0, :, 0:n], op=ALU.subtract), r=[mr, hl], w=[mr])
            S.op("dve", lambda e, mr=mr, hl=hl, n=n: e.tensor_copy(out=hl[0:1, 1, :, 0:n], in_=mr[0:1, 1:3, 0:n]), r=[mr], w=[hl])
            for which, p in ((0, p1), (1, p2)):
                for hq in range(2):
                    S.op("pe", lambda e, p=p, which=which, hq=hq, hl=hl, n=n: e.matmul(
                        p[:, 0:n], lhsT=self.ones_bf[0:1, :], rhs=hl[0:1, hq, which, 0:n], start=(hq == 0), stop=(hq == 1)),
                        r=[hl, self.ones_bf], w=[p])
            o_b = ob[bi % 2]
            for cc in range(4):
                o_ = ot[cc % 2]
                S.op("dve", lambda e, o_=o_, cc=cc, c0=c0, n=n, p1=p1: e.tensor_tensor(out=o_[:, 0:n], in0=acc[:, cc, c0:c0 + n], in1=p1[:, 0:n], op=ALU.mult), r=[acc, p1], w=[o_])
                S.op("dve", lambda e, o_=o_, n=n, p2=p2: e.tensor_tensor(out=o_[:, 0:n], in0=o_[:, 0:n], in1=p2[:, 0:n], op=ALU.add), r=[o_, p2], w=[o_])
                S.op("act", lambda e, o_=o_, o_b=o_b, cc=cc, n=n: e.activation(out=o_b[:, cc, 0:n], in_=o_[:, 0:n], func=AF.Silu,
                                                                              scale=cb[:, l, cc, 1:2], bias=cb[:, l, cc, 2:3]), r=[o_, cb], w=[o_b])
            for (a_lo, a_hi, s_lo) in ((0, 256, 0), (271, NA, 256)):
                lo, hi = max(a_lo, c0), min(a_hi, c0 + n)
                if lo >= hi:
                    continue
                for (ap, off, ln) in self.mix_dst(mixT, 0, 512, s_lo + lo - a_lo, hi - lo):
                    S.dma("pool", ap.rearrange("(cc p) n -> p cc n", p=128), o_b[:, :, lo - c0 + off:lo - c0 + off + ln], o_b, r=[o_b], w=[mixT])
        S.pop()


    def rwkv_stage(self, l, pT, mixT):
        S = self.S
        I = S.i
        NB = 256
        ones = self.ones_bf
        yd = [S.dram(f"rw_y{d}_{l}", [512, T], F32) for d in range(2)]
        bon = [S.dram(f"rw_b{d}_{l}", [512, T], F32) for d in range(2)]
        c_r, c_k, c_v, c_w, c_a, c_g = (COLS[n][0] for n in ("rr", "rk", "rv", "rw", "ra", "rg"))
        S.push()
        rwt = S.sb([64, 8, 19], F32, "rwt")
        S.dma("sp", rwt[:], self.ext["rwt"].t[:, l], rwt, w=[rwt])
        rwl = S.sb([64, 2, 2, 3], F32, "rwl")
        S.dma("sp", rwl[:], self.ext["rwl"].t[:, l], rwl, w=[rwl])
        w2f = S.sb([64, 2, 2, 512], F32, "w2f")
        w2b = S.sb([64, 2, 2, 512], BF16, "w2b")
        S.dma("sp", w2f[:], self.ext["rw2"].t[:, l], w2f, w=[w2f])
        I("dve", "tensor_copy", out=w2b[:], in_=w2f[:], r=[w2f], w=[w2b])
        msk = S.sb([64, 2, 192], F32, "rmask")
        S.dma("sp", msk[:], self.ext["rmask"].t, msk, w=[msk])

        def tb(col):
            return rwt[:, :, col:col + 1]

        for d in range(2):
            S.push()
            ST = S.sb([64, 8, 64], F32, "ST")
            STb = S.sb([64, 8, 64], BF16, "STb")
            I("dve", "memset", ST[:], 0.0, w=[ST])
            I("dve", "memset", STb[:], 0.0, w=[STb])
            xr = S.sb([64, 8, NB + 2], F32, "xr")
            xk = S.sb([64, 8, NB + 2], F32, "xk")
            xv = S.sb([64, 8, NB + 2], F32, "xv")
            xl = S.sb([64, 2, NB + 2], F32, "xl")
            rs_ = S.sb([64, 8, NB], F32, "rs_")
            ks_ = S.sb([64, 8, NB], F32, "ks_")
            vs_ = S.sb([64, 8, NB], F32, "vs_")
            ls_ = S.sb([64, 2, NB], F32, "ls_")
            lb_ = S.sb([64, 2, NB], BF16, "lb_")
            t1 = S.sb([64, 8, NB], F32, "t1")
            t2 = S.sb([64, 8, NB], F32, "t2")
            tbf = S.sb([64, 8, NB], BF16, "tbf")
            lw = S.sb([64, 8, NB], F32, "lw")
            av = S.sb([64, 8, NB], F32, "av")
            kk = S.sb([64, 8, NB], F32, "kk")
            Lp = S.sb([64, 64 + 8 * NB], F32, "Lp")
            onesf = S.sb([64, 8 * NB], BF16, "onesf")
            tot = S.sb([64, 32], F32, "tot")
            wcc = S.sb([64, 32], F32, "wcc")
            AR = S.sb([64, 8, 4, 2, 64], BF16, "AR")
            KT = S.sb([64, 8, 4, 64], BF16, "KT")
            BT = S.sb([64, 8, 4, 64], BF16, "BT")
            vb = S.sb([64, 8, NB], BF16, "vb")
            vtok = S.sb([64, 8, 64], BF16, "vtok")
            kbtok = S.sb([64, 2, 8, 64], BF16, "kbtok")
            Mkb = S.sb([64, 4, 128], BF16, "Mkb")
            Mbb = S.sb([64, 4, 128], BF16, "Mbb")
            Pk = [S.sb([64, 2, 4, 64], BF16, "Pk") for _ in range(2)]
            U = S.sb([64, 4, 64], F32, "U")
            Ub = S.sb([64, 4, 64], BF16, "Ub")
            yblk = S.sb([64, 8, NB], F32, "yblk")
            tS = S.sb([64, 4, 64], F32, "tS")
            psA = S.ps([64, 512], F32, "rpsA")
            psB = S.ps([64, 512], F32, "rpsB")
            psP = S.ps([64, 512], F32, "rpsP")
            psQ = S.ps([64, 512], F32, "rpsQ")
            psX = S.ps([64, 512], F32, "rpsX")
            psT = S.ps([64, 1024], BF16, "rpsT")
            psT2 = S.ps([64, 1024], BF16, "rpsT2")
            I("dve", "memset", Lp[:, 0:64], 0.0, w=[Lp])
            I("dve", "memset", onesf[:], 1.0, w=[onesf])
            blks = [(0, 0, 256)] + [(256 + NB * q, 256, T) for q in range(16)]
            if d == 1:
                blks = [blks[0]] + blks[:0:-1]
            blks = blks[:RW_NBLK]
            for (s0, lo, hi) in blks:
                a0, a1 = max(lo, s0 - 1), min(hi, s0 + NB + 1)
                c0_, c1_ = a0 - (s0 - 1), a1 - (s0 - 1)
                for (xt, col) in ((xr, c_r), (xk, c_k), (xv, c_v)):
                    I("pool", "memset", xt[:], 0.0, w=[xt])
                    S.dma("sp", xt[:, :, c0_:c1_], pT.t[col:col + 512, a0:a1].rearrange("(h i) t -> i h t", i=64), xt, r=[pT], w=[xt])
                I("pool", "memset", xl[:], 0.0, w=[xl])
                S.dma("act", xl[:, 0, c0_:c1_], pT.t[c_w + d * 64:c_w + d * 64 + 64, a0:a1], xl, r=[pT], w=[xl])
                S.dma("act", xl[:, 1, c0_:c1_], pT.t[c_a + d * 64:c_a + d * 64 + 64, a0:a1], xl, r=[pT], w=[xl])
                for (xt, out_, tcol) in ((xr, rs_, 0), (xk, ks_, 3), (xv, vs_, 6)):
                    I("dve", "tensor_tensor", out=out_[:], in0=xt[:, :, 1:NB + 1], in1=tb(tcol).to_broadcast([64, 8, NB]), op=ALU.mult, r=[xt, rwt], w=[out_])
                    I("dve", "tensor_tensor", out=t1[:], in0=xt[:, :, 0:NB], in1=tb(tcol + 1).to_broadcast([64, 8, NB]), op=ALU.mult, r=[xt, rwt], w=[t1])
                    I("dve", "tensor_tensor", out=out_[:], in0=out_[:], in1=t1[:], op=ALU.add, r=[out_, t1], w=[out_])
                    I("dve", "tensor_tensor", out=t1[:], in0=xt[:, :, 2:NB + 2], in1=tb(tcol + 2).to_broadcast([64, 8, NB]), op=ALU.mult, r=[xt, rwt], w=[t1])
                    I("dve", "tensor_tensor", out=out_[:], in0=out_[:], in1=t1[:], op=ALU.add, r=[out_, t1], w=[out_])
                for q in range(2):
                    I("dve", "tensor_scalar", out=ls_[:, q, :], in0=xl[:, q, 1:NB + 1], scalar1=rwl[:, d, q, 0:1], scalar2=None, op0=ALU.mult, r=[xl, rwl], w=[ls_])
                    I("dve", "scalar_tensor_tensor", out=ls_[:, q, :], in0=xl[:, q, 0:NB], scalar=rwl[:, d, q, 1:2], in1=ls_[:, q, :], op0=ALU.mult, op1=ALU.add, r=[xl, rwl, ls_], w=[ls_])
                    I("dve", "scalar_tensor_tensor", out=ls_[:, q, :], in0=xl[:, q, 2:NB + 2], scalar=rwl[:, d, q, 2:3], in1=ls_[:, q, :], op0=ALU.mult, op1=ALU.add, r=[xl, rwl, ls_], w=[ls_])
                I("act", "activation", out=lb_[:, 0, :], in_=ls_[:, 0, :], func=AF.Tanh, r=[ls_], w=[lb_])
                I("act", "copy", out=lb_[:, 1, :], in_=ls_[:, 1, :], r=[ls_], w=[lb_])
                for (q, dst, bcol) in ((0, lw, 15 + d), (1, av, 17 + d)):
                    for hp in range(4):
                        ps = psA if hp % 2 == 0 else psB
                        for hh in range(2):
                            h = hp * 2 + hh
                            I("pe", "matmul", ps[:, hh * NB:(hh + 1) * NB], lhsT=w2b[:, d, q, h * 64:(h + 1) * 64], rhs=lb_[:, q, :], start=True, stop=True, r=[w2b, lb_], w=[ps])
                        I("dve", "tensor_tensor", out=dst[:, hp * 2:hp * 2 + 2, :], in0=ps[:, 0:2 * NB].rearrange("p (h n) -> p h n", h=2),
                          in1=rwt[:, hp * 2:hp * 2 + 2, bcol:bcol + 1].to_broadcast([64, 2, NB]), op=ALU.add, r=[ps, rwt], w=[dst])
                    I("act", "activation", out=dst[:], in_=dst[:], func=AF.Sigmoid, r=[dst], w=[dst])
                I("dve", "tensor_scalar", out=lw[:], in0=lw[:], scalar1=-float(np.exp(-0.5)), scalar2=None, op0=ALU.mult, r=[lw], w=[lw])
                I("dve", "tensor_tensor", out=kk[:], in0=ks_[:], in1=tb(9).to_broadcast([64, 8, NB]), op=ALU.mult, r=[ks_, rwt], w=[kk])
                I("act", "activation", out=tbf[:], in_=kk[:], func=AF.Square, r=[kk], w=[tbf])
                for hp in range(4):
                    ps = psA if hp % 2 == 0 else psB
                    I("pe", "matmul", ps[:, 0:2 * NB], lhsT=ones[0:64, 0:64], rhs=tbf[:, hp * 2:hp * 2 + 2, :], start=True, stop=True, r=[ones, tbf], w=[ps])
                    I("act", "activation", out=t1[:, hp * 2:hp * 2 + 2, :], in_=ps[:, 0:2 * NB].rearrange("p (h n) -> p h n", h=2), func=AF.Sqrt, r=[ps], w=[t1])
                I("dve", "tensor_scalar", out=t1[:], in0=t1[:], scalar1=1e-12, scalar2=None, op0=ALU.max, r=[t1], w=[t1])
                I("dve", "reciprocal", out=t1[:], in_=t1[:], r=[t1], w=[t1])
                I("dve", "tensor_tensor", out=kk[:], in0=kk[:], in1=t1[:], op=ALU.mult, r=[kk, t1], w=[kk])
                I("dve", "tensor_tensor", out=t1[:], in0=av[:], in1=tb(10).to_broadcast([64, 8, NB]), op=ALU.mult, r=[av, rwt], w=[t1])
                I("dve", "tensor_tensor", out=t1[:], in0=t1[:], in1=tb(11).to_broadcast([64, 8, NB]), op=ALU.add, r=[t1, rwt], w=[t1])
                I("dve", "tensor_tensor", out=ks_[:], in0=ks_[:], in1=t1[:], op=ALU.mult, r=[ks_, t1], w=[ks_])
                I("dve", "tensor_tensor", out=av[:], in0=av[:], in1=kk[:], op=ALU.mult, r=[av, kk], w=[av])
                I("dve", "tensor_tensor", out=t1[:], in0=rs_[:], in1=ks_[:], op=ALU.mult, r=[rs_, ks_], w=[t1])
                I("dve", "tensor_tensor", out=tbf[:], in0=t1[:], in1=tb(12).to_broadcast([64, 8, NB]), op=ALU.mult, r=[t1, rwt], w=[tbf])
                for hp in range(4):
                    ps = psA if hp % 2 == 0 else psB
                    I("pe", "matmul", ps[:, 0:2 * NB], lhsT=ones[0:64, 0:64], rhs=tbf[:, hp * 2:hp * 2 + 2, :], start=True, stop=True, r=[ones, tbf], w=[ps])
                    I("dve", "tensor_tensor", out=t2[:, hp * 2:hp * 2 + 2, :], in0=ps[:, 0:2 * NB].rearrange("p (h n) -> p h n", h=2), in1=vs_[:, hp * 2:hp * 2 + 2, :], op=ALU.mult, r=[ps, vs_], w=[t2])
                S.dma("act", bon[d].t[:, s0:s0 + NB].rearrange("(h i) t -> i h t", i=64), t2[:], t2, r=[t2], w=[bon[d]])
                I("dve", "tensor_tensor_scan", out=Lp[:, 64:], data0=onesf[:], data1=lw[:].rearrange("p h n -> p (h n)"), initial=0.0, op0=ALU.mult, op1=ALU.add, r=[onesf, lw], w=[Lp])
                Lc = Lp[:, 64:].rearrange("p (m n) -> p m n", n=64)
                Lprev = Lp[:, 0:8 * NB].rearrange("p (m n) -> p m n", n=64)[:, :, 63:64]
                t1v = t1[:].rearrange("p h (c n) -> p (h c) n", n=64)
                t2v = t2[:].rearrange("p h (c n) -> p (h c) n", n=64)
                lwv = lw[:].rearrange("p h (c n) -> p (h c) n", n=64)
                I("dve", "tensor_tensor", out=t1v, in0=Lc, in1=Lprev.to_broadcast([64, 32, 64]), op=ALU.subtract, r=[Lp], w=[t1])
                I("dve", "tensor_copy", out=tot[:].unsqueeze(2), in_=t1v[:, :, 63:64], r=[t1], w=[tot])
                I("act", "activation", out=wcc[:], in_=tot[:], func=AF.Exp, r=[tot], w=[wcc])
                I("dve", "tensor_tensor", out=t2v, in0=t1v, in1=lwv, op=ALU.subtract, r=[t1, lw], w=[t2])
                if d == 1:
                    I("dve", "tensor_tensor", out=lwv, in0=tot[:].unsqueeze(2).to_broadcast([64, 32, 64]), in1=t2v, op=ALU.subtract, r=[tot, t2], w=[lw])
                    I("dve", "tensor_tensor", out=t2v, in0=tot[:].unsqueeze(2).to_broadcast([64, 32, 64]), in1=t1v, op=ALU.subtract, r=[tot, t1], w=[t2])
                    I("dve", "tensor_copy", out=t1[:], in_=lw[:], r=[lw], w=[t1])
                ARv = AR[:].rearrange("p h c a n -> p (h c) a n")
                I("act", "activation", out=lw[:], in_=t2[:], func=AF.Exp, r=[t2], w=[lw])
                I("dve", "scalar_tensor_tensor", out=ARv[:, :, 0, :], in0=kk[:].rearrange("p h (c n) -> p (h c) n", n=64), scalar=-1.0, in1=lwv, op0=ALU.mult, op1=ALU.mult, r=[kk, lw], w=[AR])
                I("act", "activation", out=lw[:], in_=t1[:], func=AF.Exp, r=[t1], w=[lw])
                I("dve", "tensor_tensor", out=ARv[:, :, 1, :], in0=rs_[:].rearrange("p h (c n) -> p (h c) n", n=64), in1=lwv, op=ALU.mult, r=[rs_, lw], w=[AR])
                I("act", "activation", out=lw[:], in_=t1[:], func=AF.Exp, scale=-1.0, r=[t1], w=[lw])
                I("dve", "tensor_tensor", out=KT[:].rearrange("p h c n -> p h (c n)"), in0=ks_[:], in1=lw[:], op=ALU.mult, r=[ks_, lw], w=[KT])
                I("dve", "tensor_tensor", out=BT[:].rearrange("p h c n -> p h (c n)"), in0=av[:], in1=lw[:], op=ALU.mult, r=[av, lw], w=[BT])
                I("act", "copy", out=vb[:], in_=vs_[:], r=[vs_], w=[vb])
                chunks = range(4) if d == 0 else range(3, -1, -1)
                if RW_LEVEL < 2:
                    chunks = []
                for q in chunks:
                    for h in range(8):
                        I("pe", "transpose", out=psT[:, h * 64:(h + 1) * 64], in_=vb[:, h, q * 64:(q + 1) * 64], identity=self.ident[0:64, 0:64], r=[vb, self.ident], w=[psT])
                    I("act", "copy", out=vtok[:].rearrange("p h n -> p (h n)"), in_=psT[:, 0:512], r=[psT], w=[vtok])
                    for h in range(8):
                        I("pe", "transpose", out=psT2[:, h * 64:(h + 1) * 64], in_=KT[:, h, q, :], identity=self.ident[0:64, 0:64], r=[KT, self.ident], w=[psT2])
                        I("pe", "transpose", out=psT2[:, 512 + h * 64:512 + (h + 1) * 64], in_=BT[:, h, q, :], identity=self.ident[0:64, 0:64], r=[BT, self.ident], w=[psT2])
                    I("act", "copy", out=kbtok[:].rearrange("p a h n -> p (a h n)"), in_=psT2[:, 0:1024], r=[psT2], w=[kbtok])
                    for hg in (range(2) if RW_LEVEL >= 3 else []):
                        hs = [hg * 4 + x for x in range(4)]
                        for x, h in enumerate(hs):
                            I("pe", "matmul", psA[:, x * 128:(x + 1) * 128], lhsT=KT[:, h, q, :], rhs=AR[:, h, q, :, :], start=True, stop=True, r=[KT, AR], w=[psA])
                        for x, h in enumerate(hs):
                            I("pe", "matmul", psB[:, x * 128:(x + 1) * 128], lhsT=BT[:, h, q, :], rhs=AR[:, h, q, :, :], start=True, stop=True, r=[BT, AR], w=[psB])
                        for x, h in enumerate(hs):
                            I("pe", "matmul", psP[:, x * 64:(x + 1) * 64], lhsT=AR[:, h, q, 0, :], rhs=BT[:, h, q, :], start=True, stop=True, r=[AR, BT], w=[psP])
                        I("dve", "tensor_tensor", out=Mkb[:], in0=psA[:, 0:512].rearrange("p (x n) -> p x n", x=4), in1=msk[:, d:d + 1, 0:128].to_broadcast([64, 4, 128]), op=ALU.mult, r=[psA, msk], w=[Mkb])
                        I("dve", "tensor_tensor", out=Mbb[:], in0=psB[:, 0:512].rearrange("p (x n) -> p x n", x=4), in1=msk[:, d:d + 1, 0:128].to_broadcast([64, 4, 128]), op=ALU.mult, r=[psB, msk], w=[Mbb])
                        pk = Pk[0]
                        I("dve", "tensor_tensor", out=pk[:, 0, :, :], in0=psP[:, 0:256].rearrange("p (x n) -> p x n", x=4), in1=msk[:, d:d + 1, 128:192].to_broadcast([64, 4, 64]), op=ALU.mult, r=[psP, msk], w=[pk])
                        I("act", "copy", out=pk[:, 1, :, :], in_=Mbb[:, :, 0:64], r=[Mbb], w=[pk])
                        if RW_LEVEL < 4:
                            continue
                        for x, h in enumerate(hs):
                            I("pe", "matmul", psX[:, x * 64:(x + 1) * 64], lhsT=Mkb[:, x, 0:64], rhs=vtok[:, h, :], start=True, stop=False, r=[Mkb, vtok], w=[psX])
                            I("pe", "matmul", psX[:, x * 64:(x + 1) * 64], lhsT=AR[:, h, q, 0, :], rhs=STb[:, h, :], start=False, stop=True, r=[AR, STb], w=[psX])
                        I("dve", "tensor_copy", out=U[:].rearrange("p x n -> p (x n)"), in_=psX[:, 0:256], r=[psX], w=[U])
                        I("dve", "tensor_copy", out=Ub[:], in_=U[:], r=[U], w=[Ub])
                        for lev in range(RW_NLEV):
                            pk = Pk[lev % 2]
                            for x in range(4):
                                I("pe", "matmul", psX[:, x * 64:(x + 1) * 64], lhsT=pk[:, 1, x, :], rhs=Ub[:, x, :], start=True, stop=True, r=[pk, Ub], w=[psX])
                            if lev < 5:
                                pn = Pk[(lev + 1) % 2]
                                for x in range(4):
                                    I("pe", "matmul", psQ[:, x * 64:(x + 1) * 64], lhsT=pk[:, 1, x, :], rhs=pk[:, 0, x, :], start=True, stop=True, r=[pk], w=[psQ])
                                    I("pe", "matmul", psQ[:, 256 + x * 64:256 + (x + 1) * 64], lhsT=pk[:, 0, x, :], rhs=pk[:, 1, x, :], start=True, stop=True, r=[pk], w=[psQ])
                                I("act", "copy", out=pn[:].rearrange("p a x n -> p (a x n)"), in_=psQ[:, 0:512], r=[psQ], w=[pn])
                            I("dve", "tensor_tensor", out=U[:].rearrange("p x n -> p (x n)"), in0=U[:].rearrange("p x n -> p (x n)"), in1=psX[:, 0:256], op=ALU.add, r=[U, psX], w=[U])
                            I("dve", "tensor_copy", out=Ub[:], in_=U[:], r=[U], w=[Ub])
                        if RW_LEVEL < 5:
                            continue
                        for x, h in enumerate(hs):
                            I("pe", "matmul", psP[:, 256 + x * 64:256 + (x + 1) * 64], lhsT=vtok[:, h, :], rhs=Mkb[:, x, 64:128], start=True, stop=False, r=[vtok, Mkb], w=[psP])
                            I("pe", "matmul", psP[:, 256 + x * 64:256 + (x + 1) * 64], lhsT=Ub[:, x, :], rhs=Mbb[:, x, 64:128], start=False, stop=False, r=[Ub, Mbb], w=[psP])
                            I("pe", "matmul", psP[:, 256 + x * 64:256 + (x + 1) * 64], lhsT=STb[:, h, :], rhs=AR[:, h, q, 1, :], start=False, stop=True, r=[STb, AR], w=[psP])
                        I("act", "copy", out=yblk[:, hg * 4:hg * 4 + 4, q * 64:(q + 1) * 64], in_=psP[:, 256:512].rearrange("p (x n) -> p x n", x=4), r=[psP], w=[yblk])
                        if RW_LEVEL < 6:
                            continue
                        for x, h in enumerate(hs):
                            I("pe", "matmul", psQ[:, x * 64:(x + 1) * 64], lhsT=kbtok[:, 0, h, :], rhs=vtok[:, h, :], start=True, stop=False, r=[kbtok, vtok], w=[psQ])
                            I("pe", "matmul", psQ[:, x * 64:(x + 1) * 64], lhsT=kbtok[:, 1, h, :], rhs=Ub[:, x, :], start=False, stop=True, r=[kbtok, Ub], w=[psQ])
                        I("dve", "tensor_tensor", out=tS[:], in0=ST[:, hg * 4:hg * 4 + 4, :], in1=psQ[:, 0:256].rearrange("p (x n) -> p x n", x=4), op=ALU.add, r=[ST, psQ], w=[tS])
                        wv = wcc[:].rearrange("p (h c) -> p h c", c=4)[:, hg * 4:hg * 4 + 4, q:q + 1]
                        I("dve", "tensor_tensor", out=ST[:, hg * 4:hg * 4 + 4, :], in0=tS[:], in1=wv.to_broadcast([64, 4, 64]), op=ALU.mult, r=[tS, wcc], w=[ST])
                        I("act", "copy", out=STb[:, hg * 4:hg * 4 + 4, :], in_=ST[:, hg * 4:hg * 4 + 4, :], r=[ST], w=[STb])
                        if RW_BAR:
                            S.barrier()
                S.dma("act", yd[d].t[:, s0:s0 + NB].rearrange("(h i) t -> i h t", i=64), yblk[:], yblk, r=[yblk], w=[yd[d]])
            S.pop()
        g2f = S.sb([128, 2, 512], F32, "g2f")
        g2b = S.sb([128, 2, 512], BF16, "g2b")
        S.dma("sp", g2f[:], self.ext["rg2"].t[:, l], g2f, w=[g2f])
        I("dve", "tensor_copy", out=g2b[:], in_=g2f[:], r=[g2f], w=[g2b])
        rgl = S.sb([128, 2, 3], F32, "rgl")
        S.dma("sp", rgl[:], self.ext["rgl"].t[:, l], rgl, w=[rgl])
        NF = 256
        y0 = S.sb([64, 8, NF], F32, "fy0")
        y1 = S.sb([64, 8, NF], F32, "fy1")
        b0 = S.sb([64, 8, NF], F32, "fb0")
        b1 = S.sb([64, 8, NF], F32, "fb1")
        yb = S.sb([64, 8, NF], BF16, "fyb")
        mu = S.sb([64, 8, NF], F32, "fmu")
        va = S.sb([64, 8, NF], F32, "fva")
        xg = S.sb([128, 2, NF + 2], F32, "fxg")
        sg = S.sb([128, 2, NF], F32, "fsg")
        sgb = S.sb([128, 2, NF], BF16, "fsgb")
        ob = S.sb([64, 8, NF], BF16, "fob")
        psA = S.ps([64, 512], F32, "fpsA")
        psB = S.ps([64, 512], F32, "fpsB")
        psG = S.ps([64, 512], F32, "fpsG")
        for (s0, lo, hi) in ([(0, 0, 256)] + [(256 + NF * q, 256, T) for q in range(16)])[:RW_NFIN]:
            for (tl, src) in ((y0, yd[0]), (y1, yd[1]), (b0, bon[0]), (b1, bon[1])):
                S.dma("sp", tl[:], src.t[:, s0:s0 + NF].rearrange("(h i) t -> i h t", i=64), tl, r=[src], w=[tl])
            I("dve", "tensor_tensor", out=y0[:], in0=y0[:], in1=y1[:], op=ALU.add, r=[y0, y1], w=[y0])
            I("dve", "tensor_tensor", out=b0[:], in0=b0[:], in1=b1[:], op=ALU.add, r=[b0, b1], w=[b0])
            I("act", "copy", out=yb[:], in_=y0[:], r=[y0], w=[yb])
            for hp in range(4):
                ps = psA if hp % 2 == 0 else psB
                I("pe", "matmul", ps[:, 0:2 * NF], lhsT=ones[0:64, 0:64], rhs=yb[:, hp * 2:hp * 2 + 2, :], start=True, stop=True, r=[ones, yb], w=[ps])
                I("act", "activation", out=mu[:, hp * 2:hp * 2 + 2, :], in_=ps[:, 0:2 * NF].rearrange("p (h n) -> p h n", h=2), func=AF.Identity, scale=1.0 / 64, r=[ps], w=[mu])
            I("dve", "tensor_tensor", out=y0[:], in0=y0[:], in1=mu[:], op=ALU.subtract, r=[y0, mu], w=[y0])
            I("act", "activation", out=yb[:], in_=y0[:], func=AF.Square, r=[y0], w=[yb])
            for hp in range(4):
                ps = psA if hp % 2 == 0 else psB
                I("pe", "matmul", ps[:, 0:2 * NF], lhsT=ones[0:64, 0:64], rhs=yb[:, hp * 2:hp * 2 + 2, :], start=True, stop=True, r=[ones, yb], w=[ps])
                I("dve", "tensor_scalar", out=va[:, hp * 2:hp * 2 + 2, :], in0=ps[:, 0:2 * NF].rearrange("p (h n) -> p h n", h=2), scalar1=1.0 / 64, scalar2=64e-5, op0=ALU.mult, op1=ALU.add, r=[ps], w=[va])
            I("act", "activation", out=va[:], in_=va[:], func=AF.Sqrt, r=[va], w=[va])
            I("dve", "reciprocal", out=va[:], in_=va[:], r=[va], w=[va])
            I("dve", "tensor_tensor", out=y0[:], in0=y0[:], in1=va[:], op=ALU.mult, r=[y0, va], w=[y0])
            I("dve", "tensor_tensor", out=y0[:], in0=y0[:], in1=rwt[:, :, 13:14].to_broadcast([64, 8, NF]), op=ALU.mult, r=[y0, rwt], w=[y0])
            I("dve", "tensor_tensor", out=y0[:], in0=y0[:], in1=rwt[:, :, 14:15].to_broadcast([64, 8, NF]), op=ALU.add, r=[y0, rwt], w=[y0])
            I("dve", "tensor_tensor", out=y0[:], in0=y0[:], in1=b0[:], op=ALU.add, r=[y0, b0], w=[y0])
            a0, a1 = max(lo, s0 - 1), min(hi, s0 + NF + 1)
            c0_, c1_ = a0 - (s0 - 1), a1 - (s0 - 1)
            I("pool", "memset", xg[:], 0.0, w=[xg])
            S.dma("act", xg[:, 0, c0_:c1_], pT.t[c_g:c_g + 128, a0:a1], xg, r=[pT], w=[xg])
            S.dma("act", xg[0:32, 1, c0_:c1_], pT.t[c_g + 128:c_g + 160, a0:a1], xg, r=[pT], w=[xg])
            for q in range(2):
                I("dve", "tensor_scalar", out=sg[:, q, :], in0=xg[:, q, 1:NF + 1], scalar1=rgl[:, q, 0:1], scalar2=None, op0=ALU.mult, r=[xg, rgl], w=[sg])
                I("dve", "scalar_tensor_tensor", out=sg[:, q, :], in0=xg[:, q, 0:NF], scalar=rgl[:, q, 1:2], in1=sg[:, q, :], op0=ALU.mult, op1=ALU.add, r=[xg, rgl, sg], w=[sg])
                I("dve", "scalar_tensor_tensor", out=sg[:, q, :], in0=xg[:, q, 2:NF + 2], scalar=rgl[:, q, 2:3], in1=sg[:, q, :], op0=ALU.mult, op1=ALU.add, r=[xg, rgl, sg], w=[sg])
            I("act", "activation", out=sgb[:], in_=sg[:], func=AF.Sigmoid, r=[sg], w=[sgb])
            for hp in range(4):
                for hh in range(2):
                    h = hp * 2 + hh
                    I("pe", "matmul", psG[:, hh * NF:(hh + 1) * NF], lhsT=g2b[:, 0, h * 64:(h + 1) * 64], rhs=sgb[:, 0, :], start=True, stop=False, r=[g2b, sgb], w=[psG])
                    I("pe", "matmul", psG[:, hh * NF:(hh + 1) * NF], lhsT=g2b[0:32, 1, h * 64:(h + 1) * 64], rhs=sgb[0:32, 1, :], start=False, stop=True, r=[g2b, sgb], w=[psG])
                I("dve", "tensor_tensor", out=ob[:, hp * 2:hp * 2 + 2, :], in0=y0[:, hp * 2:hp * 2 + 2, :], in1=psG[:, 0:2 * NF].rearrange("p (h n) -> p h n", h=2), op=ALU.mult, r=[y0, psG], w=[ob])
            for (ap, off, ln) in self.mix_dst(mixT, 512, 512, s0, NF):
                S.dma("pool", ap.rearrange("(h i) n -> i h n", i=64), ob[:, :, off:off + ln], ob, r=[ob], w=[mixT])
        S.pop()

    def convert_mla(self):
        S = self.S
        NL = self.NL
        wuq = self.ext_in("wuq", [NL, 1536, 1536], F32)
        wukv = self.ext_in("wukv", [NL, 512, 2048], F32)
        self.wuqb = S.dram("wuqb", [NL, 8, 128, 12, 192], BF16)
        self.wukvb = S.dram("wukvb", [NL, 128, 4, 2048], BF16)
        S.push()
        f = [S.sb([128, 2048], F32, "mcf") for _ in range(2)]
        b = [S.sb([128, 2048], BF16, "mcb") for _ in range(2)]
        eng = RR(["dve", "pool", "act"])
        it = 0
        for l in range(NL):
            for c in range(12):
                ff, bb = f[it % 2], b[it % 2]
                it += 1
                S.dma("sp", ff[:, 0:1536], wuq.t[l, c * 128:(c + 1) * 128, :], ff, w=[ff])
                for q in range(2):
                    copy_on(S, eng(), bb[:, q * 768:(q + 1) * 768], ff[:, q * 768:(q + 1) * 768], [ff], [bb])
                S.dma("pool", self.wuqb.t[l, :, :, c, :].rearrange("h p x -> p h x"),
                      bb[:, 0:1536].rearrange("p (h x) -> p h x", x=192), bb, r=[bb], w=[self.wuqb])
            for c in range(4):
                ff, bb = f[it % 2], b[it % 2]
                it += 1
                S.dma("sp", ff[:], wukv.t[l, c * 128:(c + 1) * 128, :], ff, w=[ff])
                for q in range(2):
                    copy_on(S, eng(), bb[:, q * 1024:(q + 1) * 1024], ff[:, q * 1024:(q + 1) * 1024], [ff], [bb])
                S.dma("pool", self.wukvb.t[l, :, c, :], bb[:], bb, r=[bb], w=[self.wukvb])
        S.pop()

    def rstd_from_ps(self, ps, out_sb, rows, n, inv_n, eps):
        S = self.S
        S.op("dve", lambda e: e.tensor_scalar(out=out_sb[0:rows, 0:n], in0=ps[0:rows, 0:n], scalar1=inv_n, scalar2=eps,
                                              op0=ALU.mult, op1=ALU.add), r=[ps], w=[out_sb])
        S.op("act", lambda e: e.activation(out=out_sb[0:rows, 0:n], in_=out_sb[0:rows, 0:n], func=AF.Sqrt), r=[out_sb], w=[out_sb])
        S.op("dve", lambda e: e.reciprocal(out=out_sb[0:rows, 0:n], in_=out_sb[0:rows, 0:n]), r=[out_sb], w=[out_sb])

    def mla_stage(self, l, pT, mixT):
        S = self.S
        I = S.i
        SCALE = float((128 + 64) ** -0.5)
        blocks = [(0, 256)] + [(256 + 512 * q, 512) for q in range(8)]
        S.push()
        mg = S.sb([128, 2, 24], F32, "mg")
        S.dma("sp", mg[:], self.ext["mlag"].t, mg, w=[mg])
        qsc = S.sb([128, 2], F32, "qsc")
        I("dve", "tensor_scalar", out=qsc[:], in0=mg[:, l, 16:18], scalar1=SCALE, scalar2=None, op0=ALU.mult, r=[mg], w=[qsc])
        rotf = S.sb([64, 64], F32, "rotf")
        rotb = S.sb([64, 64], BF16, "rotb")
        S.dma("sp", rotf[:], self.ext["rotm"].t, rotf, w=[rotf])
        I("dve", "tensor_copy", out=rotb[:], in_=rotf[:], r=[rotf], w=[rotb])
        nbias = S.sb([128, 1], F32, "nbias")
        I("dve", "memset", nbias[:], -6.0, w=[nbias])
        wkv = S.sb([128, 4, 2048], BF16, "wkv")
        S.dma("sp", wkv[:], self.wukvb.t[l], wkv, r=[self.wukvb], w=[wkv])
        krT = S.sb([64, T], BF16, "krT")
        kT = S.sb([128, 4, T], BF16, "kT")
        vt = S.sb([128, 34, 512], BF16, "vt")
        wq = [S.sb([128, 12, 192], BF16, "wq") for _ in range(2)]
        xin = S.sb([128, 4, 512], F32, "xin")
        krin = S.sb([64, 512], F32, "krin")
        ropeb = S.sb([64, 2, 512], F32, "ropeb")
        xsq = S.sb([128, 512], BF16, "xsq")
        xnb = S.sb([128, 12, 512], BF16, "xnb")
        rs = S.sb([128, 512], F32, "rs")
        rs2 = S.sb([128, 512], F32, "rs2")
        tmp = S.sb([128, 512], F32, "mtmp")
        tmp2 = S.sb([64, 512], F32, "mtmp2")
        xrb = S.sb([64, 512], BF16, "xrb")
        qnb = S.sb([128, 512], BF16, "qnb")
        qrb = S.sb([64, 512], BF16, "qrb")
        pb = [S.sb([128, 512], BF16, "pb") for _ in range(2)]
        ob = S.sb([128, 512], BF16, "mob")
        ps_s = [S.ps([128, 512], F32, "ps_s") for _ in range(2)]
        ps_o = S.ps([128, 512], F32, "ps_o")
        ps_d = S.ps([128, 512], F32, "ps_d")
        ps_a = S.ps([128, 512], F32, "ps_a")
        ps_b = S.ps([128, 512], F32, "ps_b")
        ps_n = S.ps([128, 512], F32, "ps_n")
        c_kv, c_kr, c_q = COLS["mkv"][0], COLS["mkr"][0], COLS["mq"][0]
        ones = self.ones_bf

        def norm_in(col0, nch, s0, n, gcol0, inv_n):
            for c4 in range(0, nch, 4):
                S.dma("sp", xin[:, :, 0:n], pT.t[col0 + c4 * 128:col0 + (c4 + 4) * 128, s0:s0 + n].rearrange("(c p) n -> p c n", p=128), xin, r=[pT], w=[xin])
                for cc in range(4):
                    c = c4 + cc
                    I("act", "activation", out=xsq[:, 0:n], in_=xin[:, cc, 0:n], func=AF.Square, r=[xin], w=[xsq])
                    I("pe", "matmul", ps_n[:, 0:n], lhsT=ones[:], rhs=xsq[:, 0:n], start=(c == 0), stop=(c == nch - 1), r=[xsq, ones], w=[ps_n])
            self.rstd_from_ps(ps_n, rs, 128, n, inv_n, 1e-6)
            for c4 in range(0, nch, 4):
                S.dma("sp", xin[:, :, 0:n], pT.t[col0 + c4 * 128:col0 + (c4 + 4) * 128, s0:s0 + n].rearrange("(c p) n -> p c n", p=128), xin, r=[pT], w=[xin])
                for cc in range(4):
                    c = c4 + cc
                    I("dve", "tensor_tensor", out=xin[:, cc, 0:n], in0=xin[:, cc, 0:n], in1=rs[:, 0:n], op=ALU.mult, r=[xin, rs], w=[xin])
                    I("act", "activation", out=xnb[:, c, 0:n], in_=xin[:, cc, 0:n], func=AF.Identity, scale=mg[:, l, gcol0 + c:gcol0 + c + 1], r=[xin, mg], w=[xnb])

        def head_norm(src, rows, n, gain_ap, out_ap, out_buf):
            I("act", "activation", out=xsq[0:rows, 0:n], in_=src[0:rows, 0:n], func=AF.Square, r=[src], w=[xsq])
            I("pe", "matmul", ps_n[0:rows, 0:n], lhsT=ones[0:rows, 0:rows], rhs=xsq[0:rows, 0:n], start=True, stop=True, r=[xsq, ones], w=[ps_n])
            self.rstd_from_ps(ps_n, rs2, rows, n, 1.0 / rows, 1e-6)
            I("dve", "tensor_tensor", out=tmp[0:rows, 0:n], in0=src[0:rows, 0:n], in1=rs2[0:rows, 0:n], op=ALU.mult, r=[src, rs2], w=[tmp])
            I("act", "activation", out=out_ap, in_=tmp[0:rows, 0:n], func=AF.Identity, scale=gain_ap, r=[tmp, mg, qsc], w=[out_buf])

        def rope_apply(n, out_ap, out_buf):
            I("dve", "tensor_copy", out=xrb[:, 0:n], in_=tmp2[:, 0:n], r=[tmp2], w=[xrb])
            I("pe", "matmul", ps_n[0:64, 0:n], lhsT=rotb[:], rhs=xrb[:, 0:n], start=True, stop=True, r=[rotb, xrb], w=[ps_n])
            I("dve", "tensor_tensor", out=tmp[0:64, 0:n], in0=ps_n[0:64, 0:n], in1=ropeb[:, 1, 0:n], op=ALU.mult, r=[ps_n, ropeb], w=[tmp])
            I("dve", "tensor_tensor", out=tmp2[:, 0:n], in0=tmp2[:, 0:n], in1=ropeb[:, 0, 0:n], op=ALU.mult, r=[tmp2, ropeb], w=[tmp2])
            I("dve", "tensor_tensor", out=out_ap, in0=tmp2[:, 0:n], in1=tmp[0:64, 0:n], op=ALU.add, r=[tmp2, tmp], w=[out_buf])

        def load_rope(s0, n):
            if s0 >= 256:
                S.dma("act", ropeb[:, :, 0:n], self.ext["ropeT"].t[:, :, s0 - 256:s0 - 256 + n], ropeb, w=[ropeb])

        wqi = 0
        for g in range(2):
            for bi, (s0, n) in enumerate(blocks):
                norm_in(c_kv, 4, s0, n, 12, 1.0 / 512)
                for hh in range(4):
                    h = g * 4 + hh
                    for c in range(4):
                        I("pe", "matmul", ps_a[:, 0:n], lhsT=wkv[:, c, h * 256:h * 256 + 128], rhs=xnb[:, c, 0:n], start=(c == 0), stop=(c == 3), r=[wkv, xnb], w=[ps_a])
                    head_norm(ps_a, 128, n, mg[:, l, 18:19], kT[:, hh, s0:s0 + n], kT)
                for tt in range(n // 128):
                    for hh in range(4):
                        h = g * 4 + hh
                        for c in range(4):
                            I("pe", "matmul", ps_b[:, hh * 128:(hh + 1) * 128], lhsT=xnb[:, c, tt * 128:(tt + 1) * 128],
                              rhs=wkv[:, c, h * 256 + 128:h * 256 + 256], start=(c == 0), stop=(c == 3), r=[wkv, xnb], w=[ps_b])
                    ti = s0 // 128 + tt
                    I("act", "copy", out=vt[:, ti, :], in_=ps_b[:, 0:512], r=[ps_b], w=[vt])
                if g == 0:
                    load_rope(s0, n)
                    S.dma("sp", krin[:, 0:n], pT.t[c_kr:c_kr + 64, s0:s0 + n], krin, r=[pT], w=[krin])
                    head_norm(krin, 64, n, mg[0:64, l, 19:20], tmp2[:, 0:n], tmp2)
                    if s0 >= 256:
                        rope_apply(n, krT[:, s0:s0 + n], krT)
                    else:
                        I("dve", "tensor_copy", out=krT[:, s0:s0 + n], in_=tmp2[:, 0:n], r=[tmp2], w=[krT])
            for bi, (s0, n) in enumerate(blocks):
                norm_in(c_q, 12, s0, n, 0, 1.0 / 1536)
                load_rope(s0, n)
                kcs = list(range(2)) if bi == 0 else list(range(34))
                for hh in range(4):
                    h = g * 4 + hh
                    wqh = wq[wqi % 2]
                    wqi += 1
                    S.dma("act", wqh[:], self.wuqb.t[l, h], wqh, r=[self.wuqb], w=[wqh])
                    for c in range(12):
                        I("pe", "matmul", ps_a[:, 0:n], lhsT=wqh[:, c, 0:128], rhs=xnb[:, c, 0:n], start=(c == 0), stop=(c == 11), r=[wqh, xnb], w=[ps_a])
                    for c in range(12):
                        I("pe", "matmul", ps_b[0:64, 0:n], lhsT=wqh[:, c, 128:192], rhs=xnb[:, c, 0:n], start=(c == 0), stop=(c == 11), r=[wqh, xnb], w=[ps_b])
                    head_norm(ps_a, 128, n, qsc[:, 0:1], qnb[:, 0:n], qnb)
                    head_norm(ps_b, 64, n, qsc[0:64, 1:2], tmp2[:, 0:n], tmp2)
                    if s0 >= 256:
                        rope_apply(n, qrb[:, 0:n], qrb)
                    else:
                        I("dve", "tensor_copy", out=qrb[:, 0:n], in_=tmp2[:, 0:n], r=[tmp2], w=[qrb])
                    for ki, kc in enumerate(kcs):
                        ps = ps_s[ki % 2]
                        pbb = pb[ki % 2]
                        I("pe", "matmul", ps[:, 0:n], lhsT=kT[:, hh, kc * 128:(kc + 1) * 128], rhs=qnb[:, 0:n], start=True, stop=False, r=[kT, qnb], w=[ps])
                        I("pe", "matmul", ps[:, 0:n], lhsT=krT[:, kc * 128:(kc + 1) * 128], rhs=qrb[:, 0:n], start=False, stop=True, r=[krT, qrb], w=[ps])
                        I("act", "activation", out=pbb[:, 0:n], in_=ps[:, 0:n], func=AF.Exp, bias=nbias[:, 0:1], r=[ps, nbias], w=[pbb])
                        first, last = (ki == 0), (ki == len(kcs) - 1)
                        I("pe", "matmul", ps_o[:, 0:n], lhsT=vt[:, kc, hh * 128:(hh + 1) * 128], rhs=pbb[:, 0:n], start=first, stop=last, r=[vt, pbb], w=[ps_o])
                        I("pe", "matmul", ps_d[:, 0:n], lhsT=ones[:], rhs=pbb[:, 0:n], start=first, stop=last, r=[ones, pbb], w=[ps_d])
                    I("dve", "reciprocal", out=rs2[:, 0:n], in_=ps_d[:, 0:n], r=[ps_d], w=[rs2])
                    I("dve", "tensor_tensor", out=ob[:, 0:n], in0=ps_o[:, 0:n], in1=rs2[:, 0:n], op=ALU.mult, r=[ps_o, rs2], w=[ob])
                    for (ap, off, ln) in self.mix_dst(mixT, 1024 + h * 128, 128, s0, n):
                        S.dma("pool", ap, ob[:, off:off + ln], ob, r=[ob], w=[mixT])
        S.pop()


    def gather_chunks(self, src, nch, name):
        S = self.S
        mid = S.dram(name + "_q", [nch, 4, 524288], BF16)
        dst = Buf(name + "_g")
        dst.t = [self.nc.dram_tensor(f"{name}_g{j}", [min(16, nch - 16 * j), 4, 2, 524288], BF16).ap() for j in range((nch + 15) // 16)]
        sem = Buf(name + "_sem")
        sem.persist = True
        for i in range(nch):
            S.coll("AllGather", src.t[i:i + 1, :], mid.t[i], QUADS, sem, r=[src], w=[mid])
        self.pending_g.append((mid, dst, sem, nch))
        return dst

    def gather_finish(self, n=1):
        S = self.S
        for _ in range(n):
            if not self.pending_g:
                return
            mid, dst, sem, nch = self.pending_g.pop(0)
            for i in range(nch):
                for q in range(4):
                    S.coll("AllGather", mid.t[i, q:q + 1, :], dst.t[i // 16][i % 16, q], P4, sem, r=[mid], w=[dst])

    def convert_big(self):
        S = self.S
        wo = self.ext_in("wo", [512 * self.NL, D], F32)
        src_o = S.dram("wo_src", [4 * self.NL, 524288], BF16)
        S.push()
        f = [S.sb([128, D], F32, "bf") for _ in range(2)]
        b = [S.sb([128, D], BF16, "bb") for _ in range(2)]
        eng = RR(["dve", "pool", "act"])
        it = 0
        for i in range(4 * self.NL):
            ff, bb = f[it % 2], b[it % 2]
            it += 1
            S.dma("sp", ff[:], wo.t[i * 128:(i + 1) * 128, :], ff, w=[ff])
            for q in range(4):
                copy_on(S, eng(), bb[:, q * 1024:(q + 1) * 1024], ff[:, q * 1024:(q + 1) * 1024], [ff], [bb])
            S.dma("pool", src_o.t[i].rearrange("(p n) -> p n", p=128), bb[:], bb, r=[bb], w=[src_o])
        self.wo_g = self.gather_chunks(src_o, 4 * self.NL, "wo")
        self.moe_g = {}
        if self.moe_on:
            for nm in ("w1", "w3", "w2"):
                nle = 2 * self.NL
                w = self.ext_in("m" + nm, [nle, D if nm != "w2" else 1024, 1024 if nm != "w2" else D], F32)
                src = S.dram(nm + "_src", [nle * 8, 524288], BF16)
                for i in range(nle):
                    if nm == "w2":
                        for part in range(8):
                            ff, bb = f[it % 2], b[it % 2]
                            it += 1
                            S.dma("sp", ff[:], w.t[i, part * 128:(part + 1) * 128, :], ff, w=[ff])
                            for q in range(4):
                                copy_on(S, eng(), bb[:, q * 1024:(q + 1) * 1024], ff[:, q * 1024:(q + 1) * 1024], [ff], [bb])
                            S.dma("pool", src.t[i * 8 + part].rearrange("(p n) -> p n", p=128), bb[:], bb, r=[bb], w=[src])
                    else:
                        for c4 in range(0, 32, 4):
                            ff, bb = f[it % 2], b[it % 2]
                            it += 1
                            S.dma("sp", ff[:].rearrange("p (c n) -> p c n", c=4),
                                  w.t[i, c4 * 128:(c4 + 4) * 128, :].rearrange("(c p) n -> p c n", p=128), ff, w=[ff])
                            for q in range(4):
                                copy_on(S, eng(), bb[:, q * 1024:(q + 1) * 1024], ff[:, q * 1024:(q + 1) * 1024], [ff], [bb])
                            for dc in range(8):
                                S.dma("pool", src.t[i * 8 + dc].rearrange("(p c n) -> p c n", p=128, c=32)[:, c4:c4 + 4, :],
                                      bb[:].rearrange("p (c d n) -> p c d n", c=4, d=8)[:, :, dc, :], bb, r=[bb], w=[src])
                self.moe_g[nm] = self.gather_chunks(src, nle * 8, nm)
        S.pop()

    def wo_block(self, l, kc):
        per = 4 * self.NL
        g = l * 32 + kc
        r_, ch = g // per, g % per
        return self.wo_g.t[ch // 16][ch % 16, r_ % 4, r_ // 4].rearrange("(p n) -> p n", p=128)

    def moe_chunk(self, nm, l, e, part):
        nle = 2 * self.NL
        le = l * 16 + e
        r_, i = le // nle, le % nle
        ch = i * 8 + part
        return self.moe_g[nm].t[ch // 16][ch % 16, r_ % 4, r_ // 4]

    def bcast_table(self, tab_ap_fn, out_sb, ps):
        S = self.S
        I = S.i
        S.push()
        dg = [S.sb([128, 128], F32, "dg") for _ in range(2)]
        dh = [S.sb([128, 2, 128], BF16, "dh") for _ in range(2)]
        for c in range(32):
            d_, h_ = dg[c % 2], dh[c % 2]
            p = ps[(c // 4) % 2]
            I("dve", "tensor_scalar", out=d_[:], in0=self.identf[:], scalar1=tab_ap_fn(c), scalar2=None, op0=ALU.mult, r=[self.identf] + self.mods, w=[d_])
            I("dve", "tensor_copy", out=h_[:, 0, :], in_=d_[:], r=[d_], w=[h_])
            I("dve", "tensor_tensor", out=d_[:], in0=d_[:], in1=h_[:, 0, :], op=ALU.subtract, r=[d_, h_], w=[d_])
            I("dve", "tensor_copy", out=h_[:, 1, :], in_=d_[:], r=[d_], w=[h_])
            for q in range(2):
                I("pe", "matmul", p[:, (c % 4) * 128:(c % 4 + 1) * 128], lhsT=self.ones_bf[:], rhs=h_[:, q, :], start=(q == 0), stop=(q == 1), r=[h_, self.ones_bf], w=[p])
            if c % 4 == 3:
                I("act", "copy", out=out_sb[:, (c - 3) * 128:(c + 1) * 128], in_=p[:, :], r=[p], w=[out_sb])
        S.pop()

    def outproj_stage(self, l, mix_all, h_src, h_mid):
        S = self.S
        I = S.i
        S.push()
        idx = S.sb([128, 5, 32], I32, "mixidx")
        S.dma("sp", idx[:], self.ext["mixidx"].t, idx, w=[idx])
        gab = S.sb([128, D], F32, "ga1bc")
        mixb = S.sb([128, 32, 512], BF16, "mixb")
        wb = [S.sb([128, 32, 512], BF16, "wob") for _ in range(2)]
        hb = S.sb([128, 4, D], F32, "ohb")
        ps = [S.ps([128, 512], F32, "ops") for _ in range(4)]
        tmp = [S.sb([128, 512], F32, "otmp") for _ in range(2)]
        flat = mix_all.t.rearrange("c r p n -> (c r p) n")
        blocks = [(0, 128, 1)] + [(128 + 512 * q, 512, 0) for q in range(4)]
        cur = None
        pi = 0
        wi = 0
        for bi, (t0, n, which) in enumerate(blocks):
            if cur != which:
                self.bcast_table(lambda c, which=which: self.modcol(which, l, 2, c), gab, ps[0:2])
                cur = which
            for kc in range(32):
                S.gather_rows(mixb[:, kc, :], flat, idx[:, bi, kc:kc + 1], mixb, r=[mix_all, idx], w=[mixb])
            nt = n // 128
            for ti in range(nt):
                S.dma("act", hb[:, ti, :], h_src.t[t0 + ti * 128:t0 + (ti + 1) * 128, :], hb, r=[h_src], w=[hb])
            for dcn in range(8):
                w = wb[wi % 2]
                wi += 1
                for kc in range(32):
                    S.dma("sp" if kc % 2 else "act", w[:, kc, :], self.wo_block(l, kc)[:, dcn * 512:(dcn + 1) * 512], w, r=[self.wo_g], w=[w])
                for ti in range(nt):
                    p = ps[pi % 4]
                    t_ = tmp[pi % 2]
                    pi += 1
                    for kc in range(32):
                        I("pe", "matmul", p[:, :], lhsT=mixb[:, kc, ti * 128:(ti + 1) * 128], rhs=w[:, kc, :], start=(kc == 0), stop=(kc == 31), r=[mixb, w], w=[p])
                    I("dve", "tensor_tensor", out=t_[:], in0=p[:, :], in1=gab[:, dcn * 512:(dcn + 1) * 512], op=ALU.mult, r=[p, gab], w=[t_])
                    I("dve", "tensor_tensor", out=hb[:, ti, dcn * 512:(dcn + 1) * 512], in0=hb[:, ti, dcn * 512:(dcn + 1) * 512], in1=t_[:], op=ALU.add, r=[hb, t_], w=[hb])
            for ti in range(nt):
                S.dma("pool", h_mid.t[t0 + ti * 128:t0 + (ti + 1) * 128, :], hb[:, ti, :], hb, r=[hb], w=[h_mid])
        S.pop()

    def moe_stage(self, l, n2T, h_mid, h_out, final_out=None):
        S = self.S
        I = S.i
        S.push()
        wr = S.sb([128, 32, 16], F32, "wrf")
        wrb = S.sb([128, 32, 16], BF16, "wrb")
        S.dma("sp", wr[:], self.ext["wrT"].t, wr, w=[wr])
        I("dve", "tensor_copy", out=wrb[:], in_=wr[:], r=[wr], w=[wrb])
        rb = S.sb([128, 16], F32, "rbias")
        S.dma("sp", rb[:], self.ext["rbias"].t, rb, w=[rb])
        selEb = S.sb([16, 16, 128], BF16, "selEb")
        I("dve", "tensor_copy", out=selEb[:], in_=self.identf[0:16, 0:16].unsqueeze(2).to_broadcast([16, 16, 128]), r=[self.identf], w=[selEb])
        gab = S.sb([128, D], F32, "ga2bc")
        nb = S.sb([128, 32, 512], BF16, "mnb")
        yacc = S.sb([128, 4, D], F32, "yacc")
        uT = S.sb([128, 8, 512], BF16, "uT")
        w13 = [S.sb([128, 32, 128], BF16, "w13") for _ in range(3)]
        w2b = [S.sb([128, 8, 512], BF16, "w2b") for _ in range(2)]
        gbc = S.sb([128, 512], F32, "gbc")
        sa = S.sb([128, 512], F32, "msa")
        rt = S.sb([128, 64], F32, "rt")
        gate = S.sb([128, 4, 16], F32, "gate")
        ghl = S.sb([128, 2, 16], BF16, "ghl")
        gT = S.sb([16, 2, 512], BF16, "gT")
        ps_a = [S.ps([128, 512], F32, "mpa") for _ in range(2)]
        ps_b = [S.ps([128, 512], F32, "mpb") for _ in range(2)]
        ps_y = [S.ps([128, 512], F32, "mpy") for _ in range(2)]
        ps_r = S.ps([128, 512], F32, "mpr")
        ps_t = S.ps([16, 512], BF16, "mpt")
        blocks = [(0, 128, 1)] + [(128 + 512 * q, 512, 0) for q in range(4)]
        cur = None
        wi = 0
        w2i = 0
        pi = 0
        for bi, (t0, n, which) in enumerate(blocks):
            if final_out is not None and which == 1:
                continue
            if cur != which:
                self.bcast_table(lambda c, which=which: self.modcol(which, l, 5, c), gab, ps_y)
                cur = which
            nt = n // 128
            S.dma("sp", nb[:, :, 0:n], n2T.t[:, t0:t0 + n].rearrange("(c p) n -> p c n", p=128), nb, r=[n2T], w=[nb])
            for ti in range(nt):
                for c in range(32):
                    I("pe", "matmul", ps_r[:, 0:16], lhsT=nb[:, c, ti * 128:(ti + 1) * 128], rhs=wrb[:, c, :], start=(c == 0), stop=(c == 31), r=[nb, wrb], w=[ps_r])
                sc = rt[:, 0:16]
                bs = rt[:, 16:32]
                I("act", "activation", out=sc, in_=ps_r[:, 0:16], func=AF.Sigmoid, r=[ps_r], w=[rt])
                I("dve", "tensor_tensor", out=bs, in0=sc, in1=rb[:], op=ALU.add, r=[rt, rb], w=[rt])
                b4 = bs.rearrange("p (g k) -> p g k", k=4)
                pairs = [(0, 1), (0, 2), (0, 3), (1, 2), (1, 3), (2, 3)]
                psum6 = rt[:, 32:56].rearrange("p (g k) -> p g k", k=6)
                for qi, (x, y) in enumerate(pairs):
                    I("dve", "tensor_tensor", out=psum6[:, :, qi:qi + 1], in0=b4[:, :, x:x + 1], in1=b4[:, :, y:y + 1], op=ALU.add, r=[rt], w=[rt])
                gs = rt[:, 56:60]
                I("dve", "tensor_reduce", out=gs, in_=psum6, axis=AX.X, op=ALU.max, r=[rt], w=[rt])
                gm = rt[:, 60:64]
                I("dve", "tensor_reduce", out=gm, in_=b4, axis=AX.X, op=ALU.max, r=[rt], w=[rt])
                eqm = rt[:, 32:48].rearrange("p (g k) -> p g k", k=4)
                I("dve", "tensor_tensor", out=eqm, in0=b4, in1=gm.unsqueeze(2).to_broadcast([128, 4, 4]), op=ALU.is_ge, r=[rt], w=[rt])
                I("dve", "scalar_tensor_tensor", out=eqm, in0=eqm, scalar=-1.0e9, in1=b4, op0=ALU.mult, op1=ALU.add, r=[rt], w=[rt])
                thr = rt[:, 48:52]
                I("dve", "tensor_reduce", out=thr, in_=eqm, axis=AX.X, op=ALU.max, r=[rt], w=[rt])
                gmax = rt[:, 52:53]
                I("dve", "tensor_reduce", out=gmax, in_=gs, axis=AX.X, op=ALU.max, r=[rt], w=[rt])
                ing = rt[:, 56:60]
                I("dve", "tensor_scalar", out=ing, in0=gs, scalar1=gmax, scalar2=None, op0=ALU.is_ge, r=[rt], w=[rt])
                sel = rt[:, 32:48].rearrange("p (g k) -> p g k", k=4)
                I("dve", "tensor_tensor", out=sel, in0=b4, in1=thr.unsqueeze(2).to_broadcast([128, 4, 4]), op=ALU.is_ge, r=[rt], w=[rt])
                I("dve", "tensor_tensor", out=sel, in0=sel, in1=ing.unsqueeze(2).to_broadcast([128, 4, 4]), op=ALU.mult, r=[rt], w=[rt])
                g_ = gate[:, ti, :]
                I("dve", "tensor_tensor", out=g_, in0=rt[:, 32:48], in1=sc, op=ALU.mult, r=[rt], w=[gate])
                den = rt[:, 53:54]
                I("dve", "tensor_reduce", out=den, in_=g_, axis=AX.X, op=ALU.add, r=[gate], w=[rt])
                I("dve", "reciprocal", out=den, in_=den, r=[rt], w=[rt])
                I("dve", "tensor_scalar", out=g_, in0=g_, scalar1=den, scalar2=None, op0=ALU.mult, r=[gate, rt], w=[gate])
                I("dve", "tensor_copy", out=ghl[:, 0, :], in_=g_, r=[gate], w=[ghl])
                I("dve", "tensor_tensor", out=rt[:, 32:48], in0=g_, in1=ghl[:, 0, :], op=ALU.subtract, r=[gate, ghl], w=[rt])
                I("dve", "tensor_copy", out=ghl[:, 1, :], in_=rt[:, 32:48], r=[rt], w=[ghl])
                for q in range(2):
                    I("pe", "transpose", out=ps_t[:, q * 128:(q + 1) * 128], in_=ghl[:, q, :], identity=self.ident[:], r=[ghl, self.ident], w=[ps_t])
                I("act", "copy", out=gT[:, :, ti * 128:(ti + 1) * 128], in_=ps_t[:, 0:256].rearrange("p (q n) -> p q n", q=2), r=[ps_t], w=[gT])
            if "gate" in self.dbg and l == 0:
                S.dma("sp", self.outs["o_gate"].t[t0:t0 + n, :].rearrange("(t p) e -> p t e", p=128), gate[:, 0:nt, :], gate, r=[gate], w=[self.outs["o_gate"]])
            if not self.moe_on:
                continue
            for e in range(16):
                for q in range(2):
                    I("pe", "matmul", ps_r[:, 0:n], lhsT=selEb[:, e, :], rhs=gT[:, q, 0:n], start=(q == 0), stop=(q == 1), r=[selEb, gT], w=[ps_r])
                I("act", "copy", out=gbc[:, 0:n], in_=ps_r[:, 0:n], r=[ps_r], w=[gbc])
                for dc in range(8):
                    wa, wc = w13[wi % 3], w13[(wi + 1) % 3]
                    wi += 2
                    S.dma("sp", wa[:], self.moe_chunk("w1", l, e, dc).rearrange("(p c n) -> p c n", p=128, c=32), wa, r=[self.moe_g["w1"]], w=[wa])
                    S.dma("act", wc[:], self.moe_chunk("w3", l, e, dc).rearrange("(p c n) -> p c n", p=128, c=32), wc, r=[self.moe_g["w3"]], w=[wc])
                    pa, pb_ = ps_a[pi % 2], ps_b[pi % 2]
                    pi += 1
                    for c in range(32):
                        I("pe", "matmul", pa[:, 0:n], lhsT=wa[:, c, :], rhs=nb[:, c, 0:n], start=(c == 0), stop=(c == 31), r=[wa, nb], w=[pa])
                    for c in range(32):
                        I("pe", "matmul", pb_[:, 0:n], lhsT=wc[:, c, :], rhs=nb[:, c, 0:n], start=(c == 0), stop=(c == 31), r=[wc, nb], w=[pb_])
                    I("act", "activation", out=sa[:, 0:n], in_=pa[:, 0:n], func=AF.Silu, r=[pa], w=[sa])
                    I("dve", "tensor_tensor", out=sa[:, 0:n], in0=sa[:, 0:n], in1=pb_[:, 0:n], op=ALU.mult, r=[sa, pb_], w=[sa])
                    I("dve", "tensor_tensor", out=uT[:, dc, 0:n], in0=sa[:, 0:n], in1=gbc[:, 0:n], op=ALU.mult, r=[sa, gbc], w=[uT])
                for dcn in range(8):
                    w2 = w2b[w2i % 2]
                    w2i += 1
                    for dc in range(8):
                        S.dma("sp" if dc % 2 else "act", w2[:, dc, :],
                              self.moe_chunk("w2", l, e, dc).rearrange("(p n) -> p n", p=128)[:, dcn * 512:(dcn + 1) * 512], w2, r=[self.moe_g["w2"]], w=[w2])
                    for ti in range(nt):
                        py = ps_y[(dcn * 4 + ti) % 2]
                        for dc in range(8):
                            I("pe", "matmul", py[:, :], lhsT=uT[:, dc, ti * 128:(ti + 1) * 128], rhs=w2[:, dc, :], start=(dc == 0), stop=(dc == 7), r=[uT, w2], w=[py])
                        ya = yacc[:, ti, dcn * 512:(dcn + 1) * 512]
                        if e == 0:
                            I("act", "copy", out=ya, in_=py[:, :], r=[py], w=[yacc])
                        else:
                            I("dve", "tensor_tensor", out=ya, in0=ya, in1=py[:, :], op=ALU.add, r=[yacc, py], w=[yacc])
            for ti in range(nt):
                r0 = t0 + ti * 128
                I("dve", "tensor_tensor", out=yacc[:, ti, :], in0=yacc[:, ti, :], in1=gab[:], op=ALU.mult, r=[yacc, gab], w=[yacc])
                for dcn in range(8):
                    S.dma("sp", sa[:], h_mid.t[r0:r0 + 128, dcn * 512:(dcn + 1) * 512], sa, r=[h_mid], w=[sa])
                    I("dve", "tensor_tensor", out=yacc[:, ti, dcn * 512:(dcn + 1) * 512], in0=yacc[:, ti, dcn * 512:(dcn + 1) * 512], in1=sa[:], op=ALU.add, r=[yacc, sa], w=[yacc])
                if final_out is not None:
                    if r0 >= 128:
                        S.dma("pool", final_out.t[r0 - 128:r0, :], yacc[:, ti, :], yacc, r=[yacc], w=[final_out])
                else:
                    S.dma("pool", h_out.t[r0:r0 + 128, :], yacc[:, ti, :], yacc, r=[yacc], w=[h_out])
        S.pop()

    def build(self, NL=2, stages=("conv", "mla", "rwkv", "tok", "moe")):
        S = self.S
        self.NL = NL
        self.moe_on = "moe" in stages
        self.ext = {}
        self.consts()
        for nm, shp, dt in (("n1g", [128, 2, 32], F32), ("n2g", [128, 2, 32], F32), ("cw", [128, 2, 4, 31], F32), ("cb", [128, 2, 4, 3], F32),
                            ("mlag", [128, 2, 24], F32), ("ropeT", [64, 2, 4096], F32), ("rotm", [64, 64], F32),
                            ("mixidx", [128, 5, 32], I32), ("wrT", [128, 32, 16], F32), ("rbias", [128, 16], F32),
                            ("rwt", [64, 2, 8, 19], F32), ("rwl", [64, 2, 2, 2, 3], F32), ("rw2", [64, 2, 2, 2, 512], F32),
                            ("rmask", [64, 2, 192], F32), ("rg2", [128, 2, 2, 512], F32), ("rgl", [128, 2, 2, 3], F32)):
            self.ext[nm] = self.ext_in(nm, shp, dt)
        hx = self.ext_in("hx", [MT, D], F32)
        if "gate" in self.dbg:
            self.ext_out("o_gate", [MT, 16], F32)
        self.mod_stage()
        self.convert_win()
        self.convert_mla()
        if "tok" in stages:
            self.convert_big()
        nT_my = S.dram("nT_my", [D, MT], BF16)
        nT_all = S.dram("nT_all", [16, 2, 256, MT], BF16)
        n2T = S.dram("n2T", [D, MT], BF16)
        pT = S.dram("pT", [NCOL, T], F32)
        mixT = S.dram("mixT", [2, 5, 2048, 512], BF16)
        mix_all = S.dram("mix_all", [20, 2, 1024, 512], BF16)
        h_mid = S.dram("h_mid", [MT, D], F32)
        h_nxt = S.dram("h_nxt", [MT, D], F32)
        out = self.ext_out("out", [2048, D], F32) if "tok" in stages else None
        h_cur = hx
        for l in range(NL):
            S.lane = 2 * l
            self.norm_stage(l, h_cur, "n1g", (0, 1), nT_my)
            self.pair_gather(nT_my, nT_all, 16, 256)
            self.inproj_stage(l, nT_all, pT)
            self.gather_finish(1)
            if "conv" in stages:
                self.conv_stage(l, pT, mixT)
            self.gather_finish(1)
            if "rwkv" in stages:
                self.rwkv_stage(l, pT, mixT)
            self.gather_finish(1)
            if "mla" in stages:
                self.mla_stage(l, pT, mixT)
            self.gather_finish(9)
            if "tok" in stages:
                S.lane = 2 * l + 1
                self.pair_gather(mixT, mix_all, 20, 1024, src_ap=mixT.t.rearrange("h b f n -> (h b f) n"))
                self.outproj_stage(l, mix_all, h_cur, h_mid)
                self.norm_stage(l, h_mid, "n2g", (3, 4), n2T)
                self.moe_stage(l, n2T, h_mid, h_nxt, final_out=(out if l == NL - 1 else None))
                h_cur = h_nxt
        if "mixT" in self.dbg:
            o = self.ext_out("o_mixT", [2, 5, 2048, 512], BF16)
            S.dma("sp", o.t, mixT.t, Buf("dd3"), r=[mixT], w=[o])
        if "hmid" in self.dbg:
            o = self.ext_out("o_hmid", [MT, D], F32)
            S.dma("sp", o.t, h_mid.t, Buf("dd4"), r=[h_mid], w=[o])
        if "n2T" in self.dbg:
            o = self.ext_out("o_n2T", [D, MT], BF16)
            S.dma("sp", o.t, n2T.t, Buf("dd5"), r=[n2T], w=[o])
        return S.emit()


OFF_CONV, OFF_MQ, OFF_RR, OFF_RG, OFF_RK, OFF_RV, OFF_RW, OFF_RA, OFF_MKV, OFF_MKR = (
    0, 2048, 3584, 4608, 4768, 5792, 6816, 6944, 7072, 7584)


def my_cols(hf):
    cols = []
    cols += list(range(OFF_CONV + hf * 512, OFF_CONV + hf * 512 + 512))
    cols += list(range(OFF_CONV + 1024 + hf * 512, OFF_CONV + 1024 + hf * 512 + 512))
    cols += list(range(OFF_MQ, OFF_MQ + 1536))
    cols += list(range(OFF_RR + hf * 512, OFF_RR + hf * 512 + 512))
    cols += list(range(OFF_RK + hf * 512, OFF_RK + hf * 512 + 512))
    cols += list(range(OFF_RV + hf * 512, OFF_RV + hf * 512 + 512))
    cols += list(range(OFF_RW, OFF_RW + 128))
    cols += list(range(OFF_RA, OFF_RA + 128))
    cols += list(range(OFF_MKV, OFF_MKV + 512))
    cols += list(range(OFF_RG, OFF_RG + 160))
    cols += list(range(OFF_MKR, OFF_MKR + 64))
    return np.array(cols)


def _rope_tables():
    half = 32
    inv_freq = (10000.0 ** (-np.arange(0, half, 2, dtype=np.float32) / half)).astype(np.float32)
    r = np.repeat(np.arange(64, dtype=np.float32), 64)
    cl = np.tile(np.arange(64, dtype=np.float32), 64)
    ar = r[:, None] * inv_freq
    ac = cl[:, None] * inv_freq
    ang = np.concatenate([ar, ar, ac, ac], -1).astype(np.float32)
    return np.ascontiguousarray(np.stack([np.cos(ang).T, np.sin(ang).T], 1).astype(np.float32))


def _rot_matrix():
    R = np.zeros((64, 64), np.float32)
    for i in range(16):
        R[16 + i, i] = -1.0
        R[i, 16 + i] = 1.0
        R[48 + i, 32 + i] = -1.0
        R[32 + i, 48 + i] = 1.0
    return R


def mix_index_table(hf):
    idx = np.zeros((128, 5, 32), np.int32)
    for blk in range(5):
        for kc in range(32):
            rank, feat0 = kc // 16, (kc % 16) * 128
            R0 = (hf * 5 + blk) * 2048 + feat0
            idx[:, blk, kc] = ((R0 // 1024) * 2 + rank) * 1024 + R0 % 1024 + np.arange(128)
    return idx


MIX_PERM = np.concatenate([np.concatenate([r * 512 + np.arange(512), 1024 + r * 512 + np.arange(512), 2048 + r * 1024 + np.arange(1024)])
                           for r in range(2)])
ROPE_T = _rope_tables()
ROT_M = _rot_matrix()


def pack_inputs(inp, core, NL=2):
    b, hf = core // 2, core % 2
    f = np.float32
    m = {}
    m["hx"] = np.ascontiguousarray(np.concatenate([inp["ctx"][b, hf * 128:(hf + 1) * 128], inp["x"][b, hf * 2048:(hf + 1) * 2048]], 0))
    cv = np.concatenate([inp["c"], inp["c_ctx"][None]], 0)
    m["cvT"] = np.ascontiguousarray(cv.reshape(5, 32, 128).transpose(2, 1, 0))
    m["wmod"] = np.ascontiguousarray(inp["w_mod"][:NL, :, core * 3072:(core + 1) * 3072])
    m["bmodT"] = np.ascontiguousarray(inp["b_mod"][:, core * 3072:(core + 1) * 3072].reshape(2, 24, 128).transpose(2, 0, 1))
    sel = np.zeros((128, 2, 5), f)
    sel[:, 0, b] = 1.0
    sel[:, 1, 4] = 1.0
    m["sel"] = sel
    m["c_ident"] = np.eye(128, dtype=f)
    m["n1g"] = np.ascontiguousarray(inp["norm1_g"].reshape(2, 32, 128).transpose(2, 0, 1))
    m["win"] = np.ascontiguousarray(inp["w_in"][:NL, :, my_cols(hf)])
    ch = hf * 512 + np.arange(512)
    m["cw"] = np.ascontiguousarray(inp["conv_dw"][:, :, ch].reshape(2, 31, 4, 128).transpose(3, 0, 2, 1))
    cb = np.stack([inp["conv_b"][:, ch], inp["conv_ln_g"][:, ch], inp["conv_ln_b"][:, ch]], -1)
    m["cb"] = np.ascontiguousarray(cb.reshape(2, 4, 128, 3).transpose(2, 0, 1, 3))
    mg = np.zeros((128, 2, 24), f)
    mg[:, :, 0:12] = inp["m_cq_g"].reshape(2, 12, 128).transpose(2, 0, 1)
    mg[:, :, 12:16] = inp["m_ckv_g"].reshape(2, 4, 128).transpose(2, 0, 1)
    mg[:, :, 16] = inp["m_qn_g"].T
    mg[:64, :, 17] = inp["m_qr_g"].T
    mg[:, :, 18] = inp["m_kn_g"].T
    mg[:64, :, 19] = inp["m_kr_g"].T
    m["mlag"] = mg
    m["ropeT"] = ROPE_T
    m["rotm"] = ROT_M
    heads = hf * 8 + np.arange(8)
    qc = (heads[:, None] * 192 + np.arange(192)[None]).reshape(-1)
    kc = (heads[:, None] * 256 + np.arange(256)[None]).reshape(-1)
    m["n2g"] = np.ascontiguousarray(inp["norm2_g"].reshape(2, 32, 128).transpose(2, 0, 1))
    chn = hf * 512 + np.arange(512)
    mu = inp["r_mu"]

    def hi(a):
        return a.reshape(2, 8, 64).transpose(2, 0, 1)
    rwt = np.zeros((64, 2, 8, 19), f)
    for qi, off in enumerate((OFF_RR, OFF_RK, OFF_RV)):
        mp = mu[:, 0, off - OFF_RR + chn]
        mn = mu[:, 1, off - OFF_RR + chn]
        rwt[..., qi * 3 + 0] = hi(1.0 - mp - mn)
        rwt[..., qi * 3 + 1] = hi(mp)
        rwt[..., qi * 3 + 2] = hi(mn)
    rwt[..., 9] = hi(inp["r_kk"][:, chn])
    rwt[..., 10] = hi(inp["r_ka"][:, chn])
    rwt[..., 11] = hi(1.0 - inp["r_ka"][:, chn])
    rwt[..., 12] = hi(inp["r_rk"].reshape(2, 1024)[:, chn])
    rwt[..., 13] = hi(inp["r_ln_g"][:, chn])
    rwt[..., 14] = hi(inp["r_ln_b"][:, chn])
    for dd in range(2):
        rwt[..., 15 + dd] = hi(inp["r_w0"][:, dd, chn])
        rwt[..., 17 + dd] = hi(inp["r_a0"][:, dd, chn])
    m["rwt"] = rwt
    rwl = np.zeros((64, 2, 2, 2, 3), f)
    for dd in range(2):
        for qi, off in enumerate((OFF_RW, OFF_RA)):
            cc = off - OFF_RR + dd * 64 + np.arange(64)
            mp, mn = mu[:, 0, cc], mu[:, 1, cc]
            rwl[:, :, dd, qi, 0] = (1.0 - mp - mn).T
            rwl[:, :, dd, qi, 1] = mp.T
            rwl[:, :, dd, qi, 2] = mn.T
    m["rwl"] = rwl
    rw2 = np.zeros((64, 2, 2, 2, 512), f)
    rw2[:, :, :, 0, :] = inp["r_w2"][:, :, :, chn].transpose(2, 0, 1, 3)
    rw2[:, :, :, 1, :] = inp["r_a2"][:, :, :, chn].transpose(2, 0, 1, 3)
    m["rw2"] = rw2
    tri = np.arange(64)
    up_s = (tri[:, None] < tri[None, :]).astype(f)
    up_i = (tri[:, None] <= tri[None, :]).astype(f)
    rmask = np.zeros((64, 2, 192), f)
    rmask[:, 0, 0:64], rmask[:, 0, 64:128], rmask[:, 0, 128:192] = up_s, up_i, up_s.T
    rmask[:, 1, 0:64], rmask[:, 1, 64:128], rmask[:, 1, 128:192] = up_s.T, up_i.T, up_s
    m["rmask"] = rmask
    rg2 = np.zeros((128, 2, 2, 512), f)
    rg2[:, :, 0, :] = inp["r_g2"][:, 0:128, :][:, :, chn].transpose(1, 0, 2)
    rg2[0:32, :, 1, :] = inp["r_g2"][:, 128:160, :][:, :, chn].transpose(1, 0, 2)
    m["rg2"] = rg2
    rgl = np.zeros((128, 2, 2, 3), f)
    cg = OFF_RG - OFF_RR + np.arange(160)
    mp, mn = mu[:, 0, cg], mu[:, 1, cg]
    for qi, (a_, b_, n_) in enumerate(((0, 128, 128), (128, 160, 32))):
        rgl[0:n_, :, qi, 0] = (1.0 - mp[:, a_:b_] - mn[:, a_:b_]).T
        rgl[0:n_, :, qi, 1] = mp[:, a_:b_].T
        rgl[0:n_, :, qi, 2] = mn[:, a_:b_].T
    m["rgl"] = rgl
    m["mixidx"] = mix_index_table(hf)
    m["wrT"] = np.ascontiguousarray(inp["w_router"].reshape(32, 128, 16).transpose(1, 0, 2))
    m["rbias"] = np.ascontiguousarray(np.broadcast_to(inp["router_bias"][None, :], (128, 16))).astype(f)
    if "w_out" in inp:
        wo = inp["w_out"][:NL][:, MIX_PERM, :].reshape(NL * 4096, 4096)
        m["wo"] = np.ascontiguousarray(wo[core * 512 * NL:(core + 1) * 512 * NL])
    if "moe_w1" in inp:
        nle = 2 * NL
        for nm in ("w1", "w3", "w2"):
            w = inp["moe_" + nm][:NL]
            w = w.reshape((NL * 16,) + w.shape[2:])
            m["m" + nm] = np.ascontiguousarray(w[core * nle:(core + 1) * nle])
    m["wuq"] = np.ascontiguousarray(inp["m_w_uq"][:NL, :, qc])
    m["wukv"] = np.ascontiguousarray(inp["m_w_ukv"][:NL, :, kc])
    return m


def kernel(**inputs):
    inp = {k: np.asarray(v) for k, v in inputs.items()}
    nc = bass.Bass("TRN2", target_bir_lowering=False)
    k = K(nc)
    k.build(NL=2)
    need = None
    maps = []
    for c in range(8):
        m = pack_inputs(inp, c, 2)
        maps.append(m)
    res = run_bass_kernel_spmd(nc, maps, core_ids=list(range(8)))
    out = np.zeros((4, 4096, 4096), np.float32)
    for c in range(8):
        b, hf = c // 2, c % 2
        out[b, hf * 2048:(hf + 1) * 2048] = res.results[c]["out"]
    return out
```
